# Optimizing a Trainium2 kernel written in Bass

```python
import math
import jax
import jax.numpy as jnp
from jax import lax
import numpy as np

D_MODEL = 2048
BATCH = 16
SEQ = 2048
DEPTH = 1

BLK = 128
NEG_INF = -1e30
RMS_EPS = 1e-6
SUBLN_EPS = 1e-5
N_BUCKETS = 32
MAX_DISTANCE = 2048
A_HEADS = 8
A_HEAD_DIM = 128
DILATED_GROUPS = ((128, 1), (512, 4), (2048, 16))
N_DGROUPS = 3
B_HEADS_PER_GROUP = 8
B_HEAD_DIM = 64
N_BIAS_HEADS = A_HEADS + N_DGROUPS * B_HEADS_PER_GROUP
A_QK_WIDTH = A_HEADS * 2 * A_HEAD_DIM
A_V_WIDTH = A_HEADS * 2 * A_HEAD_DIM
B_WIDTH = N_DGROUPS * B_HEADS_PER_GROUP * B_HEAD_DIM
IN_WIDTH = 2 * A_QK_WIDTH + A_V_WIDTH + 3 * B_WIDTH
A_OUT_WIDTH = A_V_WIDTH
B_OUT_WIDTH = B_HEADS_PER_GROUP * B_HEAD_DIM
N_EXPERT_GROUPS = 4
EXPERTS_PER_GROUP = 8
N_EXPERTS = N_EXPERT_GROUPS * EXPERTS_PER_GROUP
TOP_K_FINE = 2
D_EXPERT = D_MODEL // 2
PLE_DIM = 256

kernel_name = 'hybrid_diffattn_dilated_hmoe_block'


def rmsnorm(x, g, eps=RMS_EPS):
    xf = x.astype(jnp.float32)
    y = xf * lax.rsqrt(jnp.mean(xf * xf, axis=-1, keepdims=True) + eps)
    return (y * g.astype(jnp.float32)).astype(x.dtype)


def t5_bucket(dist):
    dist = jnp.maximum(dist, 0)
    max_exact = N_BUCKETS // 2
    d_f = jnp.maximum(dist, 1).astype(jnp.float32)
    large = max_exact + (jnp.log(d_f / max_exact) / math.log(MAX_DISTANCE / max_exact)
                         * (N_BUCKETS - max_exact)).astype(jnp.int32)
    large = jnp.minimum(large, N_BUCKETS - 1)
    return jnp.where(dist < max_exact, dist, large)


def diff_attention(q, k, v, lam, bias_tab):
    S_ = q.shape[1]
    scale = A_HEAD_DIM ** -0.5
    outs = []
    for blk in range(S_ // BLK):
        s0 = blk * BLK
        kv_len = s0 + BLK
        qb = q[:, s0:s0 + BLK]
        kb = k[:, :kv_len]
        vb = v[:, :kv_len]
        s = jnp.einsum('bqhmd,bkhmd->bhmqk', qb, kb).astype(jnp.float32) * scale
        dist = (s0 + jnp.arange(BLK))[:, None] - jnp.arange(kv_len)[None, :]
        bias = bias_tab[t5_bucket(dist)].astype(jnp.float32).transpose(2, 0, 1)
        s = jnp.where((dist >= 0)[None, None, None], s + bias[None, :, None], NEG_INF)
        a = jax.nn.softmax(s, axis=-1)
        w = a[:, :, 0] - lam * a[:, :, 1]
        outs.append(jnp.einsum('bhqk,bkhe->bqhe', w.astype(v.dtype), vb))
    return jnp.concatenate(outs, axis=1)


def dilated_group(q, k, v, window, dilation, bias_tab):
    Bsz, S_, H, E = q.shape
    band = window // dilation
    L = S_ // dilation
    nq = -(-L // BLK)
    Lq = nq * BLK

    def streams(a):
        return a.reshape(Bsz, L, dilation, H, E).transpose(0, 2, 1, 3, 4)

    qs = jnp.pad(streams(q), ((0, 0), (0, 0), (0, Lq - L), (0, 0), (0, 0)))
    qs = qs.reshape(Bsz, dilation, nq, BLK, H, E)

    def windows(a):
        ap = jnp.pad(streams(a), ((0, 0), (0, 0), (BLK, Lq - L), (0, 0), (0, 0)))
        ap = ap.reshape(Bsz, dilation, nq + 1, BLK, H, E)
        return jnp.concatenate([ap[:, :, :-1], ap[:, :, 1:]], axis=3)

    kw = windows(k)
    vw = windows(v)
    s = jnp.einsum('bcnihe,bcnjhe->bcnhij', qs, kw).astype(jnp.float32) * (E ** -0.5)
    i = jnp.arange(BLK)[:, None]
    j = jnp.arange(2 * BLK)[None, :]
    rel = i - j + BLK
    kj = jnp.arange(nq)[:, None, None] * BLK + j[None] - BLK
    valid = (rel >= 0)[None] & (rel <= band)[None] & (kj >= 0) & (kj < L)
    bias = bias_tab[t5_bucket(rel * dilation)].astype(jnp.float32).transpose(2, 0, 1)
    s = jnp.where(valid[None, None, :, None], s + bias[None, None, None], NEG_INF)
    m = jnp.max(s, axis=-1, keepdims=True)
    e = jnp.exp(s - m)
    den = jnp.sum(e, axis=-1, keepdims=True)
    pr = e / den
    lse = (m + jnp.log(den))[..., 0]
    o = jnp.einsum('bcnhij,bcnjhe->bcnihe', pr.astype(v.dtype), vw)
    o = o.reshape(Bsz, dilation, Lq, H, E)[:, :, :L]
    o = o.transpose(0, 2, 1, 3, 4).reshape(Bsz, S_, H, E)
    lse = lse.transpose(0, 1, 2, 4, 3).reshape(Bsz, dilation, Lq, H)[:, :, :L]
    lse = lse.transpose(0, 2, 1, 3).reshape(Bsz, S_, H)
    return o, lse


def hier_moe(t, w_coarse, w_fine, w1, w3, w2):
    T, D = t.shape
    coarse = (t @ w_coarse).astype(jnp.float32)
    p_group = jax.nn.softmax(coarse, axis=-1)
    g_sel = jnp.argmax(coarse, axis=-1)
    pg_sel = jnp.take_along_axis(p_group, g_sel[:, None], axis=1)[:, 0]
    fine = jnp.einsum('td,gde->tge', t, w_fine).astype(jnp.float32)
    fine = jnp.take_along_axis(fine, g_sel[:, None, None], axis=1)[:, 0]
    top_v, top_i = lax.top_k(fine, TOP_K_FINE)
    gate = pg_sel[:, None] * jax.nn.softmax(top_v, axis=-1)
    expert_id = (g_sel[:, None] * EXPERTS_PER_GROUP + top_i).astype(jnp.int32)

    TK = T * TOP_K_FINE
    flat_e = expert_id.reshape(TK)
    flat_tok = jnp.repeat(jnp.arange(T, dtype=jnp.int32), TOP_K_FINE)
    flat_w = gate.reshape(TK).astype(t.dtype)
    order = jnp.argsort(flat_e)
    se = flat_e[order]
    counts = jnp.bincount(flat_e, length=N_EXPERTS)
    starts = jnp.cumsum(counts) - counts
    pcounts = ((counts + BLK - 1) // BLK) * BLK
    pends = jnp.cumsum(pcounts)
    pstarts = pends - pcounts
    dest = pstarts[se] + (jnp.arange(TK) - starts[se])
    n_blocks = TK // BLK + N_EXPERTS
    n_rows = n_blocks * BLK
    row_tok = jnp.full((n_rows,), T, jnp.int32).at[dest].set(flat_tok[order])
    row_w = jnp.zeros((n_rows,), t.dtype).at[dest].set(flat_w[order])
    blk_e = jnp.searchsorted(pends, jnp.arange(n_blocks) * BLK, side='right')
    blk_e = jnp.minimum(blk_e, N_EXPERTS - 1).astype(jnp.int32)
    t_pad = jnp.concatenate([t, jnp.zeros((1, D), t.dtype)], axis=0)
    xin = t_pad[row_tok].reshape(n_blocks, BLK, D)

    def expert_block(args):
        xb, e = args
        hdn = jax.nn.silu(xb @ w1[e]) * (xb @ w3[e])
        return hdn @ w2[e]

    yb = lax.map(expert_block, (xin, blk_e)).reshape(n_rows, D)
    y = jax.ops.segment_sum(yb * row_w[:, None], row_tok, num_segments=T + 1)[:T]
    return y


def setup_inputs(seed: int = 0) -> dict:
    key = jax.random.key(seed)
    ks = jax.random.split(key, 24)
    f32 = jnp.float32

    def nrm(k, shape, scale):
        return jax.random.normal(k, shape, f32) * scale

    def gain(k, shape):
        return 1.0 + 0.01 * jax.random.normal(k, shape, f32)

    return {
        'x': nrm(ks[0], (BATCH, SEQ, D_MODEL), 1.0),
        'p': nrm(ks[1], (DEPTH, BATCH, SEQ, PLE_DIM), 1.0),
        'rel_bias': nrm(ks[2], (N_BUCKETS, N_BIAS_HEADS), 0.2),
        'norm_mix_g': gain(ks[3], (DEPTH, D_MODEL)),
        'w_in': nrm(ks[4], (DEPTH, D_MODEL, IN_WIDTH), D_MODEL ** -0.5),
        'w_gate': nrm(ks[5], (DEPTH, D_MODEL, 2 * D_MODEL), D_MODEL ** -0.5),
        'lambda_q1': nrm(ks[6], (DEPTH, A_HEAD_DIM), 0.1),
        'lambda_k1': nrm(ks[7], (DEPTH, A_HEAD_DIM), 0.1),
        'lambda_q2': nrm(ks[8], (DEPTH, A_HEAD_DIM), 0.1),
        'lambda_k2': nrm(ks[9], (DEPTH, A_HEAD_DIM), 0.1),
        'subln_g': gain(ks[10], (DEPTH, 2 * A_HEAD_DIM)),
        'w_proj_a': nrm(ks[11], (DEPTH, A_OUT_WIDTH, D_MODEL), A_OUT_WIDTH ** -0.5),
        'w_proj_b': nrm(ks[12], (DEPTH, B_OUT_WIDTH, D_MODEL), B_OUT_WIDTH ** -0.5),
        'w_out': nrm(ks[13], (DEPTH, D_MODEL, D_MODEL), D_MODEL ** -0.5),
        'norm_ffn_g': gain(ks[14], (DEPTH, D_MODEL)),
        'w_coarse': nrm(ks[15], (DEPTH, D_MODEL, N_EXPERT_GROUPS), D_MODEL ** -0.5),
        'w_fine': nrm(ks[16], (DEPTH, N_EXPERT_GROUPS, D_MODEL, EXPERTS_PER_GROUP), D_MODEL ** -0.5),
        'w1': nrm(ks[17], (DEPTH, N_EXPERTS, D_MODEL, D_EXPERT), D_MODEL ** -0.5),
        'w3': nrm(ks[18], (DEPTH, N_EXPERTS, D_MODEL, D_EXPERT), D_MODEL ** -0.5),
        'w2': nrm(ks[19], (DEPTH, N_EXPERTS, D_EXPERT, D_MODEL), D_EXPERT ** -0.5),
        'norm_ple_g': gain(ks[20], (DEPTH, D_MODEL)),
        'w_ple_gate': nrm(ks[21], (DEPTH, D_MODEL, D_MODEL), D_MODEL ** -0.5),
        'w_ple_proj': nrm(ks[22], (DEPTH, PLE_DIM, D_MODEL), PLE_DIM ** -0.5),
        'final_norm_g': gain(ks[23], (D_MODEL,)),
    }


def reference(x, p, rel_bias, norm_mix_g, w_in, w_gate, lambda_q1, lambda_k1, lambda_q2,
              lambda_k2, subln_g, w_proj_a, w_proj_b, w_out, norm_ffn_g, w_coarse, w_fine,
              w1, w3, w2, norm_ple_g, w_ple_gate, w_ple_proj, final_norm_g):
    Bsz, S_, D = x.shape
    bias_a = rel_bias[:, :A_HEADS]
    for layer in range(DEPTH):
        h = rmsnorm(x, norm_mix_g[layer])
        z = h @ w_in[layer]
        o0 = 0
        qa = z[..., o0:o0 + A_QK_WIDTH].reshape(Bsz, S_, A_HEADS, 2, A_HEAD_DIM); o0 += A_QK_WIDTH
        ka = z[..., o0:o0 + A_QK_WIDTH].reshape(Bsz, S_, A_HEADS, 2, A_HEAD_DIM); o0 += A_QK_WIDTH
        va = z[..., o0:o0 + A_V_WIDTH].reshape(Bsz, S_, A_HEADS, 2 * A_HEAD_DIM); o0 += A_V_WIDTH
        qb = z[..., o0:o0 + B_WIDTH].reshape(Bsz, S_, N_DGROUPS, B_HEADS_PER_GROUP, B_HEAD_DIM); o0 += B_WIDTH
        kb = z[..., o0:o0 + B_WIDTH].reshape(Bsz, S_, N_DGROUPS, B_HEADS_PER_GROUP, B_HEAD_DIM); o0 += B_WIDTH
        vb = z[..., o0:o0 + B_WIDTH].reshape(Bsz, S_, N_DGROUPS, B_HEADS_PER_GROUP, B_HEAD_DIM)

        lam_init = 0.8 - 0.6 * math.exp(-0.3 * layer)
        lam = (jnp.exp(jnp.sum(lambda_q1[layer] * lambda_k1[layer]))
               - jnp.exp(jnp.sum(lambda_q2[layer] * lambda_k2[layer])) + lam_init).astype(jnp.float32)
        oa = diff_attention(qa, ka, va, lam, bias_a)
        oa = (rmsnorm(oa, subln_g[layer], SUBLN_EPS) * (1.0 - lam_init)).reshape(Bsz, S_, A_OUT_WIDTH)

        o_list = []
        lse_list = []
        for g, (window, dilation) in enumerate(DILATED_GROUPS):
            c0 = A_HEADS + g * B_HEADS_PER_GROUP
            og, lg = dilated_group(qb[:, :, g], kb[:, :, g], vb[:, :, g], window, dilation,
                                   rel_bias[:, c0:c0 + B_HEADS_PER_GROUP])
            o_list.append(og)
            lse_list.append(lg)
        alpha = jax.nn.softmax(jnp.stack(lse_list, axis=0), axis=0)
        ob = jnp.sum(alpha[..., None].astype(x.dtype) * jnp.stack(o_list, axis=0), axis=0)
        ob = ob.reshape(Bsz, S_, B_OUT_WIDTH)

        gates = jax.nn.sigmoid(h @ w_gate[layer])
        merged = gates[..., :D] * (oa @ w_proj_a[layer]) + gates[..., D:] * (ob @ w_proj_b[layer])
        x = x + merged @ w_out[layer]

        h2 = rmsnorm(x, norm_ffn_g[layer]).reshape(Bsz * S_, D)
        y = hier_moe(h2, w_coarse[layer], w_fine[layer], w1[layer], w3[layer], w2[layer])
        x = x + y.reshape(Bsz, S_, D)

        ple_gate = jax.nn.sigmoid(rmsnorm(x, norm_ple_g[layer]) @ w_ple_gate[layer])
        x = x + ple_gate * (p[layer] @ w_ple_proj[layer])
    return rmsnorm(x, final_norm_g)
```

```python
import math
from contextlib import ExitStack

import numpy as np
import concourse.bass as bass
import concourse.mybir as mybir
from concourse.bass_utils import run_bass_kernel_spmd

F32 = mybir.dt.float32
BF16 = mybir.dt.bfloat16
I32 = mybir.dt.int32
AF = mybir.ActivationFunctionType
ALU = mybir.AluOpType
AX = mybir.AxisListType

NCORES = 8
D = 2048
SEQ = 2048
T = 2 * SEQ
NT = T // 128
CAP = 512
CCH = 256
NE = 32
NSLOT = NE * CAP
LAM_INIT = 0.2
SC_A = 128 ** -0.5
SC_B = 64 ** -0.5
DIL = ((128, 1), (512, 4), (2048, 16))


class Buf:
    __slots__ = ("name", "writers", "readers")

    def __init__(self, name=""):
        self.name = name
        self.writers = {}
        self.readers = {}


class Rec:
    __slots__ = ("eng", "cnt", "is_dma", "sem")

    def __init__(self, eng, is_dma, sem):
        self.eng = eng
        self.is_dma = is_dma
        self.sem = sem
        self.cnt = None


class TB:
    def __init__(self, h, name=""):
        self.h = h
        self.b = Buf(name)

    def __getitem__(self, k):
        return self.h[k]


class Sch:
    def __init__(self, nc):
        self.nc = nc
        self.E = {"pe": nc.tensor, "act": nc.scalar, "dve": nc.vector, "pool": nc.gpsimd, "sp": nc.sync}
        self.sem = {e: nc.alloc_semaphore("s_" + e) for e in ("pe", "act", "dve", "pool")}
        self.cnt = {e: 0 for e in self.sem}
        self.waited = {e: {} for e in self.E}
        self.dsem = {}
        self.pending = []
        self.nsem = 4
        self.n_ops = 0
        self.rec = None

    def _deps(self, eng, reads, writes):
        need = {}

        def add(r, raw):
            if not r.is_dma:
                if r.eng == eng and (eng == "pe" or not raw):
                    return
            assert r.cnt is not None, "dependency on unsignaled op (%s)" % r.eng
            k = id(r.sem)
            if need.get(k, (None, 0))[1] < r.cnt:
                need[k] = (r.sem, r.cnt)

        for b in reads:
            for r in b.writers.values():
                add(r, True)
        for b in writes:
            if b.readers:
                for r in b.readers.values():
                    add(r, False)
                for r in b.writers.values():
                    add(r, False)
        return need

    def _emit_waits(self, eng, need):
        E = self.E[eng]
        w = self.waited[eng]
        for k, (sem, val) in need.items():
            if w.get(k, 0) < val:
                E.wait_ge(sem, val)
                w[k] = val

    def _update(self, rec, key, reads, writes):
        for b in writes:
            if b.readers:
                b.writers = {key: rec}
                b.readers = {}
            else:
                b.writers[key] = rec
        for b in reads:
            b.readers[key] = rec

    def record(self, f):
        self.rec = []
        f()
        r, self.rec = self.rec, None
        return r

    def replay(self, item):
        kind, args = item
        (self.op if kind == "op" else self.dma)(*args)

    def op(self, eng, fn, reads=(), writes=(), sig=True):
        if self.rec is not None:
            self.rec.append(("op", (eng, fn, list(reads), list(writes), sig)))
            return None
        reads = [x.b if isinstance(x, TB) else x for x in reads]
        writes = [x.b if isinstance(x, TB) else x for x in writes]
        need = self._deps(eng, reads, writes)
        self._emit_waits(eng, need)
        ins = fn(self.E[eng])
        rec = Rec(eng, False, self.sem[eng])
        if sig:
            self.cnt[eng] += 1
            ins.then_inc(self.sem[eng], 1)
            rec.cnt = self.cnt[eng]
            if eng == "pe" and self.pending:
                for p in self.pending:
                    p.cnt = rec.cnt
                self.pending = []
        else:
            assert eng == "pe"
            self.pending.append(rec)
        self._update(rec, eng, reads, writes)
        self.n_ops += 1
        return rec

    def dma(self, q, fn, reads=(), writes=()):
        if self.rec is not None:
            self.rec.append(("dma", (q, fn, list(reads), list(writes))))
            return None
        reads = [x.b if isinstance(x, TB) else x for x in reads]
        writes = [x.b if isinstance(x, TB) else x for x in writes]
        need = self._deps(q, reads, writes)
        self._emit_waits(q, need)
        b0 = writes[0]
        if id(b0) not in self.dsem:
            self.dsem[id(b0)] = [self.nc.alloc_semaphore("d_%d" % self.nsem), 0]
            self.nsem += 1
            assert self.nsem <= 100, "too many DMA semaphores"
        ds = self.dsem[id(b0)]
        ins = fn(self.E[q])
        ds[1] += 16
        ins.then_inc(ds[0], 16)
        rec = Rec(q, True, ds[0])
        rec.cnt = ds[1]
        self._update(rec, ("dma", id(ds[0])), reads, writes)
        self.n_ops += 1
        return rec

    def _all_need(self, skip=None):
        need = {}
        for e in self.sem:
            if e != skip and self.cnt[e] > 0:
                need[id(self.sem[e])] = (self.sem[e], self.cnt[e])
        for s, c in self.dsem.values():
            if c > 0:
                need[id(s)] = (s, c)
        return need

    def barrier(self):
        assert not self.pending
        for f in self.E:
            self._emit_waits(f, self._all_need(skip=f))

    def finish(self):
        assert not self.pending
        self._emit_waits("sp", self._all_need())


def build_nc(cfg=None):
    cfg = cfg or {}
    dbg = cfg.get('dbg', False)
    nc = bass.Bass("TRN2", target_bir_lowering=False)
    S = Sch(nc)

    def din(name, shape, dt=F32):
        return nc.dram_tensor(name, list(shape), dt, kind="ExternalInput").ap()

    def dscr(name, shape, dt):
        return nc.dram_tensor(name, list(shape), dt, kind=("ExternalOutput" if dbg else "Internal")).ap()

    x_d = din("x", [T, D])
    p_d = din("p", [T, 256])
    bias_a_d = din("bias_a", [8, 128, 2048])
    bias_b_d = din("bias_b", [6, 128, 1024])
    g_mix_d = din("norm_mix_g", [D])
    g_ffn_d = din("norm_ffn_g", [D])
    g_ple_d = din("norm_ple_g", [D])
    g_fin_d = din("final_norm_g", [D])
    w_in_d = din("w_in", [D, 10752])
    w_gate_d = din("w_gate", [D, 4096])
    lam_d = din("lam", [4, 128])
    subln_d = din("subln_g", [256])
    w_pa_d = din("w_proj_a", [D, D])
    w_pb_d = din("w_proj_b", [512, D])
    w_out_d = din("w_out", [D, D])
    w_rt_d = din("w_router", [D, 36])
    NEd = cfg.get('nEdecl', NE)
    w1_d = din("w1", [NEd, D, 1024])
    w3_d = din("w3", [NEd, D, 1024])
    w2_d = din("w2", [NEd, 1024, D])
    w_pg_d = din("w_ple_gate", [D, D])
    w_pp_d = din("w_ple_proj", [256, D])
    identf_d = din("identf", [128, 128])
    tri_d = din("tri", [128, 128])
    ecrow_d = din("ecrow", [128, NE])
    out_d = nc.dram_tensor("out", [T, D], F32, kind="ExternalOutput").ap()
    out_b = Buf("out")

    oaT_d = dscr("oaT", [2, 128, 16, SEQ], BF16)
    obT_d = dscr("obT", [2, 128, 4, SEQ], BF16)
    x1_d = dscr("x1", [T, D], F32)
    xin_d = dscr("xin", [NSLOT, D], BF16)
    ybuf_d = dscr("ybuf", [NSLOT + 128, D], F32)
    oaT_b, obT_b, x1_b, xin_b, ybuf_b = Buf("oaT"), Buf("obT"), Buf("x1"), Buf("xin"), Buf("ybuf")

    gstack = ExitStack()
    bc_reg = nc.gpsimd.to_reg(NSLOT - 1)
    bc_reg2 = nc.gpsimd.to_reg(NSLOT + 127)

    uid = [0]

    def mk(stack, name, shape, dt):
        uid[0] += 1
        return TB(stack.enter_context(nc.sbuf_tensor("sb%d_%s" % (uid[0], name), list(shape), dt)), name)

    def mkp(stack, name, shape, dt):
        uid[0] += 1
        return TB(stack.enter_context(nc.psum_tensor("ps%d_%s" % (uid[0], name), list(shape), dt)), name)

    identf = mk(gstack, "identf", [128, 128], F32)
    identb = mk(gstack, "identb", [128, 128], BF16)
    trib = mk(gstack, "trib", [128, 128], BF16)
    onesb = mk(gstack, "onesb", [128, 128], BF16)
    ecrow = mk(gstack, "ecrow", [128, NE], F32)
    neglam = mk(gstack, "neglam", [128, 1], F32)
    gsub = mk(gstack, "gsub", [128, 256], F32)
    slots = mk(gstack, "slots", [128, NT * 2], I32)
    slotg = mk(gstack, "slotg", [128, NT * 2], I32)
    gates = mk(gstack, "gates", [128, NT, 2], F32)
    cbase = mk(gstack, "cbase", [128, NE], F32)

    S.dma("sp", lambda e: e.dma_start(out=identf[:], in_=identf_d), writes=[identf])
    S.dma("sp", lambda e: e.dma_start(out=ecrow[:], in_=ecrow_d), writes=[ecrow])
    S.op("dve", lambda e: e.tensor_copy(out=identb[:], in_=identf[:]), reads=[identf], writes=[identb])
    S.op("dve", lambda e: e.memset(onesb[:], 1.0), writes=[onesb])
    S.op("dve", lambda e: e.memset(cbase[:], 0.0), writes=[cbase])
    with ExitStack() as st:
        trif = mk(st, "trif", [128, 128], F32)
        lamv = mk(st, "lamv", [128, 4, 128], F32)
        lamp = mk(st, "lamp", [128, 2, 128], F32)
        lams = mk(st, "lams", [128, 2], F32)
        lame = mk(st, "lame", [128, 2], F32)
        S.dma("sp", lambda e: e.dma_start(out=trif[:], in_=tri_d), writes=[trif])
        S.op("dve", lambda e: e.tensor_copy(out=trib[:], in_=trif[:]), reads=[trif], writes=[trib])
        for i in range(4):
            S.dma("sp", lambda e, i=i: e.dma_start(out=lamv[:, i, :], in_=lam_d[i].partition_broadcast(128)), writes=[lamv])
        S.dma("sp", lambda e: e.dma_start(out=gsub[:], in_=subln_d.partition_broadcast(128)), writes=[gsub])
        S.op("dve", lambda e: e.tensor_tensor(out=lamp[:, 0, :], in0=lamv[:, 0, :], in1=lamv[:, 1, :], op=ALU.mult), reads=[lamv], writes=[lamp])
        S.op("dve", lambda e: e.tensor_tensor(out=lamp[:, 1, :], in0=lamv[:, 2, :], in1=lamv[:, 3, :], op=ALU.mult), reads=[lamv], writes=[lamp])
        S.op("dve", lambda e: e.reduce_sum(out=lams[:, 0:1], in_=lamp[:, 0, :], axis=AX.X), reads=[lamp], writes=[lams])
        S.op("dve", lambda e: e.reduce_sum(out=lams[:, 1:2], in_=lamp[:, 1, :], axis=AX.X), reads=[lamp], writes=[lams])
        S.op("act", lambda e: e.activation(out=lame[:], in_=lams[:], func=AF.Exp), reads=[lams], writes=[lame])
        S.op("dve", lambda e: e.tensor_tensor(out=neglam[:], in0=lame[:, 1:2], in1=lame[:, 0:1], op=ALU.subtract), reads=[lame], writes=[neglam])
        S.op("dve", lambda e: e.tensor_scalar(out=neglam[:], in0=neglam[:], scalar1=-LAM_INIT, scalar2=None, op0=ALU.add), reads=[neglam], writes=[neglam])
        S.op("dve", lambda e: e.tensor_scalar(out=gsub[:], in0=gsub[:], scalar1=1.0 - LAM_INIT, scalar2=None, op0=ALU.mult), reads=[gsub], writes=[gsub])
        S.barrier()

    def wload(dst, dst_ap_fn, src_ap, kchunks, nsplit, reads=()):
        step = kchunks // nsplit
        for i in range(nsplit):
            S.dma("pool", lambda e, i=i: e.dma_start(out=dst_ap_fn(i * step, (i + 1) * step), in_=src_ap[:, i * step:(i + 1) * step, :]),
                  reads=list(reads), writes=[dst])

    def rstd_ops(ss, t1, rstd, inv_n, eps):
        S.op("dve", lambda e: e.tensor_scalar(out=t1[:], in0=ss[:], scalar1=inv_n, scalar2=eps, op0=ALU.mult, op1=ALU.add), reads=[ss], writes=[t1])
        S.op("act", lambda e: e.activation(out=t1[:], in_=t1[:], func=AF.Ln), reads=[t1], writes=[t1])
        S.op("act", lambda e: e.activation(out=rstd[:], in_=t1[:], func=AF.Exp, scale=-0.5), reads=[t1], writes=[rstd])

    evac_flip = [0]

    def evac(out_ap, in_ap, reads, writes):
        evac_flip[0] ^= 1
        if evac_flip[0]:
            S.op("act", lambda e: e.activation(out=out_ap, in_=in_ap, func=AF.Copy), reads=reads, writes=writes)
        else:
            S.op("dve", lambda e: e.tensor_copy(out=out_ap, in_=in_ap), reads=reads, writes=writes)

    w_in_v = w_in_d.rearrange("(k p) c -> p k c", p=128)

    for s in range(cfg.get('nseq', 2)):
        hstack = ExitStack()
        hT = mk(hstack, "hT", [128, 16, SEQ], BF16)
        with ExitStack() as st:
            stage = [mk(st, "stage%d" % i, [128, D], F32) for i in range(2)]
            junk = mk(st, "junk", [128, D], BF16)
            xs = mk(st, "xs", [128, D], BF16)
            gb = mk(st, "gb", [128, D], F32)
            ss = mk(st, "ss", [128, 1], F32)
            t1 = mk(st, "t1", [128, 1], F32)
            rstd = mk(st, "rstd", [128, 1], F32)
            Wq = mk(st, "Wq", [128, 16, 256], BF16)
            Wk = mk(st, "Wk", [128, 16, 256], BF16)
            Wv = mk(st, "Wv", [128, 16, 256], BF16)
            QT = mk(st, "QT", [128, 2, SEQ], BF16)
            KT = mk(st, "KT", [128, 2, SEQ], BF16)
            V = mk(st, "V", [128, 16, 264], BF16)
            EA = mk(st, "EA", [128, 2048], BF16)
            EB = EA
            PT = [mk(st, "PT%d" % i, [128, 2, 256], BF16) for i in range(3)]
            PTB = [mk(st, "PTB%d" % i, [128, 512], BF16) for i in range(3)]
            o1 = [mk(st, "o1_%d" % i, [128, 256], F32) for i in range(2)]
            oo = [mk(st, "oo_%d" % i, [128, 256], F32) for i in range(2)]
            junk2 = mk(st, "junk2", [128, 256], BF16)
            oab = [mk(st, "oab%d" % i, [128, 256], BF16) for i in range(2)]
            oaTu = mk(st, "oaTu", [128, 2, 512], BF16)
            rr = [mk(st, "rr%d" % i, [128, 2], F32) for i in range(2)]
            nl = [mk(st, "nl%d" % i, [128, 1], F32) for i in range(2)]
            ss2 = mk(st, "ss2", [128, 1], F32)
            t2 = mk(st, "t2", [128, 1], F32)
            rs2 = mk(st, "rs2", [128, 1], F32)
            accUD = mk(st, "accUD", [128, 2, 2, SEQ], F32)
            obTq = mk(st, "obTq", [128, 2, 512], BF16)
            acc = [mkp(st, "acc%d" % i, [128, 512], F32) for i in range(4)]
            pj = [mkp(st, "pj%d" % i, [128, 512], F32) for i in range(3)]
            ptb = [mkp(st, "ptb%d" % i, [128, 1024], BF16) for i in range(1)]

            S.dma("sp", lambda e: e.dma_start(out=gb[:], in_=g_mix_d.partition_broadcast(128)), writes=[gb])
            S.op("dve", lambda e: e.memset(V[:, :, 256:264], 1.0), writes=[V])

            for tt in range(16):
                r0 = s * SEQ + tt * 128
                stg = stage[tt % 2]
                S.dma("sp", lambda e, stg=stg, r0=r0: e.dma_start(out=stg[:], in_=x_d[r0:r0 + 128, :]), writes=[stg])
                S.op("act", lambda e, stg=stg: e.activation(out=junk[:], in_=stg[:], func=AF.Square, accum_out=ss[:]), reads=[stg], writes=[junk, ss])
                rstd_ops(ss, t1, rstd, 1.0 / D, 1e-6)
                S.op("dve", lambda e, stg=stg: e.scalar_tensor_tensor(out=xs[:], in0=stg[:], scalar=rstd[:], in1=gb[:], op0=ALU.mult, op1=ALU.mult),
                     reads=[stg, rstd, gb], writes=[xs])
                for j in range(2):
                    for k in range(8 * j, 8 * j + 8):
                        S.op("pe", lambda e, k=k: e.transpose(ptb[0][:, (k % 8) * 128:(k % 8 + 1) * 128], xs[:, k * 128:(k + 1) * 128], identb[:]),
                             reads=[xs, identb], writes=[ptb[0]], sig=(k % 8 == 7))
                    evac(hT[:, 8 * j:8 * j + 8, tt * 128:(tt + 1) * 128], ptb[0][:].rearrange("p (k t) -> p k t", k=8), [ptb[0]], [hT])

            units = [("A", h) for h in range(8)] + [("B", q * 3 + g) for q in range(2) for g in range(3)]
            if 'units' in cfg:
                units = cfg['units']
            pjn = [0]

            def next_pj():
                pjn[0] = (pjn[0] + 1) % 3
                return pj[pjn[0]]

            for kind, ui in units:
                if kind == "A":
                    h = ui
                    qc, kc, vc = h * 256, 2048 + h * 256, 4096 + h * 256
                    d, nblk = 1, 16
                else:
                    quad, g = divmod(ui, 3)
                    d = DIL[g][1]
                    nblk = (SEQ // d) // 128
                    qc = 6144 + g * 512 + quad * 256
                    kc = 7680 + g * 512 + quad * 256
                    vc = 9216 + g * 512 + quad * 256
                L = SEQ // d
                for Wt, c0 in ((Wq, qc), (Wk, kc), (Wv, vc)):
                    wload(Wt, lambda a, b, Wt=Wt: Wt[:, a:b, :], w_in_v[:, :, c0:c0 + 256], 16, 4)
                if kind == "A":
                    stg = stage[0]
                    S.dma("sp", lambda e, stg=stg, h=h: e.dma_start(out=stg[:], in_=bias_a_d[h]), writes=[stg])
                    S.op("act", lambda e, stg=stg: e.activation(out=EA[:], in_=stg[:], func=AF.Exp), reads=[stg], writes=[EA])
                elif not cfg.get('Bnobias'):
                    stg = stage[1]
                    S.dma("sp", lambda e, stg=stg, ui=ui: e.dma_start(out=stg[:, 0:1024], in_=bias_b_d[ui]), writes=[stg])
                    if not cfg.get('Bnoexp'):
                        S.op("act", lambda e, stg=stg: e.activation(out=EB[:, 0:1024], in_=stg[:, 0:1024], func=AF.Exp), reads=[stg], writes=[EB])
                def proj_half(dst, Wt, half, bsplit=False):
                    for tg in range(4):
                        bank = next_pj()
                        for k in range(16):
                            S.op("pe", lambda e, bank=bank, Wt=Wt, k=k, half=half, tg=tg: e.matmul(
                                bank[:, 0:512], lhsT=Wt[:, k, half * 128:(half + 1) * 128], rhs=hT[:, k, tg * 512:(tg + 1) * 512],
                                start=(k == 0), stop=(k == 15)), reads=[Wt, hT], writes=[bank], sig=(k == 15))
                        l0 = tg * 512 // d
                        if not bsplit:
                            if d == 1:
                                evac(dst[:, half, tg * 512:(tg + 1) * 512], bank[:, 0:512], [bank], [dst])
                            else:
                                oap = dst[:, half, :].rearrange("p (c l) -> p c l", c=d)[:, :, l0:l0 + 512 // d]
                                iap = bank[:, 0:512].rearrange("p (l c) -> p c l", c=d)
                                evac(oap, iap, [bank], [dst])
                        else:
                            for hh in range(2):
                                p0 = hh * 64
                                if d == 1:
                                    evac(dst[p0:p0 + 64, hh, tg * 512:(tg + 1) * 512], bank[p0:p0 + 64, 0:512], [bank], [dst])
                                else:
                                    oap = dst[p0:p0 + 64, hh, :].rearrange("p (c l) -> p c l", c=d)[:, :, l0:l0 + 512 // d]
                                    iap = bank[p0:p0 + 64, 0:512].rearrange("p (l c) -> p c l", c=d)
                                    evac(oap, iap, [bank], [dst])

                for half in range(2):
                    proj_half(QT, Wq, half)
                if kind == "A":
                    for half in range(2):
                        proj_half(KT, Wk, half)
                else:
                    S.op("dve", lambda e: e.memset(KT[64:128, 0, :], 0.0), writes=[KT])
                    S.op("dve", lambda e: e.memset(KT[0:64, 1, :], 0.0), writes=[KT])
                for sbk in range(16):
                    c_, n_ = divmod(sbk, nblk)
                    t0 = n_ * 128 * d + c_
                    bank = next_pj()
                    for k in range(16):
                        S.op("pe", lambda e, bank=bank, k=k, t0=t0, d=d: e.matmul(
                            bank[:, 0:256], lhsT=hT[:, k, t0:t0 + 127 * d + 1:d], rhs=Wv[:, k, :],
                            start=(k == 0), stop=(k == 15)), reads=[Wv, hT], writes=[bank], sig=(k == 15))
                    evac(V[:, sbk, 0:256], bank[:, 0:256], [bank], [V])

                if kind == "A":
                    steps = [(c, kb) for c in range(8) for kb in range(2 * c + 2)]

                    def qk_stage(i):
                        c, kb = steps[i]
                        qlo = 2 * c if kb <= 2 * c else 2 * c + 1
                        off = (qlo - 2 * c) * 128
                        bank = pj[i % 3]
                        pt = PT[i % 3]
                        for m in range(2):
                            S.op("pe", lambda e, bank=bank, m=m, kb=kb, qlo=qlo, c=c, off=off: e.matmul(
                                bank[:, m * 256 + off:m * 256 + 256], lhsT=KT[:, m, kb * 128:(kb + 1) * 128],
                                rhs=QT[:, m, qlo * 128:(2 * c + 2) * 128], start=True, stop=True),
                                reads=[KT, QT], writes=[bank], sig=(m == 1))
                        S.op("act", lambda e, bank=bank, pt=pt, off=off: e.activation(
                            out=pt[:, :, off:256], in_=bank[:, 0:512].rearrange("p (m q) -> p m q", m=2)[:, :, off:256],
                            func=AF.Exp, scale=SC_A), reads=[bank], writes=[pt])
                        e0 = (qlo - kb) * 128
                        e1 = (2 * c + 2 - kb) * 128
                        for m in range(2):
                            S.op("dve", lambda e, pt=pt, m=m, off=off, e0=e0, e1=e1: e.tensor_tensor(
                                out=pt[:, m, off:256], in0=pt[:, m, off:256], in1=EA[:, e0:e1], op=ALU.mult),
                                reads=[pt, EA], writes=[pt])

                    def pv_stage(i):
                        c, kb = steps[i]
                        qlo = 2 * c if kb <= 2 * c else 2 * c + 1
                        pt = PT[i % 3]
                        mm = [(m, qb) for m in range(2) for qb in range(qlo, 2 * c + 2)]
                        for ii, (m, qb) in enumerate(mm):
                            j = qb - 2 * c
                            a_ = acc[m * 2 + j]
                            S.op("pe", lambda e, a_=a_, pt=pt, m=m, j=j, kb=kb, qb=qb: e.matmul(
                                a_[:, 0:264], lhsT=pt[:, m, j * 128:(j + 1) * 128], rhs=V[:, kb, 0:264],
                                start=(kb == 0), stop=(kb == qb)), reads=[pt, V], writes=[a_], sig=(ii == len(mm) - 1))

                    def epi1(c):
                        for j in range(2):
                            a0, a1 = acc[j], acc[2 + j]
                            rr_, nl_, o1_, oo_ = rr[j], nl[j], o1[j], oo[j]
                            S.op("dve", lambda e, a0=a0, rr_=rr_: e.reciprocal(out=rr_[:, 0:1], in_=a0[:, 256:257]), reads=[a0], writes=[rr_])
                            S.op("dve", lambda e, a1=a1, rr_=rr_: e.reciprocal(out=rr_[:, 1:2], in_=a1[:, 256:257]), reads=[a1], writes=[rr_])
                            S.op("dve", lambda e, rr_=rr_, nl_=nl_: e.tensor_tensor(out=nl_[:], in0=rr_[:, 1:2], in1=neglam[:], op=ALU.mult), reads=[rr_, neglam], writes=[nl_])
                            S.op("act", lambda e, a0=a0, rr_=rr_, o1_=o1_: e.activation(out=o1_[:], in_=a0[:, 0:256], func=AF.Copy, scale=rr_[:, 0:1]), reads=[a0, rr_], writes=[o1_])
                            S.op("dve", lambda e, a1=a1, nl_=nl_, o1_=o1_, oo_=oo_: e.scalar_tensor_tensor(out=oo_[:], in0=a1[:, 0:256], scalar=nl_[:], in1=o1_[:], op0=ALU.mult, op1=ALU.add),
                                 reads=[a1, nl_, o1_], writes=[oo_])
                        for j in range(2):
                            oo_ = oo[j]
                            ob_ = oab[j]
                            S.op("act", lambda e, oo_=oo_: e.activation(out=junk2[:], in_=oo_[:], func=AF.Square, accum_out=ss2[:]), reads=[oo_], writes=[junk2, ss2])
                            rstd_ops(ss2, t2, rs2, 1.0 / 256, 1e-5)
                            S.op("dve", lambda e, ob_=ob_, oo_=oo_: e.scalar_tensor_tensor(out=ob_[:], in0=oo_[:], scalar=rs2[:], in1=gsub[:], op0=ALU.mult, op1=ALU.mult),
                                 reads=[oo_, rs2, gsub], writes=[ob_])

                    def epi2(c):
                        for j in range(2):
                            qb = 2 * c + j
                            ob_ = oab[j]
                            pb_ = ptb[0]
                            for hh in range(2):
                                S.op("pe", lambda e, pb_=pb_, hh=hh, ob_=ob_: e.transpose(pb_[:, hh * 128:(hh + 1) * 128], ob_[:, hh * 128:(hh + 1) * 128], identb[:]),
                                     reads=[ob_, identb], writes=[pb_], sig=(hh == 1))
                            ql = qb % 4
                            evac(oaTu[:, :, ql * 128:(ql + 1) * 128], pb_[:, 0:256].rearrange("p (k t) -> p k t", k=2), [pb_], [oaTu])
                            if ql == 3:
                                q0 = (qb - 3) * 128
                                S.dma("sp", lambda e, h=h, q0=q0: e.dma_start(out=oaT_d[s, :, 2 * h:2 * h + 2, q0:q0 + 512], in_=oaTu[:]),
                                      reads=[oaTu], writes=[oaT_b])

                    pend_epi2 = None
                    qk_stage(0)
                    qk_stage(1)
                    for i, (c, kb) in enumerate(steps):
                        if i + 2 < len(steps):
                            qk_stage(i + 2)
                        pv_stage(i)
                        if kb == 1 and pend_epi2 is not None:
                            epi2(pend_epi2)
                            pend_epi2 = None
                        if kb == 2 * c + 1:
                            epi1(c)
                            pend_epi2 = c
                    epi2(pend_epi2)
                else:
                    it = 0
                    EBv = EB[:, 0:1024].rearrange("p (h w q) -> p h w q", h=4, w=2)
                    for pair in range(2):
                        proj_half(KT, Wk, pair, bsplit=True)
                        def b_qk(sbk, it_):
                            c_, n_ = divmod(sbk, nblk)
                            hp = n_ > 0
                            bank = pj[it_ % 3]
                            pt = PTB[it_ % 3]
                            nw = 2 if hp else 1
                            i = 0
                            for hh in range(2):
                                for wh in range(nw):
                                    i += 1
                                    S.op("pe", lambda e, bank=bank, hh=hh, wh=wh, pair=pair, sbk=sbk: e.matmul(
                                        bank[:, (hh * 2 + wh) * 128:(hh * 2 + wh + 1) * 128],
                                        lhsT=KT[:, hh, (sbk - wh) * 128:(sbk - wh + 1) * 128],
                                        rhs=QT[:, pair, sbk * 128:(sbk + 1) * 128], start=True, stop=True),
                                        reads=[KT, QT], writes=[bank], sig=(i == 2 * nw))
                            ptv = pt[:].rearrange("p (h w q) -> p h w q", h=2, w=2)
                            bkv = bank[:, 0:512].rearrange("p (h w q) -> p h w q", h=2, w=2)
                            if hp:
                                S.op("act", lambda e, bank=bank, pt=pt: e.activation(out=pt[:], in_=bank[:, 0:512], func=AF.Exp, scale=SC_B),
                                     reads=[bank], writes=[pt])
                                S.op("dve", lambda e, pt=pt, pair=pair: e.tensor_tensor(out=pt[:], in0=pt[:], in1=EB[:, pair * 512:(pair + 1) * 512], op=ALU.mult),
                                     reads=[pt, EB], writes=[pt])
                            else:
                                S.op("act", lambda e, ptv=ptv, bkv=bkv: e.activation(out=ptv[:, :, 0, :], in_=bkv[:, :, 0, :], func=AF.Exp, scale=SC_B),
                                     reads=[bank], writes=[pt])
                                S.op("dve", lambda e, ptv=ptv, pair=pair: e.tensor_tensor(out=ptv[:, :, 0, :], in0=ptv[:, :, 0, :],
                                                                                         in1=EBv[:, pair * 2:pair * 2 + 2, 0, :], op=ALU.mult),
                                     reads=[pt, EB], writes=[pt])

                        def b_pv(sbk, it_):
                            c_, n_ = divmod(sbk, nblk)
                            hp = n_ > 0
                            pt = PTB[it_ % 3]
                            ut = acc[it_ % 4]
                            nw = 2 if hp else 1
                            ptv = pt[:].rearrange("p (h w q) -> p h w q", h=2, w=2)
                            for hh in range(2):
                                for wh in range(nw):
                                    kblk = sbk - wh
                                    S.op("pe", lambda e, ut=ut, kblk=kblk, pair=pair, ptv=ptv, hh=hh, wh=wh, nw=nw: e.matmul(
                                        ut[:, hh * 128:(hh + 1) * 128], lhsT=V[:, kblk, pair * 128:(pair + 1) * 128], rhs=ptv[:, hh, wh, :],
                                        start=(wh == 0), stop=(wh == nw - 1)), reads=[V, pt], writes=[ut], sig=False)
                            for hh in range(2):
                                for wh in range(nw):
                                    S.op("pe", lambda e, ut=ut, ptv=ptv, hh=hh, wh=wh, nw=nw: e.matmul(
                                        ut[:, (2 + hh) * 128:(3 + hh) * 128], lhsT=onesb[:], rhs=ptv[:, hh, wh, :],
                                        start=(wh == 0), stop=(wh == nw - 1)), reads=[onesb, pt], writes=[ut], sig=(hh == 1 and wh == nw - 1))
                            t0 = n_ * 128 * d + c_
                            for hh in range(2):
                                p0 = hh * 64
                                oap = accUD[p0:p0 + 64, pair, :, t0:t0 + 127 * d + 1:d]
                                iap = ut[p0:p0 + 64, 0:512].rearrange("p (u h q) -> p u h q", u=2, h=2)[:, :, hh, :]
                                if g == 0:
                                    S.op("dve", lambda e, oap=oap, iap=iap: e.tensor_copy(out=oap, in_=iap), reads=[ut], writes=[accUD])
                                else:
                                    S.op("dve", lambda e, oap=oap, iap=iap: e.tensor_tensor(out=oap, in0=oap, in1=iap, op=ALU.add), reads=[ut], writes=[accUD])

                        b_qk(0, it)
                        b_qk(1, it + 1)
                        for sbk in range(16):
                            if sbk + 2 < 16:
                                b_qk(sbk + 2, it + 2)
                            b_pv(sbk, it)
                            it += 1
                    if g == 2:
                        for pair in range(2):
                            S.op("dve", lambda e, pair=pair: e.reciprocal(out=accUD[:, pair, 1, :], in_=accUD[:, pair, 1, :]), reads=[accUD], writes=[accUD])
                        for tq in range(4):
                            for pair in range(2):
                                S.op("dve", lambda e, pair=pair, tq=tq: e.tensor_tensor(
                                    out=obTq[:, pair, :], in0=accUD[:, pair, 0, tq * 512:(tq + 1) * 512],
                                    in1=accUD[:, pair, 1, tq * 512:(tq + 1) * 512], op=ALU.mult), reads=[accUD], writes=[obTq])
                            S.dma("sp", lambda e, quad=quad, tq=tq: e.dma_start(out=obT_d[s, :, quad * 2:quad * 2 + 2, tq * 512:(tq + 1) * 512], in_=obTq[:]),
                                  reads=[obTq], writes=[obT_b])
            S.barrier()

        with ExitStack() as st:
          if cfg.get('C', True):
              oaTh = mk(st, "oaTh", [128, 16, 1024], BF16)
              obTh = mk(st, "obTh", [128, 4, 1024], BF16)
              mT = mk(st, "mT", [128, 16, 1024], BF16)
              Wg1 = [mk(st, "Wg1_%d" % i, [128, 16, 128], BF16) for i in range(2)]
              Wg2 = [mk(st, "Wg2_%d" % i, [128, 16, 128], BF16) for i in range(2)]
              Wa = [mk(st, "Wa_%d" % i, [128, 16, 128], BF16) for i in range(2)]
              Wb = [mk(st, "Wb_%d" % i, [128, 4, 128], BF16) for i in range(2)]
              Wo = mk(st, "Wo", [128, 16, 512], BF16)
              xt = [mk(st, "xt%d" % i, [128, 512], F32) for i in range(2)]
              x1t = [mk(st, "x1t%d" % i, [128, 512], F32) for i in range(2)]
              s1 = mk(st, "s1", [128, 512], F32)
              s2 = mk(st, "s2", [128, 512], F32)
              pG1 = mkp(st, "pG1", [128, 512], F32)
              pG2 = mkp(st, "pG2", [128, 512], F32)
              pPA = mkp(st, "pPA", [128, 512], F32)
              pPB = mkp(st, "pPB", [128, 512], F32)
              pO = [mkp(st, "pO%d" % i, [128, 512], F32) for i in range(2)]
              wg_v = w_gate_d.rearrange("(k p) c -> p k c", p=128)
              wa_v = w_pa_d.rearrange("(k p) c -> p k c", p=128)
              wb_v = w_pb_d.rearrange("(k p) c -> p k c", p=128)
              wo_v = w_out_d.rearrange("(k p) c -> p k c", p=128)
              for hf in range(2):
                  t0 = hf * 1024
                  for kk in range(4):
                      S.dma("sp", lambda e, kk=kk, t0=t0: e.dma_start(out=oaTh[:, kk * 4:(kk + 1) * 4, :], in_=oaT_d[s, :, kk * 4:(kk + 1) * 4, t0:t0 + 1024]),
                            reads=[oaT_b], writes=[oaTh])
                  S.dma("sp", lambda e, t0=t0: e.dma_start(out=obTh[:], in_=obT_d[s, :, :, t0:t0 + 1024]), reads=[obT_b], writes=[obTh])
                  for c in range(16):
                      w1_, w2_, wa_, wb_ = Wg1[c % 2], Wg2[c % 2], Wa[c % 2], Wb[c % 2]
                      wload(w1_, lambda a, b, w=w1_: w[:, a:b, :], wg_v[:, :, c * 128:(c + 1) * 128], 16, 2)
                      wload(w2_, lambda a, b, w=w2_: w[:, a:b, :], wg_v[:, :, 2048 + c * 128:2048 + (c + 1) * 128], 16, 2)
                      wload(wa_, lambda a, b, w=wa_: w[:, a:b, :], wa_v[:, :, c * 128:(c + 1) * 128], 16, 2)
                      wload(wb_, lambda a, b, w=wb_: w[:, a:b, :], wb_v[:, :, c * 128:(c + 1) * 128], 4, 1)
                      for tg in range(2):
                          ta = t0 + tg * 512
                          for bank, Wt, src, off_, nk in ((pG1, w1_, hT, ta, 16), (pG2, w2_, hT, ta, 16), (pPA, wa_, oaTh, tg * 512, 16), (pPB, wb_, obTh, tg * 512, 4)):
                              for k in range(nk):
                                  S.op("pe", lambda e, bank=bank, Wt=Wt, src=src, off_=off_, k=k, nk=nk: e.matmul(
                                      bank[:, 0:512], lhsT=Wt[:, k, :], rhs=src[:, k, off_:off_ + 512], start=(k == 0), stop=(k == nk - 1)),
                                      reads=[Wt, src], writes=[bank], sig=(k == nk - 1))
                          S.op("act", lambda e: e.activation(out=s1[:], in_=pG1[:, 0:512], func=AF.Sigmoid), reads=[pG1], writes=[s1])
                          S.op("act", lambda e: e.activation(out=s2[:], in_=pG2[:, 0:512], func=AF.Sigmoid), reads=[pG2], writes=[s2])
                          S.op("dve", lambda e: e.tensor_tensor(out=s1[:], in0=s1[:], in1=pPA[:, 0:512], op=ALU.mult), reads=[s1, pPA], writes=[s1])
                          S.op("dve", lambda e: e.tensor_tensor(out=s2[:], in0=s2[:], in1=pPB[:, 0:512], op=ALU.mult), reads=[s2, pPB], writes=[s2])
                          S.op("dve", lambda e, c=c, tg=tg: e.tensor_tensor(out=mT[:, c, tg * 512:(tg + 1) * 512], in0=s1[:], in1=s2[:], op=ALU.add),
                               reads=[s1, s2], writes=[mT])
                  n = 0
                  for cg in range(4):
                      wload(Wo, lambda a, b: Wo[:, a:b, :], wo_v[:, :, cg * 512:(cg + 1) * 512], 16, 4)
                      for tt in range(8):
                          r0 = s * SEQ + t0 + tt * 128
                          bank, xt_, x1_ = pO[n % 2], xt[n % 2], x1t[n % 2]
                          n += 1
                          S.dma("sp", lambda e, xt_=xt_, r0=r0, cg=cg: e.dma_start(out=xt_[:], in_=x_d[r0:r0 + 128, cg * 512:(cg + 1) * 512]), writes=[xt_])
                          for k in range(16):
                              S.op("pe", lambda e, bank=bank, k=k, tt=tt: e.matmul(bank[:, 0:512], lhsT=mT[:, k, tt * 128:(tt + 1) * 128], rhs=Wo[:, k, :],
                                                                                    start=(k == 0), stop=(k == 15)), reads=[mT, Wo], writes=[bank], sig=(k == 15))
                          S.op("dve", lambda e, bank=bank, xt_=xt_, x1_=x1_: e.tensor_tensor(out=x1_[:], in0=bank[:, 0:512], in1=xt_[:], op=ALU.add),
                               reads=[bank, xt_], writes=[x1_])
                          S.dma("sp", lambda e, x1_=x1_, r0=r0, cg=cg: e.dma_start(out=x1_d[r0:r0 + 128, cg * 512:(cg + 1) * 512], in_=x1_[:]),
                                reads=[x1_], writes=[x1_b])
              S.barrier()
        hstack.close()

    with ExitStack() as st:
        stage = [mk(st, "rstage%d" % i, [128, D], F32) for i in range(2)]
        junk = mk(st, "rjunk", [128, D], BF16)
        gb = mk(st, "rgb", [128, D], F32)
        h2 = [mk(st, "h2_%d" % i, [128, D], F32) for i in range(2)]
        h2b = [mk(st, "h2b%d" % i, [128, D], BF16) for i in range(2)]
        h2T = [mk(st, "h2T%d" % i, [128, 16, 128], F32) for i in range(2)]
        Wr = mk(st, "Wr", [128, 16, 36], F32)
        ssr = [mk(st, "rss%d" % i, [128, 1], F32) for i in range(2)]
        t1r = [mk(st, "rt1%d" % i, [128, 1], F32) for i in range(2)]
        rstdr = [mk(st, "rrstd%d" % i, [128, 1], F32) for i in range(2)]
        lgs = [mk(st, "lg%d" % i, [128, 36], F32) for i in range(2)]
        sms = [{n: mk(st, "r%d_%s" % (i, n), [128, w], F32) for n, w in (
            ("cmax", 1), ("ncm", 1), ("ohg", 4), ("ce", 4), ("csum", 1), ("pg", 1), ("fsel", 8), ("v1", 1), ("oh1", 8), ("fm", 8),
            ("v2", 1), ("oh2", 8), ("dl", 1), ("ed", 1), ("den", 1), ("rden", 1), ("A1", 32), ("A2", 32), ("As", 32), ("pos", 32),
            ("ovf", 32), ("sl", 32), ("tmp", 32), ("sf", 2))} for i in range(2)]
        Asbs = [mk(st, "Asb%d" % i, [128, 32], BF16) for i in range(2)]
        ptf = [mkp(st, "ptf%d" % i, [128, 512], F32) for i in range(4)]
        plgs = [mkp(st, "plg%d" % i, [128, 512], F32) for i in range(2)]
        pcns = [mkp(st, "pcn%d" % i, [128, 512], F32) for i in range(2)]
        S.dma("sp", lambda e: e.dma_start(out=gb[:], in_=g_ffn_d.partition_broadcast(128)), writes=[gb])
        S.dma("sp", lambda e: e.dma_start(out=Wr[:], in_=w_rt_d.rearrange("(k p) c -> p k c", p=128)), writes=[Wr])

        def dv(fn, reads, writes):
            S.op("dve", fn, reads=reads, writes=writes)

        def r_front(i):
            stg = stage[i % 2]
            hb = h2b[i % 2]
            h2_ = h2[i % 2]
            h2T_ = h2T[i % 2]
            ss, t1, rstd = ssr[i % 2], t1r[i % 2], rstdr[i % 2]
            plg = plgs[i % 2]
            S.dma("sp", lambda e, stg=stg, i=i: e.dma_start(out=stg[:], in_=x1_d[i * 128:(i + 1) * 128, :]), reads=[x1_b], writes=[stg])
            S.op("act", lambda e, stg=stg: e.activation(out=junk[:], in_=stg[:], func=AF.Square, accum_out=ss[:]), reads=[stg], writes=[junk, ss])
            rstd_ops(ss, t1, rstd, 1.0 / D, 1e-6)
            dv(lambda e, stg=stg: e.scalar_tensor_tensor(out=h2_[:], in0=stg[:], scalar=rstd[:], in1=gb[:], op0=ALU.mult, op1=ALU.mult), [stg, rstd, gb], [h2_])
            S.op("act", lambda e, hb=hb: e.activation(out=hb[:], in_=h2_[:], func=AF.Copy), reads=[h2_], writes=[hb])
            for k in range(16):
                S.op("pe", lambda e, k=k: e.transpose(ptf[k // 4][:, (k % 4) * 128:(k % 4 + 1) * 128], h2_[:, k * 128:(k + 1) * 128], identf[:]),
                     reads=[h2_, identf], writes=[ptf[k // 4]], sig=(k % 4 == 3))
            for j in range(4):
                evac(h2T_[:, 4 * j:4 * j + 4, :], ptf[j][:].rearrange("p (k t) -> p k t", k=4), [ptf[j]], [h2T_])
            for k in range(16):
                S.op("pe", lambda e, k=k: e.matmul(plg[:, 0:36], lhsT=h2T_[:, k, :], rhs=Wr[:, k, :], start=(k == 0), stop=(k == 15)),
                     reads=[h2T_, Wr], writes=[plg], sig=(k == 15))

        def r_back(i):
            hb = h2b[i % 2]
            m = sms[i % 2]
            lg = lgs[i % 2]
            Asb = Asbs[i % 2]
            plg = plgs[i % 2]
            pcn = pcns[i % 2]
            dv(lambda e: e.tensor_copy(out=lg[:], in_=plg[:, 0:36]), [plg], [lg])
            fine = lg[:, 4:36].rearrange("p (g j) -> p g j", g=4)
            dv(lambda e: e.reduce_max(out=m["cmax"][:], in_=lg[:, 0:4], axis=AX.X), [lg], [m["cmax"]])
            dv(lambda e: e.tensor_scalar(out=m["ohg"][:], in0=lg[:, 0:4], scalar1=m["cmax"][:], scalar2=None, op0=ALU.is_equal), [lg, m["cmax"]], [m["ohg"]])
            dv(lambda e: e.tensor_scalar(out=m["ncm"][:], in0=m["cmax"][:], scalar1=-1.0, scalar2=None, op0=ALU.mult), [m["cmax"]], [m["ncm"]])
            S.op("act", lambda e: e.activation(out=m["ce"][:], in_=lg[:, 0:4], func=AF.Exp, bias=m["ncm"][:], accum_out=m["csum"][:]),
                 reads=[lg, m["ncm"]], writes=[m["ce"], m["csum"]])
            dv(lambda e: e.reciprocal(out=m["pg"][:], in_=m["csum"][:]), [m["csum"]], [m["pg"]])
            dv(lambda e: e.tensor_scalar(out=m["fsel"][:], in0=fine[:, 0, :], scalar1=m["ohg"][:, 0:1], scalar2=None, op0=ALU.mult), [lg, m["ohg"]], [m["fsel"]])
            for g in range(1, 4):
                dv(lambda e, g=g: e.scalar_tensor_tensor(out=m["fsel"][:], in0=fine[:, g, :], scalar=m["ohg"][:, g:g + 1], in1=m["fsel"][:], op0=ALU.mult, op1=ALU.add),
                   [lg, m["ohg"], m["fsel"]], [m["fsel"]])
            dv(lambda e: e.reduce_max(out=m["v1"][:], in_=m["fsel"][:], axis=AX.X), [m["fsel"]], [m["v1"]])
            dv(lambda e: e.tensor_scalar(out=m["oh1"][:], in0=m["fsel"][:], scalar1=m["v1"][:], scalar2=None, op0=ALU.is_equal), [m["fsel"], m["v1"]], [m["oh1"]])
            dv(lambda e: e.scalar_tensor_tensor(out=m["fm"][:], in0=m["oh1"][:], scalar=-1e30, in1=m["fsel"][:], op0=ALU.mult, op1=ALU.add),
               [m["oh1"], m["fsel"]], [m["fm"]])
            dv(lambda e: e.reduce_max(out=m["v2"][:], in_=m["fm"][:], axis=AX.X), [m["fm"]], [m["v2"]])
            dv(lambda e: e.tensor_scalar(out=m["oh2"][:], in0=m["fm"][:], scalar1=m["v2"][:], scalar2=None, op0=ALU.is_equal), [m["fm"], m["v2"]], [m["oh2"]])
            dv(lambda e: e.tensor_tensor(out=m["dl"][:], in0=m["v2"][:], in1=m["v1"][:], op=ALU.subtract), [m["v1"], m["v2"]], [m["dl"]])
            S.op("act", lambda e: e.activation(out=m["ed"][:], in_=m["dl"][:], func=AF.Exp), reads=[m["dl"]], writes=[m["ed"]])
            dv(lambda e: e.tensor_scalar(out=m["den"][:], in0=m["ed"][:], scalar1=1.0, scalar2=None, op0=ALU.add), [m["ed"]], [m["den"]])
            dv(lambda e: e.reciprocal(out=m["rden"][:], in_=m["den"][:]), [m["den"]], [m["rden"]])
            dv(lambda e: e.tensor_tensor(out=gates[:, i, 0:1], in0=m["pg"][:], in1=m["rden"][:], op=ALU.mult), [m["pg"], m["rden"]], [gates])
            dv(lambda e: e.tensor_tensor(out=gates[:, i, 1:2], in0=gates[:, i, 0:1], in1=m["ed"][:], op=ALU.mult), [gates, m["ed"]], [gates])
            for g in range(4):
                dv(lambda e, g=g: e.tensor_scalar(out=m["A1"][:, g * 8:(g + 1) * 8], in0=m["oh1"][:], scalar1=m["ohg"][:, g:g + 1], scalar2=None, op0=ALU.mult),
                   [m["oh1"], m["ohg"]], [m["A1"]])
                dv(lambda e, g=g: e.tensor_scalar(out=m["A2"][:, g * 8:(g + 1) * 8], in0=m["oh2"][:], scalar1=m["ohg"][:, g:g + 1], scalar2=None, op0=ALU.mult),
                   [m["oh2"], m["ohg"]], [m["A2"]])
            dv(lambda e: e.tensor_tensor(out=m["As"][:], in0=m["A1"][:], in1=m["A2"][:], op=ALU.add), [m["A1"], m["A2"]], [m["As"]])
            dv(lambda e: e.tensor_copy(out=Asb[:], in_=m["As"][:]), [m["As"]], [Asb])
            S.op("pe", lambda e: e.matmul(pcn[:, 0:32], lhsT=trib[:], rhs=Asb[:], start=True, stop=True), reads=[trib, Asb], writes=[pcn], sig=False)
            S.op("pe", lambda e: e.matmul(pcn[:, 32:64], lhsT=onesb[:], rhs=Asb[:], start=True, stop=True), reads=[onesb, Asb], writes=[pcn])
            dv(lambda e: e.tensor_tensor(out=m["pos"][:], in0=pcn[:, 0:32], in1=cbase[:], op=ALU.add), [pcn, cbase], [m["pos"]])
            dv(lambda e: e.tensor_tensor(out=cbase[:], in0=pcn[:, 32:64], in1=cbase[:], op=ALU.add), [pcn, cbase], [cbase])
            dv(lambda e: e.tensor_scalar(out=m["ovf"][:], in0=m["pos"][:], scalar1=float(CAP), scalar2=None, op0=ALU.is_ge), [m["pos"]], [m["ovf"]])
            dv(lambda e: e.scalar_tensor_tensor(out=m["sl"][:], in0=m["ovf"][:], scalar=1.0e7, in1=m["pos"][:], op0=ALU.mult, op1=ALU.add),
               [m["ovf"], m["pos"]], [m["sl"]])
            dv(lambda e: e.tensor_tensor(out=m["sl"][:], in0=m["sl"][:], in1=ecrow[:], op=ALU.add), [m["sl"], ecrow], [m["sl"]])
            for kx, An in ((0, "A1"), (1, "A2")):
                dv(lambda e, An=An: e.tensor_tensor(out=m["tmp"][:], in0=m[An][:], in1=m["sl"][:], op=ALU.mult), [m[An], m["sl"]], [m["tmp"]])
                dv(lambda e, kx=kx: e.reduce_sum(out=m["sf"][:, kx:kx + 1], in_=m["tmp"][:], axis=AX.X), [m["tmp"]], [m["sf"]])
            dv(lambda e: e.tensor_copy(out=slots[:, 2 * i:2 * i + 2], in_=m["sf"][:]), [m["sf"]], [slots])
            dv(lambda e: e.tensor_scalar(out=m["sf"][:], in0=m["sf"][:], scalar1=float(NSLOT), scalar2=None, op0=ALU.min), [m["sf"]], [m["sf"]])
            dv(lambda e: e.tensor_copy(out=slotg[:, 2 * i:2 * i + 2], in_=m["sf"][:]), [m["sf"]], [slotg])
            for kx in range(2):
                S.dma("pool", lambda e, kx=kx, hb=hb: e.indirect_dma_start(
                    out=xin_d, out_offset=bass.IndirectOffsetOnAxis(ap=slots[:, 2 * i + kx:2 * i + kx + 1], axis=0), in_=hb[:], in_offset=None,
                    bounds_check=bc_reg, oob_is_err=False), reads=[hb, slots], writes=[xin_b])

        ntr = cfg.get('ntR', NT)
        LAG = 3
        for i0 in range(0, ntr, 2):
            tiles = [i0] + ([i0 + 1] if i0 + 1 < ntr else [])
            for i in tiles:
                r_front(i)
            lists = [S.record(lambda i=i: r_back(i)) for i in tiles]
            pos_ = [0] * len(lists)
            step = 0
            while any(pos_[k] < len(lists[k]) for k in range(len(lists))):
                for k in range(len(lists)):
                    if pos_[k] >= len(lists[k]) or (k == 1 and step < LAG and pos_[0] < len(lists[0])):
                        continue
                    S.replay(lists[k][pos_[k]])
                    pos_[k] += 1
                step += 1
        S.barrier()

    with ExitStack() as st:
        xtok = [mk(st, "xtok%d" % i, [128, D], BF16) for i in range(2)]
        XTs = [mk(st, "XT%d" % i, [128, 16, CAP], BF16) for i in range(2)]
        W13 = [mk(st, "W13_%d" % i, [128, 16, 512], BF16) for i in range(4)]
        W2s = [mk(st, "W2s_%d" % i, [128, 8, 512], BF16) for i in range(4)]
        W2f = [mk(st, "W2f_%d" % i, [128, 8, 512], F32) for i in range(2)]
        HT = mk(st, "HT", [128, 8, CAP], BF16)
        sl1 = mk(st, "sl1", [128, CCH], F32)
        yt = [mk(st, "yt%d" % i, [128, 512], F32) for i in range(4)]
        ptb = [mkp(st, "eptb%d" % i, [128, 1024], BF16) for i in range(2)]
        pb1 = [mkp(st, "pb1_%d" % i, [128, 512], F32) for i in range(2)]
        pb3 = [mkp(st, "pb3_%d" % i, [128, 512], F32) for i in range(2)]
        py = [mkp(st, "py%d" % i, [128, 512], F32) for i in range(2)]
        NTI = CAP // 128
        nE = cfg.get('nE', NE)
        S.op("dve", lambda e: e.memset(yt[0][:], 0.0), writes=[yt[0]])
        for cg in range(4):
            S.dma("sp", lambda e, cg=cg: e.dma_start(out=ybuf_d[NSLOT:NSLOT + 128, cg * 512:(cg + 1) * 512], in_=yt[0][:]), reads=[yt[0]], writes=[ybuf_b])
        n13 = 0
        n2 = 0
        ny = 0
        nxc = [0]

        def t_chunks(ex):
            XTe = XTs[ex % 2]
            chunks = []
            for ti in range(NTI):
                holder = {}
                for j in range(2):
                    def chunk(ti=ti, j=j, holder=holder):
                        if j == 0:
                            xk = xtok[nxc[0] % 2]
                            nxc[0] += 1
                            holder["xk"] = xk
                            r0 = ex * CAP + ti * 128
                            S.dma("sp", lambda e: e.dma_start(out=xk[:], in_=xin_d[r0:r0 + 128, :]), reads=[xin_b], writes=[xk])
                        xk = holder["xk"]
                        for k in range(8 * j, 8 * j + 8):
                            S.op("pe", lambda e, k=k: e.transpose(ptb[j][:, (k % 8) * 128:(k % 8 + 1) * 128], xk[:, k * 128:(k + 1) * 128], identb[:]),
                                 reads=[xk, identb], writes=[ptb[j]], sig=(k % 8 == 7))
                        evac(XTe[:, 8 * j:8 * j + 8, ti * 128:(ti + 1) * 128], ptb[j][:].rearrange("p (k t) -> p k t", k=8), [ptb[j]], [XTe])
                    chunks.append(chunk)
            return chunks

        if nE:
            for ch in t_chunks(0):
                ch()
        for ex in range(nE):
            XT = XTs[ex % 2]
            w1v = w1_d[ex].rearrange("(k p) c -> p k c", p=128)
            w3v = w3_d[ex].rearrange("(k p) c -> p k c", p=128)
            w2v = w2_d[ex].rearrange("(k p) c -> p k c", p=128)
            w2slot = [W2s[(n2 + cg) % 4] for cg in range(4)]

            def w2_load(cg):
                stg_ = W2f[cg % 2]
                for hh_ in range(2):
                    S.dma("sp", lambda e, hh_=hh_: e.dma_start(out=stg_[:, hh_ * 4:(hh_ + 1) * 4, :], in_=w2v[:, hh_ * 4:(hh_ + 1) * 4, cg * 512:(cg + 1) * 512]),
                          writes=[stg_])

            def w2_cast(cg):
                stg_ = W2f[cg % 2]
                dst_ = w2slot[cg]
                if cg % 2 == 0:
                    S.op("act", lambda e: e.activation(out=dst_[:], in_=stg_[:], func=AF.Copy), reads=[stg_], writes=[dst_])
                else:
                    S.op("dve", lambda e: e.tensor_copy(out=dst_[:], in_=stg_[:]), reads=[stg_], writes=[dst_])

            w2_load(0)
            w2_load(1)
            for hf in range(2):
                wa_ = W13[n13 % 4]
                wb_ = W13[(n13 + 1) % 4]
                n13 += 2
                wload(wa_, lambda a, b, w=wa_: w[:, a:b, :], w1v[:, :, hf * 512:(hf + 1) * 512], 16, 4)
                wload(wb_, lambda a, b, w=wb_: w[:, a:b, :], w3v[:, :, hf * 512:(hf + 1) * 512], 16, 4)
                for hc in range(4):
                    for ci in range(CAP // CCH):
                        b1, b3 = pb1[ci % 2], pb3[ci % 2]
                        for bank, Wt in ((b1, wa_), (b3, wb_)):
                            for k in range(16):
                                S.op("pe", lambda e, bank=bank, Wt=Wt, k=k, hc=hc, ci=ci: e.matmul(
                                    bank[:, 0:CCH], lhsT=Wt[:, k, hc * 128:(hc + 1) * 128], rhs=XT[:, k, ci * CCH:(ci + 1) * CCH],
                                    start=(k == 0), stop=(k == 15)), reads=[Wt, XT], writes=[bank], sig=(k == 15))
                        S.op("act", lambda e, b1=b1: e.activation(out=sl1[:], in_=b1[:, 0:CCH], func=AF.Silu), reads=[b1], writes=[sl1])
                        S.op("dve", lambda e, hf=hf, hc=hc, ci=ci, b3=b3: e.tensor_tensor(out=HT[:, hf * 4 + hc, ci * CCH:(ci + 1) * CCH], in0=sl1[:], in1=b3[:, 0:CCH], op=ALU.mult),
                             reads=[sl1, b3], writes=[HT])
                    if hc in (1, 3):
                        cgc = hf * 2 + (hc // 2)
                        w2_cast(cgc)
                        if cgc + 2 < 4:
                            w2_load(cgc + 2)
            nxt = t_chunks(ex + 1) if ex + 1 < nE else []
            it_ = 0
            for cg in range(4):
                w2_ = w2slot[cg]
                n2 += 1
                for ti in range(NTI):
                    bank, y_ = py[ny % 2], yt[ny % 4]
                    ny += 1
                    for k in range(8):
                        S.op("pe", lambda e, bank=bank, w2_=w2_, k=k, ti=ti: e.matmul(bank[:, 0:512], lhsT=HT[:, k, ti * 128:(ti + 1) * 128], rhs=w2_[:, k, :],
                                                                                      start=(k == 0), stop=(k == 7)), reads=[HT, w2_], writes=[bank], sig=(k == 7))
                    evac(y_[:], bank[:, 0:512], [bank], [y_])
                    r0 = ex * CAP + ti * 128
                    S.dma("sp", lambda e, y_=y_, r0=r0, cg=cg: e.dma_start(out=ybuf_d[r0:r0 + 128, cg * 512:(cg + 1) * 512], in_=y_[:]),
                          reads=[y_], writes=[ybuf_b])
                    it_ += 1
                    if it_ % 2 == 0 and nxt:
                        nxt.pop(0)()
            while nxt:
                nxt.pop(0)()
        S.barrier()

    with ExitStack() as st:
        Wpg = mk(st, "Wpg", [128, 16, D], BF16)
        Wpp = mk(st, "Wpp", [128, 2, D], BF16)
        gbp = mk(st, "gbp", [128, D], F32)
        gbf = mk(st, "gbf", [128, D], F32)
        junk = mk(st, "fjunk", [128, D], BF16)
        stage = [mk(st, "fstage%d" % i, [128, D], F32) for i in range(2)]
        Y1s = [mk(st, "Y1_%d" % i, [128, D], F32) for i in range(2)]
        Y2s = [mk(st, "Y2_%d" % i, [128, D], F32) for i in range(2)]
        x3s = [mk(st, "x3_%d" % i, [128, D], F32) for i in range(2)]
        h3s = [mk(st, "h3_%d" % i, [128, D], BF16) for i in range(2)]
        h3Ts = [mk(st, "h3T%d" % i, [128, 16, 128], BF16) for i in range(2)]
        ptls = [mk(st, "ptl%d" % i, [128, 256], F32) for i in range(2)]
        pbls = [mk(st, "pbl%d" % i, [128, 256], BF16) for i in range(2)]
        pTls = [mk(st, "pTl%d" % i, [128, 2, 128], BF16) for i in range(2)]
        sgs = [mk(st, "sg%d" % i, [128, 512], F32) for i in range(2)]
        ssa = [mk(st, "fssa%d" % i, [128, 1], F32) for i in range(2)]
        t1a = [mk(st, "ft1a%d" % i, [128, 1], F32) for i in range(2)]
        rsa = [mk(st, "frsa%d" % i, [128, 1], F32) for i in range(2)]
        ssb = [mk(st, "fssb%d" % i, [128, 1], F32) for i in range(2)]
        t1b = [mk(st, "ft1b%d" % i, [128, 1], F32) for i in range(2)]
        rsb = [mk(st, "frsb%d" % i, [128, 1], F32) for i in range(2)]
        ptb = [mkp(st, "fptb%d" % i, [128, 1024], BF16) for i in range(2)]
        ptp = mkp(st, "fptp", [128, 1024], BF16)
        pG = [mkp(st, "fpG%d" % i, [128, 512], F32) for i in range(2)]
        pP = [mkp(st, "fpP%d" % i, [128, 512], F32) for i in range(2)]
        wload(Wpg, lambda a, b: Wpg[:, a:b, :], w_pg_d.rearrange("(k p) c -> p k c", p=128), 16, 16)
        wload(Wpp, lambda a, b: Wpp[:, a:b, :], w_pp_d.rearrange("(k p) c -> p k c", p=128), 2, 2)
        S.dma("sp", lambda e: e.dma_start(out=gbp[:], in_=g_ple_d.partition_broadcast(128)), writes=[gbp])
        S.dma("sp", lambda e: e.dma_start(out=gbf[:], in_=g_fin_d.partition_broadcast(128)), writes=[gbf])
        fn_ = [0]
        ntf = cfg.get('ntF', NT)

        def f_front(i):
            stg, Y1, Y2, h3, ptl, pbl = stage[i % 2], Y1s[i % 2], Y2s[i % 2], h3s[i % 2], ptls[i % 2], pbls[i % 2]
            ss, t1, rstd = ssa[i % 2], t1a[i % 2], rsa[i % 2]
            S.dma("sp", lambda e: e.dma_start(out=stg[:], in_=x1_d[i * 128:(i + 1) * 128, :]), reads=[x1_b], writes=[stg])
            S.dma("sp", lambda e: e.dma_start(out=ptl[:], in_=p_d[i * 128:(i + 1) * 128, :]), writes=[ptl])
            for kx, Y in ((0, Y1), (1, Y2)):
                S.dma("pool", lambda e, kx=kx, Y=Y: e.indirect_dma_start(
                    out=Y[:], out_offset=None, in_=ybuf_d, in_offset=bass.IndirectOffsetOnAxis(ap=slotg[:, 2 * i + kx:2 * i + kx + 1], axis=0),
                    bounds_check=bc_reg2, oob_is_err=False), reads=[ybuf_b, slotg], writes=[Y])

        def f_prep(i):
            stg, Y1, Y2, h3, ptl, pbl = stage[i % 2], Y1s[i % 2], Y2s[i % 2], h3s[i % 2], ptls[i % 2], pbls[i % 2]
            ss, t1, rstd = ssa[i % 2], t1a[i % 2], rsa[i % 2]
            S.op("dve", lambda e: e.scalar_tensor_tensor(out=Y1[:], in0=Y1[:], scalar=gates[:, i, 0:1], in1=stg[:], op0=ALU.mult, op1=ALU.add),
                 reads=[Y1, gates, stg], writes=[Y1])
            S.op("dve", lambda e: e.scalar_tensor_tensor(out=Y1[:], in0=Y2[:], scalar=gates[:, i, 1:2], in1=Y1[:], op0=ALU.mult, op1=ALU.add),
                 reads=[Y2, gates, Y1], writes=[Y1])
            S.op("act", lambda e: e.activation(out=junk[:], in_=Y1[:], func=AF.Square, accum_out=ss[:]), reads=[Y1], writes=[junk, ss])
            rstd_ops(ss, t1, rstd, 1.0 / D, 1e-6)
            S.op("dve", lambda e: e.scalar_tensor_tensor(out=h3[:], in0=Y1[:], scalar=rstd[:], in1=gbp[:], op0=ALU.mult, op1=ALU.mult),
                 reads=[Y1, rstd, gbp], writes=[h3])
            S.op("act", lambda e: e.activation(out=pbl[:], in_=ptl[:], func=AF.Copy), reads=[ptl], writes=[pbl])

        def f_back(i):
            x2, x3, h3, h3T, pbl, pTl, sg = Y1s[i % 2], x3s[i % 2], h3s[i % 2], h3Ts[i % 2], pbls[i % 2], pTls[i % 2], sgs[i % 2]
            ss, t1, rstd = ssb[i % 2], t1b[i % 2], rsb[i % 2]
            for k in range(16):
                S.op("pe", lambda e, k=k: e.transpose(ptb[k // 8][:, (k % 8) * 128:(k % 8 + 1) * 128], h3[:, k * 128:(k + 1) * 128], identb[:]),
                     reads=[h3, identb], writes=[ptb[k // 8]], sig=(k % 8 == 7))
            for j in range(2):
                evac(h3T[:, 8 * j:8 * j + 8, :], ptb[j][:].rearrange("p (k t) -> p k t", k=8), [ptb[j]], [h3T])
            for k in range(2):
                S.op("pe", lambda e, k=k: e.transpose(ptp[:, k * 128:(k + 1) * 128], pbl[:, k * 128:(k + 1) * 128], identb[:]),
                     reads=[pbl, identb], writes=[ptp], sig=(k == 1))
            evac(pTl[:], ptp[:, 0:256].rearrange("p (k t) -> p k t", k=2), [ptp], [pTl])
            for cg in range(4):
                bG, bP = pG[fn_[0] % 2], pP[fn_[0] % 2]
                fn_[0] += 1
                for k in range(16):
                    S.op("pe", lambda e, bG=bG, k=k, cg=cg: e.matmul(bG[:, 0:512], lhsT=h3T[:, k, :], rhs=Wpg[:, k, cg * 512:(cg + 1) * 512],
                                                                     start=(k == 0), stop=(k == 15)), reads=[h3T, Wpg], writes=[bG], sig=(k == 15))
                for k in range(2):
                    S.op("pe", lambda e, bP=bP, k=k, cg=cg: e.matmul(bP[:, 0:512], lhsT=pTl[:, k, :], rhs=Wpp[:, k, cg * 512:(cg + 1) * 512],
                                                                     start=(k == 0), stop=(k == 1)), reads=[pTl, Wpp], writes=[bP], sig=(k == 1))
                S.op("act", lambda e, bG=bG: e.activation(out=sg[:], in_=bG[:, 0:512], func=AF.Sigmoid), reads=[bG], writes=[sg])
                S.op("dve", lambda e, bP=bP: e.tensor_tensor(out=sg[:], in0=sg[:], in1=bP[:, 0:512], op=ALU.mult), reads=[sg, bP], writes=[sg])
                S.op("dve", lambda e, cg=cg: e.tensor_tensor(out=x3[:, cg * 512:(cg + 1) * 512], in0=sg[:], in1=x2[:, cg * 512:(cg + 1) * 512], op=ALU.add),
                     reads=[sg, x2], writes=[x3])
                if cg == 1 and i + 1 < ntf:
                    f_prep(i + 1)
            S.op("act", lambda e: e.activation(out=junk[:], in_=x3[:], func=AF.Square, accum_out=ss[:]), reads=[x3], writes=[junk, ss])
            rstd_ops(ss, t1, rstd, 1.0 / D, 1e-6)
            S.op("dve", lambda e: e.scalar_tensor_tensor(out=x3[:], in0=x3[:], scalar=rstd[:], in1=gbf[:], op0=ALU.mult, op1=ALU.mult),
                 reads=[x3, rstd, gbf], writes=[x3])
            S.dma("sp", lambda e: e.dma_start(out=out_d[i * 128:(i + 1) * 128, :], in_=x3[:]), reads=[x3], writes=[out_b])

        if ntf:
            f_front(0)
            f_prep(0)
        for i in range(ntf):
            if i + 1 < ntf:
                f_front(i + 1)
            f_back(i)
        S.barrier()
    S.finish()
    gstack.close()
    return nc


def _t5_bucket(dist):
    dist = np.maximum(dist, 0)
    d_f = np.maximum(dist, 1).astype(np.float32)
    large = 16 + (np.log(d_f / np.float32(16)) / np.float32(math.log(2048 / 16)) * np.float32(16)).astype(np.int32)
    large = np.minimum(large, 31)
    return np.where(dist < 16, dist, large)


def _bias_layouts(rel_bias):
    rb = np.asarray(rel_bias, np.float32)
    NEG = np.float32(-1e30)
    k = np.arange(128)[:, None]
    col = np.arange(2048)[None, :]
    dist = col - k
    bk = _t5_bucket(dist)
    ba = np.empty((8, 128, 2048), np.float32)
    for h in range(8):
        ba[h] = np.where(dist >= 0, rb[bk, h], NEG)
    bb = np.empty((6, 128, 4, 2, 128), np.float32)
    q = np.arange(128)[None, :]
    for quad in range(2):
        for g in range(3):
            d = DIL[g][1]
            for hl in range(4):
                col_h = 8 + g * 8 + quad * 4 + hl
                rel_c = q - k
                rel_p = q - k + 128
                bb[quad * 3 + g, :, hl, 0, :] = np.where(rel_c >= 0, rb[_t5_bucket(rel_c * d), col_h], NEG)
                bb[quad * 3 + g, :, hl, 1, :] = np.where(rel_p <= 128, rb[_t5_bucket(rel_p * d), col_h], NEG)
    return ba, bb.reshape(6, 128, 1024)


_NC = None


def prep_inputs(x, p, rel_bias, norm_mix_g, w_in, w_gate, lambda_q1, lambda_k1, lambda_q2, lambda_k2, subln_g,
                w_proj_a, w_proj_b, w_out, norm_ffn_g, w_coarse, w_fine, w1, w3, w2, norm_ple_g, w_ple_gate,
                w_ple_proj, final_norm_g):
    f = lambda a: np.ascontiguousarray(np.asarray(a, dtype=np.float32))
    x = f(x).reshape(NCORES, T, D)
    p = f(p)[0].reshape(NCORES, T, 256)
    ba, bb = _bias_layouts(rel_bias)
    w_router = np.ascontiguousarray(np.concatenate(
        [f(w_coarse)[0], np.transpose(f(w_fine)[0], (1, 0, 2)).reshape(D, 32)], axis=1))
    tri = (np.arange(128)[:, None] < np.arange(128)[None, :]).astype(np.float32)
    ecrow = np.ascontiguousarray(np.broadcast_to((np.arange(NE, dtype=np.float32) * CAP)[None, :], (128, NE)))
    lam = np.ascontiguousarray(np.stack([f(lambda_q1)[0], f(lambda_k1)[0], f(lambda_q2)[0], f(lambda_k2)[0]], 0))
    shared = {
        "bias_a": ba, "bias_b": bb,
        "norm_mix_g": f(norm_mix_g)[0], "norm_ffn_g": f(norm_ffn_g)[0], "norm_ple_g": f(norm_ple_g)[0],
        "final_norm_g": f(final_norm_g),
        "w_in": f(w_in)[0], "w_gate": f(w_gate)[0], "lam": lam, "subln_g": f(subln_g)[0],
        "w_proj_a": f(w_proj_a)[0], "w_proj_b": f(w_proj_b)[0], "w_out": f(w_out)[0], "w_router": w_router,
        "w1": f(w1)[0], "w3": f(w3)[0], "w2": f(w2)[0], "w_ple_gate": f(w_ple_gate)[0], "w_ple_proj": f(w_ple_proj)[0],
        "identf": np.eye(128, dtype=np.float32), "tri": tri, "ecrow": ecrow,
    }
    in_maps = []
    for c in range(NCORES):
        m = dict(shared)
        m["x"] = np.ascontiguousarray(x[c])
        m["p"] = np.ascontiguousarray(p[c])
        in_maps.append(m)
    return in_maps


def kernel(**inputs):
    global _NC
    in_maps = prep_inputs(**inputs)
    if _NC is None:
        _NC = build_nc()
    res = run_bass_kernel_spmd(_NC, in_maps, core_ids=list(range(NCORES)))
    out = np.stack([np.asarray(r["out"], dtype=np.float32) for r in res.results], 0)
    return out.reshape(16, SEQ, D)
```

```python
import math
from contextlib import ExitStack

import numpy as np
import concourse.bass as bass
import concourse.mybir as mybir
from concourse.bass_utils import run_bass_kernel_spmd

F32 = mybir.dt.float32
BF16 = mybir.dt.bfloat16
I32 = mybir.dt.int32
AF = mybir.ActivationFunctionType
ALU = mybir.AluOpType
AX = mybir.AxisListType

NCORES = 8
D = 2048
SEQ = 2048
T = 2 * SEQ
NT = T // 128
CAP = 512
CCH = 256
NE = 32
NSLOT = NE * CAP
LAM_INIT = 0.2
SC_A = 128 ** -0.5
SC_B = 64 ** -0.5
DIL = ((128, 1), (512, 4), (2048, 16))


class Buf:
    __slots__ = ("name", "writers", "readers")

    def __init__(self, name=""):
        self.name = name
        self.writers = {}
        self.readers = {}


class Rec:
    __slots__ = ("eng", "cnt", "is_dma", "sem")

    def __init__(self, eng, is_dma, sem):
        self.eng = eng
        self.is_dma = is_dma
        self.sem = sem
        self.cnt = None


class TB:
    def __init__(self, h, name=""):
        self.h = h
        self.b = Buf(name)

    def __getitem__(self, k):
        return self.h[k]


class Sch:
    def __init__(self, nc):
        self.nc = nc
        self.E = {"pe": nc.tensor, "act": nc.scalar, "dve": nc.vector, "pool": nc.gpsimd, "sp": nc.sync}
        self.sem = {e: nc.alloc_semaphore("s_" + e) for e in ("pe", "act", "dve", "pool")}
        self.cnt = {e: 0 for e in self.sem}
        self.waited = {e: {} for e in self.E}
        self.dsem = {}
        self.pending = []
        self.nsem = 4
        self.n_ops = 0
        self.rec = None

    def _deps(self, eng, reads, writes):
        need = {}

        def add(r, raw):
            if not r.is_dma:
                if r.eng == eng and (eng == "pe" or not raw):
                    return
            assert r.cnt is not None, "dependency on unsignaled op (%s)" % r.eng
            k = id(r.sem)
            if need.get(k, (None, 0))[1] < r.cnt:
                need[k] = (r.sem, r.cnt)

        for b in reads:
            for r in b.writers.values():
                add(r, True)
        for b in writes:
            if b.readers:
                for r in b.readers.values():
                    add(r, False)
                for r in b.writers.values():
                    add(r, False)
        return need

    def _emit_waits(self, eng, need):
        E = self.E[eng]
        w = self.waited[eng]
        for k, (sem, val) in need.items():
            if w.get(k, 0) < val:
                E.wait_ge(sem, val)
                w[k] = val

    def _update(self, rec, key, reads, writes):
        for b in writes:
            if b.readers:
                b.writers = {key: rec}
                b.readers = {}
            else:
                b.writers[key] = rec
        for b in reads:
            b.readers[key] = rec

    def record(self, f):
        self.rec = []
        f()
        r, self.rec = self.rec, None
        return r

    def replay(self, item):
        kind, args = item
        (self.op if kind == "op" else self.dma)(*args)

    def op(self, eng, fn, reads=(), writes=(), sig=True):
        if self.rec is not None:
            self.rec.append(("op", (eng, fn, list(reads), list(writes), sig)))
            return None
        reads = [x.b if isinstance(x, TB) else x for x in reads]
        writes = [x.b if isinstance(x, TB) else x for x in writes]
        need = self._deps(eng, reads, writes)
        self._emit_waits(eng, need)
        ins = fn(self.E[eng])
        rec = Rec(eng, False, self.sem[eng])
        if sig:
            self.cnt[eng] += 1
            ins.then_inc(self.sem[eng], 1)
            rec.cnt = self.cnt[eng]
            if eng == "pe" and self.pending:
                for p in self.pending:
                    p.cnt = rec.cnt
                self.pending = []
        else:
            assert eng == "pe"
            self.pending.append(rec)
        self._update(rec, eng, reads, writes)
        self.n_ops += 1
        return rec

    def dma(self, q, fn, reads=(), writes=()):
        if self.rec is not None:
            self.rec.append(("dma", (q, fn, list(reads), list(writes))))
            return None
        reads = [x.b if isinstance(x, TB) else x for x in reads]
        writes = [x.b if isinstance(x, TB) else x for x in writes]
        need = self._deps(q, reads, writes)
        self._emit_waits(q, need)
        b0 = writes[0]
        if id(b0) not in self.dsem:
            self.dsem[id(b0)] = [self.nc.alloc_semaphore("d_%d" % self.nsem), 0]
            self.nsem += 1
            assert self.nsem <= 100, "too many DMA semaphores"
        ds = self.dsem[id(b0)]
        ins = fn(self.E[q])
        ds[1] += 16
        ins.then_inc(ds[0], 16)
        rec = Rec(q, True, ds[0])
        rec.cnt = ds[1]
        self._update(rec, ("dma", id(ds[0])), reads, writes)
        self.n_ops += 1
        return rec

    def _all_need(self, skip=None):
        need = {}
        for e in self.sem:
            if e != skip and self.cnt[e] > 0:
                need[id(self.sem[e])] = (self.sem[e], self.cnt[e])
        for s, c in self.dsem.values():
            if c > 0:
                need[id(s)] = (s, c)
        return need

    def barrier(self):
        assert not self.pending
        for f in self.E:
            self._emit_waits(f, self._all_need(skip=f))

    def finish(self):
        assert not self.pending
        self._emit_waits("sp", self._all_need())


def build_nc(cfg=None):
    cfg = cfg or {}
    dbg = cfg.get('dbg', False)
    nc = bass.Bass("TRN2", target_bir_lowering=False)
    S = Sch(nc)

    def din(name, shape, dt=F32):
        return nc.dram_tensor(name, list(shape), dt, kind="ExternalInput").ap()

    def dscr(name, shape, dt):
        return nc.dram_tensor(name, list(shape), dt, kind=("ExternalOutput" if dbg else "Internal")).ap()

    x_d = din("x", [T, D])
    p_d = din("p", [T, 256])
    bias_a_d = din("bias_a", [8, 128, 2048])
    bias_b_d = din("bias_b", [6, 128, 1024])
    g_mix_d = din("norm_mix_g", [D])
    g_ffn_d = din("norm_ffn_g", [D])
    g_ple_d = din("norm_ple_g", [D])
    g_fin_d = din("final_norm_g", [D])
    w_in_d = din("w_in", [D, 10752])
    w_gate_d = din("w_gate", [D, 4096])
    lam_d = din("lam", [4, 128])
    subln_d = din("subln_g", [256])
    w_pa_d = din("w_proj_a", [D, D])
    w_pb_d = din("w_proj_b", [512, D])
    w_out_d = din("w_out", [D, D])
    w_rt_d = din("w_router", [D, 36])
    NEd = cfg.get('nEdecl', NE)
    w1_d = din("w1", [NEd, D, 1024])
    w3_d = din("w3", [NEd, D, 1024])
    w2_d = din("w2", [NEd, 1024, D])
    w_pg_d = din("w_ple_gate", [D, D])
    w_pp_d = din("w_ple_proj", [256, D])
    identf_d = din("identf", [128, 128])
    tri_d = din("tri", [128, 128])
    ecrow_d = din("ecrow", [128, NE])
    out_d = nc.dram_tensor("out", [T, D], F32, kind="ExternalOutput").ap()
    out_b = Buf("out")

    oaT_d = dscr("oaT", [2, 128, 16, SEQ], BF16)
    obT_d = dscr("obT", [2, 128, 4, SEQ], BF16)
    x1_d = dscr("x1", [T, D], F32)
    xin_d = dscr("xin", [NSLOT, D], BF16)
    ybuf_d = dscr("ybuf", [NSLOT + 128, D], F32)
    oaT_b, obT_b, x1_b, xin_b, ybuf_b = Buf("oaT"), Buf("obT"), Buf("x1"), Buf("xin"), Buf("ybuf")

    gstack = ExitStack()
    bc_reg = nc.gpsimd.to_reg(NSLOT - 1)
    bc_reg2 = nc.gpsimd.to_reg(NSLOT + 127)

    uid = [0]

    def mk(stack, name, shape, dt):
        uid[0] += 1
        return TB(stack.enter_context(nc.sbuf_tensor("sb%d_%s" % (uid[0], name), list(shape), dt)), name)

    def mkp(stack, name, shape, dt):
        uid[0] += 1
        return TB(stack.enter_context(nc.psum_tensor("ps%d_%s" % (uid[0], name), list(shape), dt)), name)

    identf = mk(gstack, "identf", [128, 128], F32)
    identb = mk(gstack, "identb", [128, 128], BF16)
    trib = mk(gstack, "trib", [128, 128], BF16)
    onesb = mk(gstack, "onesb", [128, 128], BF16)
    ecrow = mk(gstack, "ecrow", [128, NE], F32)
    neglam = mk(gstack, "neglam", [128, 1], F32)
    gsub = mk(gstack, "gsub", [128, 256], F32)
    slots = mk(gstack, "slots", [128, NT * 2], I32)
    slotg = mk(gstack, "slotg", [128, NT * 2], I32)
    gates = mk(gstack, "gates", [128, NT, 2], F32)
    cbase = mk(gstack, "cbase", [128, NE], F32)

    S.dma("sp", lambda e: e.dma_start(out=identf[:], in_=identf_d), writes=[identf])
    S.dma("sp", lambda e: e.dma_start(out=ecrow[:], in_=ecrow_d), writes=[ecrow])
    S.op("dve", lambda e: e.tensor_copy(out=identb[:], in_=identf[:]), reads=[identf], writes=[identb])
    S.op("dve", lambda e: e.memset(onesb[:], 1.0), writes=[onesb])
    S.op("dve", lambda e: e.memset(cbase[:], 0.0), writes=[cbase])
    with ExitStack() as st:
        trif = mk(st, "trif", [128, 128], F32)
        lamv = mk(st, "lamv", [128, 4, 128], F32)
        lamp = mk(st, "lamp", [128, 2, 128], F32)
        lams = mk(st, "lams", [128, 2], F32)
        lame = mk(st, "lame", [128, 2], F32)
        S.dma("sp", lambda e: e.dma_start(out=trif[:], in_=tri_d), writes=[trif])
        S.op("dve", lambda e: e.tensor_copy(out=trib[:], in_=trif[:]), reads=[trif], writes=[trib])
        for i in range(4):
            S.dma("sp", lambda e, i=i: e.dma_start(out=lamv[:, i, :], in_=lam_d[i].partition_broadcast(128)), writes=[lamv])
        S.dma("sp", lambda e: e.dma_start(out=gsub[:], in_=subln_d.partition_broadcast(128)), writes=[gsub])
        S.op("dve", lambda e: e.tensor_tensor(out=lamp[:, 0, :], in0=lamv[:, 0, :], in1=lamv[:, 1, :], op=ALU.mult), reads=[lamv], writes=[lamp])
        S.op("dve", lambda e: e.tensor_tensor(out=lamp[:, 1, :], in0=lamv[:, 2, :], in1=lamv[:, 3, :], op=ALU.mult), reads=[lamv], writes=[lamp])
        S.op("dve", lambda e: e.reduce_sum(out=lams[:, 0:1], in_=lamp[:, 0, :], axis=AX.X), reads=[lamp], writes=[lams])
        S.op("dve", lambda e: e.reduce_sum(out=lams[:, 1:2], in_=lamp[:, 1, :], axis=AX.X), reads=[lamp], writes=[lams])
        S.op("act", lambda e: e.activation(out=lame[:], in_=lams[:], func=AF.Exp), reads=[lams], writes=[lame])
        S.op("dve", lambda e: e.tensor_tensor(out=neglam[:], in0=lame[:, 1:2], in1=lame[:, 0:1], op=ALU.subtract), reads=[lame], writes=[neglam])
        S.op("dve", lambda e: e.tensor_scalar(out=neglam[:], in0=neglam[:], scalar1=-LAM_INIT, scalar2=None, op0=ALU.add), reads=[neglam], writes=[neglam])
        S.op("dve", lambda e: e.tensor_scalar(out=gsub[:], in0=gsub[:], scalar1=1.0 - LAM_INIT, scalar2=None, op0=ALU.mult), reads=[gsub], writes=[gsub])
        S.barrier()

    def wload(dst, dst_ap_fn, src_ap, kchunks, nsplit, reads=()):
        step = kchunks // nsplit
        for i in range(nsplit):
            S.dma("pool", lambda e, i=i: e.dma_start(out=dst_ap_fn(i * step, (i + 1) * step), in_=src_ap[:, i * step:(i + 1) * step, :]),
                  reads=list(reads), writes=[dst])

    def rstd_ops(ss, t1, rstd, inv_n, eps):
        S.op("dve", lambda e: e.tensor_scalar(out=t1[:], in0=ss[:], scalar1=inv_n, scalar2=eps, op0=ALU.mult, op1=ALU.add), reads=[ss], writes=[t1])
        S.op("act", lambda e: e.activation(out=t1[:], in_=t1[:], func=AF.Ln), reads=[t1], writes=[t1])
        S.op("act", lambda e: e.activation(out=rstd[:], in_=t1[:], func=AF.Exp, scale=-0.5), reads=[t1], writes=[rstd])

    evac_flip = [0]

    def evac(out_ap, in_ap, reads, writes):
        evac_flip[0] ^= 1
        if evac_flip[0]:
            S.op("act", lambda e: e.activation(out=out_ap, in_=in_ap, func=AF.Copy), reads=reads, writes=writes)
        else:
            S.op("dve", lambda e: e.tensor_copy(out=out_ap, in_=in_ap), reads=reads, writes=writes)

    w_in_v = w_in_d.rearrange("(k p) c -> p k c", p=128)

    for s in range(cfg.get('nseq', 2)):
        hstack = ExitStack()
        hT = mk(hstack, "hT", [128, 16, SEQ], BF16)
        with ExitStack() as st:
            stage = [mk(st, "stage%d" % i, [128, D], F32) for i in range(2)]
            junk = mk(st, "junk", [128, D], BF16)
            xs = mk(st, "xs", [128, D], BF16)
            gb = mk(st, "gb", [128, D], F32)
            ss = mk(st, "ss", [128, 1], F32)
            t1 = mk(st, "t1", [128, 1], F32)
            rstd = mk(st, "rstd", [128, 1], F32)
            Wq = mk(st, "Wq", [128, 16, 256], BF16)
            Wk = mk(st, "Wk", [128, 16, 256], BF16)
            Wv = mk(st, "Wv", [128, 16, 256], BF16)
            QT = mk(st, "QT", [128, 2, SEQ], BF16)
            KT = mk(st, "KT", [128, 2, SEQ], BF16)
            V = mk(st, "V", [128, 16, 264], BF16)
            EA = mk(st, "EA", [128, 2048], BF16)
            EB = EA
            PT = [mk(st, "PT%d" % i, [128, 2, 256], BF16) for i in range(3)]
            PTB = [mk(st, "PTB%d" % i, [128, 512], BF16) for i in range(3)]
            o1 = [mk(st, "o1_%d" % i, [128, 256], F32) for i in range(2)]
            oo = [mk(st, "oo_%d" % i, [128, 256], F32) for i in range(2)]
            junk2 = mk(st, "junk2", [128, 256], BF16)
            oab = [mk(st, "oab%d" % i, [128, 256], BF16) for i in range(2)]
            oaTu = mk(st, "oaTu", [128, 2, 512], BF16)
            rr = [mk(st, "rr%d" % i, [128, 2], F32) for i in range(2)]
            nl = [mk(st, "nl%d" % i, [128, 1], F32) for i in range(2)]
            ss2 = mk(st, "ss2", [128, 1], F32)
            t2 = mk(st, "t2", [128, 1], F32)
            rs2 = mk(st, "rs2", [128, 1], F32)
            accUD = mk(st, "accUD", [128, 2, 2, SEQ], F32)
            obTq = mk(st, "obTq", [128, 2, 512], BF16)
            acc = [mkp(st, "acc%d" % i, [128, 512], F32) for i in range(4)]
            pj = [mkp(st, "pj%d" % i, [128, 512], F32) for i in range(3)]
            ptb = [mkp(st, "ptb%d" % i, [128, 1024], BF16) for i in range(1)]

            S.dma("sp", lambda e: e.dma_start(out=gb[:], in_=g_mix_d.partition_broadcast(128)), writes=[gb])
            S.op("dve", lambda e: e.memset(V[:, :, 256:264], 1.0), writes=[V])

            for tt in range(16):
                r0 = s * SEQ + tt * 128
                stg = stage[tt % 2]
                S.dma("sp", lambda e, stg=stg, r0=r0: e.dma_start(out=stg[:], in_=x_d[r0:r0 + 128, :]), writes=[stg])
                S.op("act", lambda e, stg=stg: e.activation(out=junk[:], in_=stg[:], func=AF.Square, accum_out=ss[:]), reads=[stg], writes=[junk, ss])
                rstd_ops(ss, t1, rstd, 1.0 / D, 1e-6)
                S.op("dve", lambda e, stg=stg: e.scalar_tensor_tensor(out=xs[:], in0=stg[:], scalar=rstd[:], in1=gb[:], op0=ALU.mult, op1=ALU.mult),
                     reads=[stg, rstd, gb], writes=[xs])
                for j in range(2):
                    for k in range(8 * j, 8 * j + 8):
                        S.op("pe", lambda e, k=k: e.transpose(ptb[0][:, (k % 8) * 128:(k % 8 + 1) * 128], xs[:, k * 128:(k + 1) * 128], identb[:]),
                             reads=[xs, identb], writes=[ptb[0]], sig=(k % 8 == 7))
                    evac(hT[:, 8 * j:8 * j + 8, tt * 128:(tt + 1) * 128], ptb[0][:].rearrange("p (k t) -> p k t", k=8), [ptb[0]], [hT])

            units = [("A", h) for h in range(8)] + [("B", q * 3 + g) for q in range(2) for g in range(3)]
            if 'units' in cfg:
                units = cfg['units']
            pjn = [0]

            def next_pj():
                pjn[0] = (pjn[0] + 1) % 3
                return pj[pjn[0]]

            for kind, ui in units:
                if kind == "A":
                    h = ui
                    qc, kc, vc = h * 256, 2048 + h * 256, 4096 + h * 256
                    d, nblk = 1, 16
                else:
                    quad, g = divmod(ui, 3)
                    d = DIL[g][1]
                    nblk = (SEQ // d) // 128
                    qc = 6144 + g * 512 + quad * 256
                    kc = 7680 + g * 512 + quad * 256
                    vc = 9216 + g * 512 + quad * 256
                L = SEQ // d
                for Wt, c0 in ((Wq, qc), (Wk, kc), (Wv, vc)):
                    wload(Wt, lambda a, b, Wt=Wt: Wt[:, a:b, :], w_in_v[:, :, c0:c0 + 256], 16, 4)
                if kind == "A":
                    stg = stage[0]
                    S.dma("sp", lambda e, stg=stg, h=h: e.dma_start(out=stg[:], in_=bias_a_d[h]), writes=[stg])
                    S.op("act", lambda e, stg=stg: e.activation(out=EA[:], in_=stg[:], func=AF.Copy, scale=1.0 / SC_A), reads=[stg], writes=[EA])
                elif not cfg.get('Bnobias'):
                    stg = stage[1]
                    S.dma("sp", lambda e, stg=stg, ui=ui: e.dma_start(out=stg[:, 0:1024], in_=bias_b_d[ui]), writes=[stg])
                    if not cfg.get('Bnoexp'):
                        S.op("act", lambda e, stg=stg: e.activation(out=EB[:, 0:1024], in_=stg[:, 0:1024], func=AF.Copy, scale=1.0 / SC_B), reads=[stg], writes=[EB])
                def proj_half(dst, Wt, half, bsplit=False):
                    for tg in range(4):
                        bank = next_pj()
                        for k in range(16):
                            S.op("pe", lambda e, bank=bank, Wt=Wt, k=k, half=half, tg=tg: e.matmul(
                                bank[:, 0:512], lhsT=Wt[:, k, half * 128:(half + 1) * 128], rhs=hT[:, k, tg * 512:(tg + 1) * 512],
                                start=(k == 0), stop=(k == 15)), reads=[Wt, hT], writes=[bank], sig=(k == 15))
                        l0 = tg * 512 // d
                        if not bsplit:
                            if d == 1:
                                evac(dst[:, half, tg * 512:(tg + 1) * 512], bank[:, 0:512], [bank], [dst])
                            else:
                                oap = dst[:, half, :].rearrange("p (c l) -> p c l", c=d)[:, :, l0:l0 + 512 // d]
                                iap = bank[:, 0:512].rearrange("p (l c) -> p c l", c=d)
                                evac(oap, iap, [bank], [dst])
                        else:
                            for hh in range(2):
                                p0 = hh * 64
                                if d == 1:
                                    evac(dst[p0:p0 + 64, hh, tg * 512:(tg + 1) * 512], bank[p0:p0 + 64, 0:512], [bank], [dst])
                                else:
                                    oap = dst[p0:p0 + 64, hh, :].rearrange("p (c l) -> p c l", c=d)[:, :, l0:l0 + 512 // d]
                                    iap = bank[p0:p0 + 64, 0:512].rearrange("p (l c) -> p c l", c=d)
                                    evac(oap, iap, [bank], [dst])

                for half in range(2):
                    proj_half(QT, Wq, half)
                if kind == "A":
                    for half in range(2):
                        proj_half(KT, Wk, half)
                else:
                    S.op("dve", lambda e: e.memset(KT[64:128, 0, :], 0.0), writes=[KT])
                    S.op("dve", lambda e: e.memset(KT[0:64, 1, :], 0.0), writes=[KT])
                for sbk in range(16):
                    c_, n_ = divmod(sbk, nblk)
                    t0 = n_ * 128 * d + c_
                    bank = next_pj()
                    for k in range(16):
                        S.op("pe", lambda e, bank=bank, k=k, t0=t0, d=d: e.matmul(
                            bank[:, 0:256], lhsT=hT[:, k, t0:t0 + 127 * d + 1:d], rhs=Wv[:, k, :],
                            start=(k == 0), stop=(k == 15)), reads=[Wv, hT], writes=[bank], sig=(k == 15))
                    evac(V[:, sbk, 0:256], bank[:, 0:256], [bank], [V])

                if kind == "A":
                    steps = [(c, kb) for c in range(8) for kb in range(2 * c + 2)]

                    def qk_stage(i):
                        c, kb = steps[i]
                        qlo = 2 * c if kb <= 2 * c else 2 * c + 1
                        off = (qlo - 2 * c) * 128
                        bank = pj[i % 3]
                        pt = PT[i % 3]
                        e0 = (qlo - kb) * 128
                        e1 = (2 * c + 2 - kb) * 128
                        for m in range(2):
                            S.op("pe", lambda e, bank=bank, m=m, kb=kb, qlo=qlo, c=c, off=off: e.matmul(
                                bank[:, m * 256 + off:m * 256 + 256], lhsT=KT[:, m, kb * 128:(kb + 1) * 128],
                                rhs=QT[:, m, qlo * 128:(2 * c + 2) * 128], start=True, stop=False),
                                reads=[KT, QT], writes=[bank], sig=False)
                            S.op("pe", lambda e, bank=bank, m=m, off=off, e0=e0, e1=e1: e.matmul(
                                bank[:, m * 256 + off:m * 256 + 256], lhsT=identb[:], rhs=EA[:, e0:e1], start=False, stop=True),
                                reads=[identb, EA], writes=[bank], sig=(m == 1))
                        S.op("act", lambda e, bank=bank, pt=pt, off=off: e.activation(
                            out=pt[:, :, off:256], in_=bank[:, 0:512].rearrange("p (m q) -> p m q", m=2)[:, :, off:256],
                            func=AF.Exp, scale=SC_A), reads=[bank], writes=[pt])

                    def pv_stage(i):
                        c, kb = steps[i]
                        qlo = 2 * c if kb <= 2 * c else 2 * c + 1
                        pt = PT[i % 3]
                        mm = [(m, qb) for m in range(2) for qb in range(qlo, 2 * c + 2)]
                        for ii, (m, qb) in enumerate(mm):
                            j = qb - 2 * c
                            a_ = acc[m * 2 + j]
                            S.op("pe", lambda e, a_=a_, pt=pt, m=m, j=j, kb=kb, qb=qb: e.matmul(
                                a_[:, 0:264], lhsT=pt[:, m, j * 128:(j + 1) * 128], rhs=V[:, kb, 0:264],
                                start=(kb == 0), stop=(kb == qb)), reads=[pt, V], writes=[a_], sig=(ii == len(mm) - 1))

                    def epi1(c):
                        for j in range(2):
                            a0, a1 = acc[j], acc[2 + j]
                            rr_, nl_, o1_, oo_ = rr[j], nl[j], o1[j], oo[j]
                            S.op("dve", lambda e, a0=a0, rr_=rr_: e.reciprocal(out=rr_[:, 0:1], in_=a0[:, 256:257]), reads=[a0], writes=[rr_])
                            S.op("dve", lambda e, a1=a1, rr_=rr_: e.reciprocal(out=rr_[:, 1:2], in_=a1[:, 256:257]), reads=[a1], writes=[rr_])
                            S.op("dve", lambda e, rr_=rr_, nl_=nl_: e.tensor_tensor(out=nl_[:], in0=rr_[:, 1:2], in1=neglam[:], op=ALU.mult), reads=[rr_, neglam], writes=[nl_])
                            S.op("act", lambda e, a0=a0, rr_=rr_, o1_=o1_: e.activation(out=o1_[:], in_=a0[:, 0:256], func=AF.Copy, scale=rr_[:, 0:1]), reads=[a0, rr_], writes=[o1_])
                            S.op("dve", lambda e, a1=a1, nl_=nl_, o1_=o1_, oo_=oo_: e.scalar_tensor_tensor(out=oo_[:], in0=a1[:, 0:256], scalar=nl_[:], in1=o1_[:], op0=ALU.mult, op1=ALU.add),
                                 reads=[a1, nl_, o1_], writes=[oo_])
                        for j in range(2):
                            oo_ = oo[j]
                            ob_ = oab[j]
                            S.op("act", lambda e, oo_=oo_: e.activation(out=junk2[:], in_=oo_[:], func=AF.Square, accum_out=ss2[:]), reads=[oo_], writes=[junk2, ss2])
                            rstd_ops(ss2, t2, rs2, 1.0 / 256, 1e-5)
                            S.op("dve", lambda e, ob_=ob_, oo_=oo_: e.scalar_tensor_tensor(out=ob_[:], in0=oo_[:], scalar=rs2[:], in1=gsub[:], op0=ALU.mult, op1=ALU.mult),
                                 reads=[oo_, rs2, gsub], writes=[ob_])

                    def epi2(c):
                        for j in range(2):
                            qb = 2 * c + j
                            ob_ = oab[j]
                            pb_ = ptb[0]
                            for hh in range(2):
                                S.op("pe", lambda e, pb_=pb_, hh=hh, ob_=ob_: e.transpose(pb_[:, hh * 128:(hh + 1) * 128], ob_[:, hh * 128:(hh + 1) * 128], identb[:]),
                                     reads=[ob_, identb], writes=[pb_], sig=(hh == 1))
                            ql = qb % 4
                            evac(oaTu[:, :, ql * 128:(ql + 1) * 128], pb_[:, 0:256].rearrange("p (k t) -> p k t", k=2), [pb_], [oaTu])
                            if ql == 3:
                                q0 = (qb - 3) * 128
                                S.dma("sp", lambda e, h=h, q0=q0: e.dma_start(out=oaT_d[s, :, 2 * h:2 * h + 2, q0:q0 + 512], in_=oaTu[:]),
                                      reads=[oaTu], writes=[oaT_b])

                    pend_epi2 = None
                    qk_stage(0)
                    qk_stage(1)
                    for i, (c, kb) in enumerate(steps):
                        if i + 2 < len(steps):
                            qk_stage(i + 2)
                        pv_stage(i)
                        if kb == 1 and pend_epi2 is not None:
                            epi2(pend_epi2)
                            pend_epi2 = None
                        if kb == 2 * c + 1:
                            epi1(c)
                            pend_epi2 = c
                    epi2(pend_epi2)
                else:
                    it = 0
                    EBv = EB[:, 0:1024].rearrange("p (h w q) -> p h w q", h=4, w=2)
                    for pair in range(2):
                        proj_half(KT, Wk, pair, bsplit=True)
                        def b_qk(sbk, it_):
                            c_, n_ = divmod(sbk, nblk)
                            hp = n_ > 0
                            bank = pj[it_ % 3]
                            pt = PTB[it_ % 3]
                            nw = 2 if hp else 1
                            i = 0
                            for hh in range(2):
                                for wh in range(nw):
                                    i += 1
                                    S.op("pe", lambda e, bank=bank, hh=hh, wh=wh, pair=pair, sbk=sbk: e.matmul(
                                        bank[:, (hh * 2 + wh) * 128:(hh * 2 + wh + 1) * 128],
                                        lhsT=KT[:, hh, (sbk - wh) * 128:(sbk - wh + 1) * 128],
                                        rhs=QT[:, pair, sbk * 128:(sbk + 1) * 128], start=True, stop=False),
                                        reads=[KT, QT], writes=[bank], sig=False)
                                    S.op("pe", lambda e, bank=bank, hh=hh, wh=wh, pair=pair: e.matmul(
                                        bank[:, (hh * 2 + wh) * 128:(hh * 2 + wh + 1) * 128],
                                        lhsT=identb[:], rhs=EBv[:, pair * 2 + hh, wh, :], start=False, stop=True),
                                        reads=[identb, EB], writes=[bank], sig=(i == 2 * nw))
                            ptv = pt[:].rearrange("p (h w q) -> p h w q", h=2, w=2)
                            bkv = bank[:, 0:512].rearrange("p (h w q) -> p h w q", h=2, w=2)
                            if hp:
                                S.op("act", lambda e, bank=bank, pt=pt: e.activation(out=pt[:], in_=bank[:, 0:512], func=AF.Exp, scale=SC_B),
                                     reads=[bank], writes=[pt])
                            else:
                                S.op("act", lambda e, ptv=ptv, bkv=bkv: e.activation(out=ptv[:, :, 0, :], in_=bkv[:, :, 0, :], func=AF.Exp, scale=SC_B),
                                     reads=[bank], writes=[pt])

                        def b_pv(sbk, it_):
                            c_, n_ = divmod(sbk, nblk)
                            hp = n_ > 0
                            pt = PTB[it_ % 3]
                            ut = acc[it_ % 4]
                            nw = 2 if hp else 1
                            ptv = pt[:].rearrange("p (h w q) -> p h w q", h=2, w=2)
                            for hh in range(2):
                                for wh in range(nw):
                                    kblk = sbk - wh
                                    S.op("pe", lambda e, ut=ut, kblk=kblk, pair=pair, ptv=ptv, hh=hh, wh=wh, nw=nw: e.matmul(
                                        ut[:, hh * 128:(hh + 1) * 128], lhsT=V[:, kblk, pair * 128:(pair + 1) * 128], rhs=ptv[:, hh, wh, :],
                                        start=(wh == 0), stop=(wh == nw - 1)), reads=[V, pt], writes=[ut], sig=False)
                            for hh in range(2):
                                for wh in range(nw):
                                    S.op("pe", lambda e, ut=ut, ptv=ptv, hh=hh, wh=wh, nw=nw: e.matmul(
                                        ut[:, (2 + hh) * 128:(3 + hh) * 128], lhsT=onesb[:], rhs=ptv[:, hh, wh, :],
                                        start=(wh == 0), stop=(wh == nw - 1)), reads=[onesb, pt], writes=[ut], sig=(hh == 1 and wh == nw - 1))
                            t0 = n_ * 128 * d + c_
                            for hh in range(2):
                                p0 = hh * 64
                                oap = accUD[p0:p0 + 64, pair, :, t0:t0 + 127 * d + 1:d]
                                iap = ut[p0:p0 + 64, 0:512].rearrange("p (u h q) -> p u h q", u=2, h=2)[:, :, hh, :]
                                if g == 0:
                                    S.op("dve", lambda e, oap=oap, iap=iap: e.tensor_copy(out=oap, in_=iap), reads=[ut], writes=[accUD])
                                else:
                                    S.op("dve", lambda e, oap=oap, iap=iap: e.tensor_tensor(out=oap, in0=oap, in1=iap, op=ALU.add), reads=[ut], writes=[accUD])

                        b_qk(0, it)
                        b_qk(1, it + 1)
                        for sbk in range(16):
                            if sbk + 2 < 16:
                                b_qk(sbk + 2, it + 2)
                            b_pv(sbk, it)
                            it += 1
                    if g == 2:
                        for pair in range(2):
                            S.op("dve", lambda e, pair=pair: e.reciprocal(out=accUD[:, pair, 1, :], in_=accUD[:, pair, 1, :]), reads=[accUD], writes=[accUD])
                        for tq in range(4):
                            for pair in range(2):
                                S.op("dve", lambda e, pair=pair, tq=tq: e.tensor_tensor(
                                    out=obTq[:, pair, :], in0=accUD[:, pair, 0, tq * 512:(tq + 1) * 512],
                                    in1=accUD[:, pair, 1, tq * 512:(tq + 1) * 512], op=ALU.mult), reads=[accUD], writes=[obTq])
                            S.dma("sp", lambda e, quad=quad, tq=tq: e.dma_start(out=obT_d[s, :, quad * 2:quad * 2 + 2, tq * 512:(tq + 1) * 512], in_=obTq[:]),
                                  reads=[obTq], writes=[obT_b])
            S.barrier()

        with ExitStack() as st:
          if cfg.get('C', True):
              oaTh = mk(st, "oaTh", [128, 16, 1024], BF16)
              obTh = mk(st, "obTh", [128, 4, 1024], BF16)
              mT = mk(st, "mT", [128, 16, 1024], BF16)
              Wg1 = [mk(st, "Wg1_%d" % i, [128, 16, 128], BF16) for i in range(2)]
              Wg2 = [mk(st, "Wg2_%d" % i, [128, 16, 128], BF16) for i in range(2)]
              Wa = [mk(st, "Wa_%d" % i, [128, 16, 128], BF16) for i in range(2)]
              Wb = [mk(st, "Wb_%d" % i, [128, 4, 128], BF16) for i in range(2)]
              Wo = mk(st, "Wo", [128, 16, 512], BF16)
              xt = [mk(st, "xt%d" % i, [128, 512], F32) for i in range(2)]
              x1t = [mk(st, "x1t%d" % i, [128, 512], F32) for i in range(2)]
              s1 = mk(st, "s1", [128, 512], F32)
              s2 = mk(st, "s2", [128, 512], F32)
              pG1 = mkp(st, "pG1", [128, 512], F32)
              pG2 = mkp(st, "pG2", [128, 512], F32)
              pPA = mkp(st, "pPA", [128, 512], F32)
              pPB = mkp(st, "pPB", [128, 512], F32)
              pO = [mkp(st, "pO%d" % i, [128, 512], F32) for i in range(2)]
              wg_v = w_gate_d.rearrange("(k p) c -> p k c", p=128)
              wa_v = w_pa_d.rearrange("(k p) c -> p k c", p=128)
              wb_v = w_pb_d.rearrange("(k p) c -> p k c", p=128)
              wo_v = w_out_d.rearrange("(k p) c -> p k c", p=128)
              for hf in range(2):
                  t0 = hf * 1024
                  for kk in range(4):
                      S.dma("sp", lambda e, kk=kk, t0=t0: e.dma_start(out=oaTh[:, kk * 4:(kk + 1) * 4, :], in_=oaT_d[s, :, kk * 4:(kk + 1) * 4, t0:t0 + 1024]),
                            reads=[oaT_b], writes=[oaTh])
                  S.dma("sp", lambda e, t0=t0: e.dma_start(out=obTh[:], in_=obT_d[s, :, :, t0:t0 + 1024]), reads=[obT_b], writes=[obTh])
                  for c in range(16):
                      w1_, w2_, wa_, wb_ = Wg1[c % 2], Wg2[c % 2], Wa[c % 2], Wb[c % 2]
                      wload(w1_, lambda a, b, w=w1_: w[:, a:b, :], wg_v[:, :, c * 128:(c + 1) * 128], 16, 2)
                      wload(w2_, lambda a, b, w=w2_: w[:, a:b, :], wg_v[:, :, 2048 + c * 128:2048 + (c + 1) * 128], 16, 2)
                      wload(wa_, lambda a, b, w=wa_: w[:, a:b, :], wa_v[:, :, c * 128:(c + 1) * 128], 16, 2)
                      wload(wb_, lambda a, b, w=wb_: w[:, a:b, :], wb_v[:, :, c * 128:(c + 1) * 128], 4, 1)
                      for tg in range(2):
                          ta = t0 + tg * 512
                          for bank, Wt, src, off_, nk in ((pG1, w1_, hT, ta, 16), (pG2, w2_, hT, ta, 16), (pPA, wa_, oaTh, tg * 512, 16), (pPB, wb_, obTh, tg * 512, 4)):
                              for k in range(nk):
                                  S.op("pe", lambda e, bank=bank, Wt=Wt, src=src, off_=off_, k=k, nk=nk: e.matmul(
                                      bank[:, 0:512], lhsT=Wt[:, k, :], rhs=src[:, k, off_:off_ + 512], start=(k == 0), stop=(k == nk - 1)),
                                      reads=[Wt, src], writes=[bank], sig=(k == nk - 1))
                          S.op("act", lambda e: e.activation(out=s1[:], in_=pG1[:, 0:512], func=AF.Sigmoid), reads=[pG1], writes=[s1])
                          S.op("act", lambda e: e.activation(out=s2[:], in_=pG2[:, 0:512], func=AF.Sigmoid), reads=[pG2], writes=[s2])
                          S.op("dve", lambda e: e.tensor_tensor(out=s1[:], in0=s1[:], in1=pPA[:, 0:512], op=ALU.mult), reads=[s1, pPA], writes=[s1])
                          S.op("dve", lambda e: e.tensor_tensor(out=s2[:], in0=s2[:], in1=pPB[:, 0:512], op=ALU.mult), reads=[s2, pPB], writes=[s2])
                          S.op("dve", lambda e, c=c, tg=tg: e.tensor_tensor(out=mT[:, c, tg * 512:(tg + 1) * 512], in0=s1[:], in1=s2[:], op=ALU.add),
                               reads=[s1, s2], writes=[mT])
                  n = 0
                  for cg in range(4):
                      wload(Wo, lambda a, b: Wo[:, a:b, :], wo_v[:, :, cg * 512:(cg + 1) * 512], 16, 4)
                      for tt in range(8):
                          r0 = s * SEQ + t0 + tt * 128
                          bank, xt_, x1_ = pO[n % 2], xt[n % 2], x1t[n % 2]
                          n += 1
                          S.dma("sp", lambda e, xt_=xt_, r0=r0, cg=cg: e.dma_start(out=xt_[:], in_=x_d[r0:r0 + 128, cg * 512:(cg + 1) * 512]), writes=[xt_])
                          for k in range(16):
                              S.op("pe", lambda e, bank=bank, k=k, tt=tt: e.matmul(bank[:, 0:512], lhsT=mT[:, k, tt * 128:(tt + 1) * 128], rhs=Wo[:, k, :],
                                                                                    start=(k == 0), stop=(k == 15)), reads=[mT, Wo], writes=[bank], sig=(k == 15))
                          S.op("dve", lambda e, bank=bank, xt_=xt_, x1_=x1_: e.tensor_tensor(out=x1_[:], in0=bank[:, 0:512], in1=xt_[:], op=ALU.add),
                               reads=[bank, xt_], writes=[x1_])
                          S.dma("sp", lambda e, x1_=x1_, r0=r0, cg=cg: e.dma_start(out=x1_d[r0:r0 + 128, cg * 512:(cg + 1) * 512], in_=x1_[:]),
                                reads=[x1_], writes=[x1_b])
              S.barrier()
        hstack.close()

    with ExitStack() as st:
        stage = [mk(st, "rstage%d" % i, [128, D], F32) for i in range(2)]
        junk = mk(st, "rjunk", [128, D], BF16)
        gb = mk(st, "rgb", [128, D], F32)
        h2 = [mk(st, "h2_%d" % i, [128, D], F32) for i in range(2)]
        h2b = [mk(st, "h2b%d" % i, [128, D], BF16) for i in range(2)]
        h2T = [mk(st, "h2T%d" % i, [128, 16, 128], F32) for i in range(2)]
        Wr = mk(st, "Wr", [128, 16, 36], F32)
        ssr = [mk(st, "rss%d" % i, [128, 1], F32) for i in range(2)]
        t1r = [mk(st, "rt1%d" % i, [128, 1], F32) for i in range(2)]
        rstdr = [mk(st, "rrstd%d" % i, [128, 1], F32) for i in range(2)]
        lgs = [mk(st, "lg%d" % i, [128, 36], F32) for i in range(2)]
        sms = [{n: mk(st, "r%d_%s" % (i, n), [128, w], F32) for n, w in (
            ("cmax", 1), ("ncm", 1), ("ohg", 4), ("ce", 4), ("csum", 1), ("pg", 1), ("fsel", 8), ("v1", 1), ("oh1", 8), ("fm", 8),
            ("v2", 1), ("oh2", 8), ("dl", 1), ("ed", 1), ("den", 1), ("rden", 1), ("A1", 32), ("A2", 32), ("As", 32), ("pos", 32),
            ("ovf", 32), ("sl", 32), ("tmp", 32), ("sf", 2))} for i in range(2)]
        Asbs = [mk(st, "Asb%d" % i, [128, 32], BF16) for i in range(2)]
        ptf = [mkp(st, "ptf%d" % i, [128, 512], F32) for i in range(4)]
        plgs = [mkp(st, "plg%d" % i, [128, 512], F32) for i in range(2)]
        pcns = [mkp(st, "pcn%d" % i, [128, 512], F32) for i in range(2)]
        S.dma("sp", lambda e: e.dma_start(out=gb[:], in_=g_ffn_d.partition_broadcast(128)), writes=[gb])
        S.dma("sp", lambda e: e.dma_start(out=Wr[:], in_=w_rt_d.rearrange("(k p) c -> p k c", p=128)), writes=[Wr])

        def dv(fn, reads, writes):
            S.op("dve", fn, reads=reads, writes=writes)

        def r_front(i):
            stg = stage[i % 2]
            hb = h2b[i % 2]
            h2_ = h2[i % 2]
            h2T_ = h2T[i % 2]
            ss, t1, rstd = ssr[i % 2], t1r[i % 2], rstdr[i % 2]
            plg = plgs[i % 2]
            S.dma("sp", lambda e, stg=stg, i=i: e.dma_start(out=stg[:], in_=x1_d[i * 128:(i + 1) * 128, :]), reads=[x1_b], writes=[stg])
            S.op("act", lambda e, stg=stg: e.activation(out=junk[:], in_=stg[:], func=AF.Square, accum_out=ss[:]), reads=[stg], writes=[junk, ss])
            rstd_ops(ss, t1, rstd, 1.0 / D, 1e-6)
            dv(lambda e, stg=stg: e.scalar_tensor_tensor(out=h2_[:], in0=stg[:], scalar=rstd[:], in1=gb[:], op0=ALU.mult, op1=ALU.mult), [stg, rstd, gb], [h2_])
            S.op("act", lambda e, hb=hb: e.activation(out=hb[:], in_=h2_[:], func=AF.Copy), reads=[h2_], writes=[hb])
            for k in range(16):
                S.op("pe", lambda e, k=k: e.transpose(ptf[k // 4][:, (k % 4) * 128:(k % 4 + 1) * 128], h2_[:, k * 128:(k + 1) * 128], identf[:]),
                     reads=[h2_, identf], writes=[ptf[k // 4]], sig=(k % 4 == 3))
            for j in range(4):
                evac(h2T_[:, 4 * j:4 * j + 4, :], ptf[j][:].rearrange("p (k t) -> p k t", k=4), [ptf[j]], [h2T_])
            for k in range(16):
                S.op("pe", lambda e, k=k: e.matmul(plg[:, 0:36], lhsT=h2T_[:, k, :], rhs=Wr[:, k, :], start=(k == 0), stop=(k == 15)),
                     reads=[h2T_, Wr], writes=[plg], sig=(k == 15))

        def r_back(i):
            hb = h2b[i % 2]
            m = sms[i % 2]
            lg = lgs[i % 2]
            Asb = Asbs[i % 2]
            plg = plgs[i % 2]
            pcn = pcns[i % 2]
            dv(lambda e: e.tensor_copy(out=lg[:], in_=plg[:, 0:36]), [plg], [lg])
            fine = lg[:, 4:36].rearrange("p (g j) -> p g j", g=4)
            dv(lambda e: e.reduce_max(out=m["cmax"][:], in_=lg[:, 0:4], axis=AX.X), [lg], [m["cmax"]])
            dv(lambda e: e.tensor_scalar(out=m["ohg"][:], in0=lg[:, 0:4], scalar1=m["cmax"][:], scalar2=None, op0=ALU.is_equal), [lg, m["cmax"]], [m["ohg"]])
            dv(lambda e: e.tensor_scalar(out=m["ncm"][:], in0=m["cmax"][:], scalar1=-1.0, scalar2=None, op0=ALU.mult), [m["cmax"]], [m["ncm"]])
            S.op("act", lambda e: e.activation(out=m["ce"][:], in_=lg[:, 0:4], func=AF.Exp, bias=m["ncm"][:], accum_out=m["csum"][:]),
                 reads=[lg, m["ncm"]], writes=[m["ce"], m["csum"]])
            dv(lambda e: e.reciprocal(out=m["pg"][:], in_=m["csum"][:]), [m["csum"]], [m["pg"]])
            dv(lambda e: e.tensor_scalar(out=m["fsel"][:], in0=fine[:, 0, :], scalar1=m["ohg"][:, 0:1], scalar2=None, op0=ALU.mult), [lg, m["ohg"]], [m["fsel"]])
            for g in range(1, 4):
                dv(lambda e, g=g: e.scalar_tensor_tensor(out=m["fsel"][:], in0=fine[:, g, :], scalar=m["ohg"][:, g:g + 1], in1=m["fsel"][:], op0=ALU.mult, op1=ALU.add),
                   [lg, m["ohg"], m["fsel"]], [m["fsel"]])
            dv(lambda e: e.reduce_max(out=m["v1"][:], in_=m["fsel"][:], axis=AX.X), [m["fsel"]], [m["v1"]])
            dv(lambda e: e.tensor_scalar(out=m["oh1"][:], in0=m["fsel"][:], scalar1=m["v1"][:], scalar2=None, op0=ALU.is_equal), [m["fsel"], m["v1"]], [m["oh1"]])
            dv(lambda e: e.scalar_tensor_tensor(out=m["fm"][:], in0=m["oh1"][:], scalar=-1e30, in1=m["fsel"][:], op0=ALU.mult, op1=ALU.add),
               [m["oh1"], m["fsel"]], [m["fm"]])
            dv(lambda e: e.reduce_max(out=m["v2"][:], in_=m["fm"][:], axis=AX.X), [m["fm"]], [m["v2"]])
            dv(lambda e: e.tensor_scalar(out=m["oh2"][:], in0=m["fm"][:], scalar1=m["v2"][:], scalar2=None, op0=ALU.is_equal), [m["fm"], m["v2"]], [m["oh2"]])
            dv(lambda e: e.tensor_tensor(out=m["dl"][:], in0=m["v2"][:], in1=m["v1"][:], op=ALU.subtract), [m["v1"], m["v2"]], [m["dl"]])
            S.op("act", lambda e: e.activation(out=m["ed"][:], in_=m["dl"][:], func=AF.Exp), reads=[m["dl"]], writes=[m["ed"]])
            dv(lambda e: e.tensor_scalar(out=m["den"][:], in0=m["ed"][:], scalar1=1.0, scalar2=None, op0=ALU.add), [m["ed"]], [m["den"]])
            dv(lambda e: e.reciprocal(out=m["rden"][:], in_=m["den"][:]), [m["den"]], [m["rden"]])
            dv(lambda e: e.tensor_tensor(out=gates[:, i, 0:1], in0=m["pg"][:], in1=m["rden"][:], op=ALU.mult), [m["pg"], m["rden"]], [gates])
            dv(lambda e: e.tensor_tensor(out=gates[:, i, 1:2], in0=gates[:, i, 0:1], in1=m["ed"][:], op=ALU.mult), [gates, m["ed"]], [gates])
            for g in range(4):
                dv(lambda e, g=g: e.tensor_scalar(out=m["A1"][:, g * 8:(g + 1) * 8], in0=m["oh1"][:], scalar1=m["ohg"][:, g:g + 1], scalar2=None, op0=ALU.mult),
                   [m["oh1"], m["ohg"]], [m["A1"]])
                dv(lambda e, g=g: e.tensor_scalar(out=m["A2"][:, g * 8:(g + 1) * 8], in0=m["oh2"][:], scalar1=m["ohg"][:, g:g + 1], scalar2=None, op0=ALU.mult),
                   [m["oh2"], m["ohg"]], [m["A2"]])
            dv(lambda e: e.tensor_tensor(out=m["As"][:], in0=m["A1"][:], in1=m["A2"][:], op=ALU.add), [m["A1"], m["A2"]], [m["As"]])
            dv(lambda e: e.tensor_copy(out=Asb[:], in_=m["As"][:]), [m["As"]], [Asb])
            S.op("pe", lambda e: e.matmul(pcn[:, 0:32], lhsT=trib[:], rhs=Asb[:], start=True, stop=True), reads=[trib, Asb], writes=[pcn], sig=False)
            S.op("pe", lambda e: e.matmul(pcn[:, 32:64], lhsT=onesb[:], rhs=Asb[:], start=True, stop=True), reads=[onesb, Asb], writes=[pcn])
            dv(lambda e: e.tensor_tensor(out=m["pos"][:], in0=pcn[:, 0:32], in1=cbase[:], op=ALU.add), [pcn, cbase], [m["pos"]])
            dv(lambda e: e.tensor_tensor(out=cbase[:], in0=pcn[:, 32:64], in1=cbase[:], op=ALU.add), [pcn, cbase], [cbase])
            dv(lambda e: e.tensor_scalar(out=m["ovf"][:], in0=m["pos"][:], scalar1=float(CAP), scalar2=None, op0=ALU.is_ge), [m["pos"]], [m["ovf"]])
            dv(lambda e: e.scalar_tensor_tensor(out=m["sl"][:], in0=m["ovf"][:], scalar=1.0e7, in1=m["pos"][:], op0=ALU.mult, op1=ALU.add),
               [m["ovf"], m["pos"]], [m["sl"]])
            dv(lambda e: e.tensor_tensor(out=m["sl"][:], in0=m["sl"][:], in1=ecrow[:], op=ALU.add), [m["sl"], ecrow], [m["sl"]])
            for kx, An in ((0, "A1"), (1, "A2")):
                dv(lambda e, An=An: e.tensor_tensor(out=m["tmp"][:], in0=m[An][:], in1=m["sl"][:], op=ALU.mult), [m[An], m["sl"]], [m["tmp"]])
                dv(lambda e, kx=kx: e.reduce_sum(out=m["sf"][:, kx:kx + 1], in_=m["tmp"][:], axis=AX.X), [m["tmp"]], [m["sf"]])
            dv(lambda e: e.tensor_copy(out=slots[:, 2 * i:2 * i + 2], in_=m["sf"][:]), [m["sf"]], [slots])
            dv(lambda e: e.tensor_scalar(out=m["sf"][:], in0=m["sf"][:], scalar1=float(NSLOT), scalar2=None, op0=ALU.min), [m["sf"]], [m["sf"]])
            dv(lambda e: e.tensor_copy(out=slotg[:, 2 * i:2 * i + 2], in_=m["sf"][:]), [m["sf"]], [slotg])
            for kx in range(2):
                S.dma("pool", lambda e, kx=kx, hb=hb: e.indirect_dma_start(
                    out=xin_d, out_offset=bass.IndirectOffsetOnAxis(ap=slots[:, 2 * i + kx:2 * i + kx + 1], axis=0), in_=hb[:], in_offset=None,
                    bounds_check=bc_reg, oob_is_err=False), reads=[hb, slots], writes=[xin_b])

        ntr = cfg.get('ntR', NT)
        LAG = 3
        for i0 in range(0, ntr, 2):
            tiles = [i0] + ([i0 + 1] if i0 + 1 < ntr else [])
            for i in tiles:
                r_front(i)
            lists = [S.record(lambda i=i: r_back(i)) for i in tiles]
            pos_ = [0] * len(lists)
            step = 0
            while any(pos_[k] < len(lists[k]) for k in range(len(lists))):
                for k in range(len(lists)):
                    if pos_[k] >= len(lists[k]) or (k == 1 and step < LAG and pos_[0] < len(lists[0])):
                        continue
                    S.replay(lists[k][pos_[k]])
                    pos_[k] += 1
                step += 1
        S.barrier()

    with ExitStack() as st:
        xtok = [mk(st, "xtok%d" % i, [128, D], BF16) for i in range(2)]
        XTs = [mk(st, "XT%d" % i, [128, 16, CAP], BF16) for i in range(2)]
        W13 = [mk(st, "W13_%d" % i, [128, 16, 512], BF16) for i in range(4)]
        W2s = [mk(st, "W2s_%d" % i, [128, 8, 512], BF16) for i in range(4)]
        W2f = [mk(st, "W2f_%d" % i, [128, 8, 512], F32) for i in range(2)]
        HT = mk(st, "HT", [128, 8, CAP], BF16)
        sl1 = mk(st, "sl1", [128, CCH], F32)
        yt = [mk(st, "yt%d" % i, [128, 512], F32) for i in range(4)]
        ptb = [mkp(st, "eptb%d" % i, [128, 1024], BF16) for i in range(2)]
        pb1 = [mkp(st, "pb1_%d" % i, [128, 512], F32) for i in range(2)]
        pb3 = [mkp(st, "pb3_%d" % i, [128, 512], F32) for i in range(2)]
        py = [mkp(st, "py%d" % i, [128, 512], F32) for i in range(2)]
        NTI = CAP // 128
        nE = cfg.get('nE', NE)
        S.op("dve", lambda e: e.memset(yt[0][:], 0.0), writes=[yt[0]])
        for cg in range(4):
            S.dma("sp", lambda e, cg=cg: e.dma_start(out=ybuf_d[NSLOT:NSLOT + 128, cg * 512:(cg + 1) * 512], in_=yt[0][:]), reads=[yt[0]], writes=[ybuf_b])
        n13 = 0
        n2 = 0
        ny = 0
        nxc = [0]

        def t_chunks(ex):
            XTe = XTs[ex % 2]
            chunks = []
            for ti in range(NTI):
                holder = {}
                for j in range(2):
                    def chunk(ti=ti, j=j, holder=holder):
                        if j == 0:
                            xk = xtok[nxc[0] % 2]
                            nxc[0] += 1
                            holder["xk"] = xk
                            r0 = ex * CAP + ti * 128
                            S.dma("sp", lambda e: e.dma_start(out=xk[:], in_=xin_d[r0:r0 + 128, :]), reads=[xin_b], writes=[xk])
                        xk = holder["xk"]
                        for k in range(8 * j, 8 * j + 8):
                            S.op("pe", lambda e, k=k: e.transpose(ptb[j][:, (k % 8) * 128:(k % 8 + 1) * 128], xk[:, k * 128:(k + 1) * 128], identb[:]),
                                 reads=[xk, identb], writes=[ptb[j]], sig=(k % 8 == 7))
                        evac(XTe[:, 8 * j:8 * j + 8, ti * 128:(ti + 1) * 128], ptb[j][:].rearrange("p (k t) -> p k t", k=8), [ptb[j]], [XTe])
                    chunks.append(chunk)
            return chunks

        if nE:
            for ch in t_chunks(0):
                ch()
        for ex in range(nE):
            XT = XTs[ex % 2]
            w1v = w1_d[ex].rearrange("(k p) c -> p k c", p=128)
            w3v = w3_d[ex].rearrange("(k p) c -> p k c", p=128)
            w2v = w2_d[ex].rearrange("(k p) c -> p k c", p=128)
            w2slot = [W2s[(n2 + cg) % 4] for cg in range(4)]

            def w2_load(cg):
                stg_ = W2f[cg % 2]
                for hh_ in range(2):
                    S.dma("sp", lambda e, hh_=hh_: e.dma_start(out=stg_[:, hh_ * 4:(hh_ + 1) * 4, :], in_=w2v[:, hh_ * 4:(hh_ + 1) * 4, cg * 512:(cg + 1) * 512]),
                          writes=[stg_])

            def w2_cast(cg):
                stg_ = W2f[cg % 2]
                dst_ = w2slot[cg]
                if cg % 2 == 0:
                    S.op("act", lambda e: e.activation(out=dst_[:], in_=stg_[:], func=AF.Copy), reads=[stg_], writes=[dst_])
                else:
                    S.op("dve", lambda e: e.tensor_copy(out=dst_[:], in_=stg_[:]), reads=[stg_], writes=[dst_])

            w2_load(0)
            w2_load(1)
            for hf in range(2):
                wa_ = W13[n13 % 4]
                wb_ = W13[(n13 + 1) % 4]
                n13 += 2
                wload(wa_, lambda a, b, w=wa_: w[:, a:b, :], w1v[:, :, hf * 512:(hf + 1) * 512], 16, 4)
                wload(wb_, lambda a, b, w=wb_: w[:, a:b, :], w3v[:, :, hf * 512:(hf + 1) * 512], 16, 4)
                for hc in range(4):
                    for ci in range(CAP // CCH):
                        b1, b3 = pb1[ci % 2], pb3[ci % 2]
                        for bank, Wt in ((b1, wa_), (b3, wb_)):
                            for k in range(16):
                                S.op("pe", lambda e, bank=bank, Wt=Wt, k=k, hc=hc, ci=ci: e.matmul(
                                    bank[:, 0:CCH], lhsT=Wt[:, k, hc * 128:(hc + 1) * 128], rhs=XT[:, k, ci * CCH:(ci + 1) * CCH],
                                    start=(k == 0), stop=(k == 15)), reads=[Wt, XT], writes=[bank], sig=(k == 15))
                        S.op("act", lambda e, b1=b1: e.activation(out=sl1[:], in_=b1[:, 0:CCH], func=AF.Silu), reads=[b1], writes=[sl1])
                        S.op("dve", lambda e, hf=hf, hc=hc, ci=ci, b3=b3: e.tensor_tensor(out=HT[:, hf * 4 + hc, ci * CCH:(ci + 1) * CCH], in0=sl1[:], in1=b3[:, 0:CCH], op=ALU.mult),
                             reads=[sl1, b3], writes=[HT])
                    if hc in (1, 3):
                        cgc = hf * 2 + (hc // 2)
                        w2_cast(cgc)
                        if cgc + 2 < 4:
                            w2_load(cgc + 2)
            nxt = t_chunks(ex + 1) if ex + 1 < nE else []
            it_ = 0
            for cg in range(4):
                w2_ = w2slot[cg]
                n2 += 1
                for ti in range(NTI):
                    bank, y_ = py[ny % 2], yt[ny % 4]
                    ny += 1
                    for k in range(8):
                        S.op("pe", lambda e, bank=bank, w2_=w2_, k=k, ti=ti: e.matmul(bank[:, 0:512], lhsT=HT[:, k, ti * 128:(ti + 1) * 128], rhs=w2_[:, k, :],
                                                                                      start=(k == 0), stop=(k == 7)), reads=[HT, w2_], writes=[bank], sig=(k == 7))
                    evac(y_[:], bank[:, 0:512], [bank], [y_])
                    r0 = ex * CAP + ti * 128
                    S.dma("sp", lambda e, y_=y_, r0=r0, cg=cg: e.dma_start(out=ybuf_d[r0:r0 + 128, cg * 512:(cg + 1) * 512], in_=y_[:]),
                          reads=[y_], writes=[ybuf_b])
                    it_ += 1
                    if it_ % 2 == 0 and nxt:
                        nxt.pop(0)()
            while nxt:
                nxt.pop(0)()
        S.barrier()

    with ExitStack() as st:
        Wpg = mk(st, "Wpg", [128, 16, D], BF16)
        Wpp = mk(st, "Wpp", [128, 2, D], BF16)
        gbp = mk(st, "gbp", [128, D], F32)
        gbf = mk(st, "gbf", [128, D], F32)
        junk = mk(st, "fjunk", [128, D], BF16)
        stage = [mk(st, "fstage%d" % i, [128, D], F32) for i in range(2)]
        Y1s = [mk(st, "Y1_%d" % i, [128, D], F32) for i in range(2)]
        Y2s = [mk(st, "Y2_%d" % i, [128, D], F32) for i in range(2)]
        x3s = [mk(st, "x3_%d" % i, [128, D], F32) for i in range(2)]
        h3s = [mk(st, "h3_%d" % i, [128, D], BF16) for i in range(2)]
        h3Ts = [mk(st, "h3T%d" % i, [128, 16, 128], BF16) for i in range(2)]
        ptls = [mk(st, "ptl%d" % i, [128, 256], F32) for i in range(2)]
        pbls = [mk(st, "pbl%d" % i, [128, 256], BF16) for i in range(2)]
        pTls = [mk(st, "pTl%d" % i, [128, 2, 128], BF16) for i in range(2)]
        sgs = [mk(st, "sg%d" % i, [128, 512], F32) for i in range(2)]
        ssa = [mk(st, "fssa%d" % i, [128, 1], F32) for i in range(2)]
        t1a = [mk(st, "ft1a%d" % i, [128, 1], F32) for i in range(2)]
        rsa = [mk(st, "frsa%d" % i, [128, 1], F32) for i in range(2)]
        ssb = [mk(st, "fssb%d" % i, [128, 1], F32) for i in range(2)]
        t1b = [mk(st, "ft1b%d" % i, [128, 1], F32) for i in range(2)]
        rsb = [mk(st, "frsb%d" % i, [128, 1], F32) for i in range(2)]
        ptb = [mkp(st, "fptb%d" % i, [128, 1024], BF16) for i in range(2)]
        ptp = mkp(st, "fptp", [128, 1024], BF16)
        pG = [mkp(st, "fpG%d" % i, [128, 512], F32) for i in range(2)]
        pP = [mkp(st, "fpP%d" % i, [128, 512], F32) for i in range(2)]
        wload(Wpg, lambda a, b: Wpg[:, a:b, :], w_pg_d.rearrange("(k p) c -> p k c", p=128), 16, 16)
        wload(Wpp, lambda a, b: Wpp[:, a:b, :], w_pp_d.rearrange("(k p) c -> p k c", p=128), 2, 2)
        S.dma("sp", lambda e: e.dma_start(out=gbp[:], in_=g_ple_d.partition_broadcast(128)), writes=[gbp])
        S.dma("sp", lambda e: e.dma_start(out=gbf[:], in_=g_fin_d.partition_broadcast(128)), writes=[gbf])
        fn_ = [0]
        ntf = cfg.get('ntF', NT)

        def f_front(i):
            stg, Y1, Y2, h3, ptl, pbl = stage[i % 2], Y1s[i % 2], Y2s[i % 2], h3s[i % 2], ptls[i % 2], pbls[i % 2]
            ss, t1, rstd = ssa[i % 2], t1a[i % 2], rsa[i % 2]
            S.dma("sp", lambda e: e.dma_start(out=stg[:], in_=x1_d[i * 128:(i + 1) * 128, :]), reads=[x1_b], writes=[stg])
            S.dma("sp", lambda e: e.dma_start(out=ptl[:], in_=p_d[i * 128:(i + 1) * 128, :]), writes=[ptl])
            for kx, Y in ((0, Y1), (1, Y2)):
                S.dma("pool", lambda e, kx=kx, Y=Y: e.indirect_dma_start(
                    out=Y[:], out_offset=None, in_=ybuf_d, in_offset=bass.IndirectOffsetOnAxis(ap=slotg[:, 2 * i + kx:2 * i + kx + 1], axis=0),
                    bounds_check=bc_reg2, oob_is_err=False), reads=[ybuf_b, slotg], writes=[Y])

        def f_prep(i):
            stg, Y1, Y2, h3, ptl, pbl = stage[i % 2], Y1s[i % 2], Y2s[i % 2], h3s[i % 2], ptls[i % 2], pbls[i % 2]
            ss, t1, rstd = ssa[i % 2], t1a[i % 2], rsa[i % 2]
            S.op("dve", lambda e: e.scalar_tensor_tensor(out=Y1[:], in0=Y1[:], scalar=gates[:, i, 0:1], in1=stg[:], op0=ALU.mult, op1=ALU.add),
                 reads=[Y1, gates, stg], writes=[Y1])
            S.op("dve", lambda e: e.scalar_tensor_tensor(out=Y1[:], in0=Y2[:], scalar=gates[:, i, 1:2], in1=Y1[:], op0=ALU.mult, op1=ALU.add),
                 reads=[Y2, gates, Y1], writes=[Y1])
            S.op("act", lambda e: e.activation(out=junk[:], in_=Y1[:], func=AF.Square, accum_out=ss[:]), reads=[Y1], writes=[junk, ss])
            rstd_ops(ss, t1, rstd, 1.0 / D, 1e-6)
            S.op("dve", lambda e: e.scalar_tensor_tensor(out=h3[:], in0=Y1[:], scalar=rstd[:], in1=gbp[:], op0=ALU.mult, op1=ALU.mult),
                 reads=[Y1, rstd, gbp], writes=[h3])
            S.op("act", lambda e: e.activation(out=pbl[:], in_=ptl[:], func=AF.Copy), reads=[ptl], writes=[pbl])

        def f_back(i):
            x2, x3, h3, h3T, pbl, pTl, sg = Y1s[i % 2], x3s[i % 2], h3s[i % 2], h3Ts[i % 2], pbls[i % 2], pTls[i % 2], sgs[i % 2]
            ss, t1, rstd = ssb[i % 2], t1b[i % 2], rsb[i % 2]
            for k in range(16):
                S.op("pe", lambda e, k=k: e.transpose(ptb[k // 8][:, (k % 8) * 128:(k % 8 + 1) * 128], h3[:, k * 128:(k + 1) * 128], identb[:]),
                     reads=[h3, identb], writes=[ptb[k // 8]], sig=(k % 8 == 7))
            for j in range(2):
                evac(h3T[:, 8 * j:8 * j + 8, :], ptb[j][:].rearrange("p (k t) -> p k t", k=8), [ptb[j]], [h3T])
            for k in range(2):
                S.op("pe", lambda e, k=k: e.transpose(ptp[:, k * 128:(k + 1) * 128], pbl[:, k * 128:(k + 1) * 128], identb[:]),
                     reads=[pbl, identb], writes=[ptp], sig=(k == 1))
            evac(pTl[:], ptp[:, 0:256].rearrange("p (k t) -> p k t", k=2), [ptp], [pTl])
            for cg in range(4):
                bG, bP = pG[fn_[0] % 2], pP[fn_[0] % 2]
                fn_[0] += 1
                for k in range(16):
                    S.op("pe", lambda e, bG=bG, k=k, cg=cg: e.matmul(bG[:, 0:512], lhsT=h3T[:, k, :], rhs=Wpg[:, k, cg * 512:(cg + 1) * 512],
                                                                     start=(k == 0), stop=(k == 15)), reads=[h3T, Wpg], writes=[bG], sig=(k == 15))
                for k in range(2):
                    S.op("pe", lambda e, bP=bP, k=k, cg=cg: e.matmul(bP[:, 0:512], lhsT=pTl[:, k, :], rhs=Wpp[:, k, cg * 512:(cg + 1) * 512],
                                                                     start=(k == 0), stop=(k == 1)), reads=[pTl, Wpp], writes=[bP], sig=(k == 1))
                S.op("act", lambda e, bG=bG: e.activation(out=sg[:], in_=bG[:, 0:512], func=AF.Sigmoid), reads=[bG], writes=[sg])
                S.op("dve", lambda e, bP=bP: e.tensor_tensor(out=sg[:], in0=sg[:], in1=bP[:, 0:512], op=ALU.mult), reads=[sg, bP], writes=[sg])
                S.op("dve", lambda e, cg=cg: e.tensor_tensor(out=x3[:, cg * 512:(cg + 1) * 512], in0=sg[:], in1=x2[:, cg * 512:(cg + 1) * 512], op=ALU.add),
                     reads=[sg, x2], writes=[x3])
                if cg == 1 and i + 1 < ntf:
                    f_prep(i + 1)
            S.op("act", lambda e: e.activation(out=junk[:], in_=x3[:], func=AF.Square, accum_out=ss[:]), reads=[x3], writes=[junk, ss])
            rstd_ops(ss, t1, rstd, 1.0 / D, 1e-6)
            S.op("dve", lambda e: e.scalar_tensor_tensor(out=x3[:], in0=x3[:], scalar=rstd[:], in1=gbf[:], op0=ALU.mult, op1=ALU.mult),
                 reads=[x3, rstd, gbf], writes=[x3])
            S.dma("sp", lambda e: e.dma_start(out=out_d[i * 128:(i + 1) * 128, :], in_=x3[:]), reads=[x3], writes=[out_b])

        if ntf:
            f_front(0)
            f_prep(0)
        for i in range(ntf):
            if i + 1 < ntf:
                f_front(i + 1)
            f_back(i)
        S.barrier()
    S.finish()
    gstack.close()
    return nc


def _t5_bucket(dist):
    dist = np.maximum(dist, 0)
    d_f = np.maximum(dist, 1).astype(np.float32)
    large = 16 + (np.log(d_f / np.float32(16)) / np.float32(math.log(2048 / 16)) * np.float32(16)).astype(np.int32)
    large = np.minimum(large, 31)
    return np.where(dist < 16, dist, large)


def _bias_layouts(rel_bias):
    rb = np.asarray(rel_bias, np.float32)
    NEG = np.float32(-1e30)
    k = np.arange(128)[:, None]
    col = np.arange(2048)[None, :]
    dist = col - k
    bk = _t5_bucket(dist)
    ba = np.empty((8, 128, 2048), np.float32)
    for h in range(8):
        ba[h] = np.where(dist >= 0, rb[bk, h], NEG)
    bb = np.empty((6, 128, 4, 2, 128), np.float32)
    q = np.arange(128)[None, :]
    for quad in range(2):
        for g in range(3):
            d = DIL[g][1]
            for hl in range(4):
                col_h = 8 + g * 8 + quad * 4 + hl
                rel_c = q - k
                rel_p = q - k + 128
                bb[quad * 3 + g, :, hl, 0, :] = np.where(rel_c >= 0, rb[_t5_bucket(rel_c * d), col_h], NEG)
                bb[quad * 3 + g, :, hl, 1, :] = np.where(rel_p <= 128, rb[_t5_bucket(rel_p * d), col_h], NEG)
    return ba, bb.reshape(6, 128, 1024)


_NC = None


def prep_inputs(x, p, rel_bias, norm_mix_g, w_in, w_gate, lambda_q1, lambda_k1, lambda_q2, lambda_k2, subln_g,
                w_proj_a, w_proj_b, w_out, norm_ffn_g, w_coarse, w_fine, w1, w3, w2, norm_ple_g, w_ple_gate,
                w_ple_proj, final_norm_g):
    f = lambda a: np.ascontiguousarray(np.asarray(a, dtype=np.float32))
    x = f(x).reshape(NCORES, T, D)
    p = f(p)[0].reshape(NCORES, T, 256)
    ba, bb = _bias_layouts(rel_bias)
    w_router = np.ascontiguousarray(np.concatenate(
        [f(w_coarse)[0], np.transpose(f(w_fine)[0], (1, 0, 2)).reshape(D, 32)], axis=1))
    tri = (np.arange(128)[:, None] < np.arange(128)[None, :]).astype(np.float32)
    ecrow = np.ascontiguousarray(np.broadcast_to((np.arange(NE, dtype=np.float32) * CAP)[None, :], (128, NE)))
    lam = np.ascontiguousarray(np.stack([f(lambda_q1)[0], f(lambda_k1)[0], f(lambda_q2)[0], f(lambda_k2)[0]], 0))
    shared = {
        "bias_a": ba, "bias_b": bb,
        "norm_mix_g": f(norm_mix_g)[0], "norm_ffn_g": f(norm_ffn_g)[0], "norm_ple_g": f(norm_ple_g)[0],
        "final_norm_g": f(final_norm_g),
        "w_in": f(w_in)[0], "w_gate": f(w_gate)[0], "lam": lam, "subln_g": f(subln_g)[0],
        "w_proj_a": f(w_proj_a)[0], "w_proj_b": f(w_proj_b)[0], "w_out": f(w_out)[0], "w_router": w_router,
        "w1": f(w1)[0], "w3": f(w3)[0], "w2": f(w2)[0], "w_ple_gate": f(w_ple_gate)[0], "w_ple_proj": f(w_ple_proj)[0],
        "identf": np.eye(128, dtype=np.float32), "tri": tri, "ecrow": ecrow,
    }
    in_maps = []
    for c in range(NCORES):
        m = dict(shared)
        m["x"] = np.ascontiguousarray(x[c])
        m["p"] = np.ascontiguousarray(p[c])
        in_maps.append(m)
    return in_maps


def kernel(**inputs):
    global _NC
    in_maps = prep_inputs(**inputs)
    if _NC is None:
        _NC = build_nc()
    res = run_bass_kernel_spmd(_NC, in_maps, core_ids=list(range(NCORES)))
    out = np.stack([np.asarray(r["out"], dtype=np.float32) for r in res.results], 0)
    return out.reshape(16, SEQ, D)
```

```python
import math
from contextlib import ExitStack

import numpy as np
import concourse.bass as bass
import concourse.mybir as mybir
from concourse.bass_utils import run_bass_kernel_spmd

F32 = mybir.dt.float32
BF16 = mybir.dt.bfloat16
I32 = mybir.dt.int32
AF = mybir.ActivationFunctionType
ALU = mybir.AluOpType
AX = mybir.AxisListType

NCORES = 8
D = 2048
SEQ = 2048
T = 2 * SEQ
NT = T // 128
CAP = 512
CCH = 256
NE = 32
NSLOT = NE * CAP
LAM_INIT = 0.2
SC_A = 128 ** -0.5
SC_B = 64 ** -0.5
DIL = ((128, 1), (512, 4), (2048, 16))


class Buf:
    __slots__ = ("name", "writers", "readers")

    def __init__(self, name=""):
        self.name = name
        self.writers = {}
        self.readers = {}


class Rec:
    __slots__ = ("eng", "cnt", "is_dma", "sem")

    def __init__(self, eng, is_dma, sem):
        self.eng = eng
        self.is_dma = is_dma
        self.sem = sem
        self.cnt = None


class TB:
    def __init__(self, h, name=""):
        self.h = h
        self.b = Buf(name)

    def __getitem__(self, k):
        return self.h[k]


class Sch:
    def __init__(self, nc):
        self.nc = nc
        self.E = {"pe": nc.tensor, "act": nc.scalar, "dve": nc.vector, "pool": nc.gpsimd, "sp": nc.sync}
        self.sem = {e: nc.alloc_semaphore("s_" + e) for e in ("pe", "act", "dve", "pool")}
        self.cnt = {e: 0 for e in self.sem}
        self.waited = {e: {} for e in self.E}
        self.dsem = {}
        self.pending = []
        self.nsem = 4
        self.n_ops = 0
        self.rec = None

    def _deps(self, eng, reads, writes):
        need = {}

        def add(r, raw):
            if not r.is_dma:
                if r.eng == eng and (eng == "pe" or not raw):
                    return
            assert r.cnt is not None, "dependency on unsignaled op (%s)" % r.eng
            k = id(r.sem)
            if need.get(k, (None, 0))[1] < r.cnt:
                need[k] = (r.sem, r.cnt)

        for b in reads:
            for r in b.writers.values():
                add(r, True)
        for b in writes:
            if b.readers:
                for r in b.readers.values():
                    add(r, False)
                for r in b.writers.values():
                    add(r, False)
        return need

    def _emit_waits(self, eng, need):
        E = self.E[eng]
        w = self.waited[eng]
        for k, (sem, val) in need.items():
            if w.get(k, 0) < val:
                E.wait_ge(sem, val)
                w[k] = val

    def _update(self, rec, key, reads, writes):
        for b in writes:
            if b.readers:
                b.writers = {key: rec}
                b.readers = {}
            else:
                b.writers[key] = rec
        for b in reads:
            b.readers[key] = rec

    def record(self, f):
        self.rec = []
        f()
        r, self.rec = self.rec, None
        return r

    def replay(self, item):
        kind, args = item
        (self.op if kind == "op" else self.dma)(*args)

    def op(self, eng, fn, reads=(), writes=(), sig=True):
        if self.rec is not None:
            self.rec.append(("op", (eng, fn, list(reads), list(writes), sig)))
            return None
        reads = [x.b if isinstance(x, TB) else x for x in reads]
        writes = [x.b if isinstance(x, TB) else x for x in writes]
        need = self._deps(eng, reads, writes)
        self._emit_waits(eng, need)
        ins = fn(self.E[eng])
        rec = Rec(eng, False, self.sem[eng])
        if sig:
            self.cnt[eng] += 1
            ins.then_inc(self.sem[eng], 1)
            rec.cnt = self.cnt[eng]
            if eng == "pe" and self.pending:
                for p in self.pending:
                    p.cnt = rec.cnt
                self.pending = []
        else:
            assert eng == "pe"
            self.pending.append(rec)
        self._update(rec, eng, reads, writes)
        self.n_ops += 1
        return rec

    def dma(self, q, fn, reads=(), writes=()):
        if self.rec is not None:
            self.rec.append(("dma", (q, fn, list(reads), list(writes))))
            return None
        reads = [x.b if isinstance(x, TB) else x for x in reads]
        writes = [x.b if isinstance(x, TB) else x for x in writes]
        need = self._deps(q, reads, writes)
        self._emit_waits(q, need)
        b0 = writes[0]
        if id(b0) not in self.dsem:
            self.dsem[id(b0)] = [self.nc.alloc_semaphore("d_%d" % self.nsem), 0]
            self.nsem += 1
            assert self.nsem <= 100, "too many DMA semaphores"
        ds = self.dsem[id(b0)]
        ins = fn(self.E[q])
        ds[1] += 16
        ins.then_inc(ds[0], 16)
        rec = Rec(q, True, ds[0])
        rec.cnt = ds[1]
        self._update(rec, ("dma", id(ds[0])), reads, writes)
        self.n_ops += 1
        return rec

    def _all_need(self, skip=None):
        need = {}
        for e in self.sem:
            if e != skip and self.cnt[e] > 0:
                need[id(self.sem[e])] = (self.sem[e], self.cnt[e])
        for s, c in self.dsem.values():
            if c > 0:
                need[id(s)] = (s, c)
        return need

    def barrier(self):
        assert not self.pending
        for f in self.E:
            self._emit_waits(f, self._all_need(skip=f))

    def finish(self):
        assert not self.pending
        self._emit_waits("sp", self._all_need())


def build_nc(cfg=None):
    cfg = cfg or {}
    dbg = cfg.get('dbg', False)
    nc = bass.Bass("TRN2", target_bir_lowering=False)
    S = Sch(nc)

    def din(name, shape, dt=F32):
        return nc.dram_tensor(name, list(shape), dt, kind="ExternalInput").ap()

    def dscr(name, shape, dt):
        return nc.dram_tensor(name, list(shape), dt, kind=("ExternalOutput" if dbg else "Internal")).ap()

    x_d = din("x", [T, D])
    p_d = din("p", [T, 256])
    bias_a_d = din("bias_a", [8, 128, 2048])
    bias_b_d = din("bias_b", [6, 128, 1024])
    g_mix_d = din("norm_mix_g", [D])
    g_ffn_d = din("norm_ffn_g", [D])
    g_ple_d = din("norm_ple_g", [D])
    g_fin_d = din("final_norm_g", [D])
    w_in_d = din("w_in", [D, 10752])
    w_gate_d = din("w_gate", [D, 4096])
    lam_d = din("lam", [4, 128])
    subln_d = din("subln_g", [256])
    w_pa_d = din("w_proj_a", [D, D])
    w_pb_d = din("w_proj_b", [512, D])
    w_out_d = din("w_out", [D, D])
    w_rt_d = din("w_router", [D, 36])
    NEd = cfg.get('nEdecl', NE)
    w1_d = din("w1", [NEd, D, 1024])
    w3_d = din("w3", [NEd, D, 1024])
    w2_d = din("w2", [NEd, 1024, D])
    w_pg_d = din("w_ple_gate", [D, D])
    w_pp_d = din("w_ple_proj", [256, D])
    identf_d = din("identf", [128, 128])
    tri_d = din("tri", [128, 128])
    ecrow_d = din("ecrow", [128, NE])
    out_d = nc.dram_tensor("out", [T, D], F32, kind="ExternalOutput").ap()
    out_b = Buf("out")

    oaT_d = dscr("oaT", [2, 128, 16, SEQ], BF16)
    obT_d = dscr("obT", [2, 128, 4, SEQ], BF16)
    x1_d = dscr("x1", [T, D], F32)
    xin_d = dscr("xin", [NSLOT, D], BF16)
    ybuf_d = dscr("ybuf", [NSLOT + 128, D], F32)
    oaT_b, obT_b, x1_b, xin_b, ybuf_b = Buf("oaT"), Buf("obT"), Buf("x1"), Buf("xin"), Buf("ybuf")

    gstack = ExitStack()
    bc_reg = nc.gpsimd.to_reg(NSLOT - 1)
    bc_reg2 = nc.gpsimd.to_reg(NSLOT + 127)

    uid = [0]

    def mk(stack, name, shape, dt):
        uid[0] += 1
        return TB(stack.enter_context(nc.sbuf_tensor("sb%d_%s" % (uid[0], name), list(shape), dt)), name)

    def mkp(stack, name, shape, dt):
        uid[0] += 1
        return TB(stack.enter_context(nc.psum_tensor("ps%d_%s" % (uid[0], name), list(shape), dt)), name)

    identf = mk(gstack, "identf", [128, 128], F32)
    identb = mk(gstack, "identb", [128, 128], BF16)
    trib = mk(gstack, "trib", [128, 128], BF16)
    onesb = mk(gstack, "onesb", [128, 128], BF16)
    ecrow = mk(gstack, "ecrow", [128, NE], F32)
    neglam = mk(gstack, "neglam", [128, 1], F32)
    gsub = mk(gstack, "gsub", [128, 256], F32)
    slots = mk(gstack, "slots", [128, NT * 2], I32)
    slotg = mk(gstack, "slotg", [128, NT * 2], I32)
    gates = mk(gstack, "gates", [128, NT, 2], F32)
    cbase = mk(gstack, "cbase", [128, NE], F32)

    S.dma("sp", lambda e: e.dma_start(out=identf[:], in_=identf_d), writes=[identf])
    S.dma("sp", lambda e: e.dma_start(out=ecrow[:], in_=ecrow_d), writes=[ecrow])
    S.op("dve", lambda e: e.tensor_copy(out=identb[:], in_=identf[:]), reads=[identf], writes=[identb])
    S.op("dve", lambda e: e.memset(onesb[:], 1.0), writes=[onesb])
    S.op("dve", lambda e: e.memset(cbase[:], 0.0), writes=[cbase])
    with ExitStack() as st:
        trif = mk(st, "trif", [128, 128], F32)
        lamv = mk(st, "lamv", [128, 4, 128], F32)
        lamp = mk(st, "lamp", [128, 2, 128], F32)
        lams = mk(st, "lams", [128, 2], F32)
        lame = mk(st, "lame", [128, 2], F32)
        S.dma("sp", lambda e: e.dma_start(out=trif[:], in_=tri_d), writes=[trif])
        S.op("dve", lambda e: e.tensor_copy(out=trib[:], in_=trif[:]), reads=[trif], writes=[trib])
        for i in range(4):
            S.dma("sp", lambda e, i=i: e.dma_start(out=lamv[:, i, :], in_=lam_d[i].partition_broadcast(128)), writes=[lamv])
        S.dma("sp", lambda e: e.dma_start(out=gsub[:], in_=subln_d.partition_broadcast(128)), writes=[gsub])
        S.op("dve", lambda e: e.tensor_tensor(out=lamp[:, 0, :], in0=lamv[:, 0, :], in1=lamv[:, 1, :], op=ALU.mult), reads=[lamv], writes=[lamp])
        S.op("dve", lambda e: e.tensor_tensor(out=lamp[:, 1, :], in0=lamv[:, 2, :], in1=lamv[:, 3, :], op=ALU.mult), reads=[lamv], writes=[lamp])
        S.op("dve", lambda e: e.reduce_sum(out=lams[:, 0:1], in_=lamp[:, 0, :], axis=AX.X), reads=[lamp], writes=[lams])
        S.op("dve", lambda e: e.reduce_sum(out=lams[:, 1:2], in_=lamp[:, 1, :], axis=AX.X), reads=[lamp], writes=[lams])
        S.op("act", lambda e: e.activation(out=lame[:], in_=lams[:], func=AF.Exp), reads=[lams], writes=[lame])
        S.op("dve", lambda e: e.tensor_tensor(out=neglam[:], in0=lame[:, 1:2], in1=lame[:, 0:1], op=ALU.subtract), reads=[lame], writes=[neglam])
        S.op("dve", lambda e: e.tensor_scalar(out=neglam[:], in0=neglam[:], scalar1=-LAM_INIT, scalar2=None, op0=ALU.add), reads=[neglam], writes=[neglam])
        S.op("dve", lambda e: e.tensor_scalar(out=gsub[:], in0=gsub[:], scalar1=1.0 - LAM_INIT, scalar2=None, op0=ALU.mult), reads=[gsub], writes=[gsub])
        S.barrier()

    def wload(dst, dst_ap_fn, src_ap, kchunks, nsplit, reads=()):
        step = kchunks // nsplit
        for i in range(nsplit):
            S.dma("pool", lambda e, i=i: e.dma_start(out=dst_ap_fn(i * step, (i + 1) * step), in_=src_ap[:, i * step:(i + 1) * step, :]),
                  reads=list(reads), writes=[dst])

    def rstd_ops(ss, t1, rstd, inv_n, eps):
        S.op("dve", lambda e: e.tensor_scalar(out=t1[:], in0=ss[:], scalar1=inv_n, scalar2=eps, op0=ALU.mult, op1=ALU.add), reads=[ss], writes=[t1])
        S.op("act", lambda e: e.activation(out=t1[:], in_=t1[:], func=AF.Ln), reads=[t1], writes=[t1])
        S.op("act", lambda e: e.activation(out=rstd[:], in_=t1[:], func=AF.Exp, scale=-0.5), reads=[t1], writes=[rstd])

    evac_flip = [0]

    def evac(out_ap, in_ap, reads, writes):
        evac_flip[0] ^= 1
        if evac_flip[0]:
            S.op("act", lambda e: e.activation(out=out_ap, in_=in_ap, func=AF.Copy), reads=reads, writes=writes)
        else:
            S.op("dve", lambda e: e.tensor_copy(out=out_ap, in_=in_ap), reads=reads, writes=writes)

    w_in_v = w_in_d.rearrange("(k p) c -> p k c", p=128)

    for s in range(cfg.get('nseq', 2)):
        hstack = ExitStack()
        hT = mk(hstack, "hT", [128, 16, SEQ], BF16)
        with ExitStack() as st:
            stage = [mk(st, "stage%d" % i, [128, D], F32) for i in range(2)]
            junk = mk(st, "junk", [128, D], BF16)
            xs = mk(st, "xs", [128, D], BF16)
            gb = mk(st, "gb", [128, D], F32)
            ss = mk(st, "ss", [128, 1], F32)
            t1 = mk(st, "t1", [128, 1], F32)
            rstd = mk(st, "rstd", [128, 1], F32)
            Wq = mk(st, "Wq", [128, 16, 256], BF16)
            Wk = mk(st, "Wk", [128, 16, 256], BF16)
            Wv = mk(st, "Wv", [128, 16, 256], BF16)
            QT = mk(st, "QT", [128, 2, SEQ], BF16)
            KT = mk(st, "KT", [128, 2, SEQ], BF16)
            V = mk(st, "V", [128, 16, 264], BF16)
            EA = mk(st, "EA", [128, 2048], BF16)
            EB = EA
            PT = [mk(st, "PT%d" % i, [128, 2, 256], BF16) for i in range(3)]
            PTB = [mk(st, "PTB%d" % i, [128, 512], BF16) for i in range(3)]
            o1 = [mk(st, "o1_%d" % i, [128, 256], F32) for i in range(2)]
            oo = [mk(st, "oo_%d" % i, [128, 256], F32) for i in range(2)]
            junk2 = mk(st, "junk2", [128, 256], BF16)
            oab = [mk(st, "oab%d" % i, [128, 256], BF16) for i in range(2)]
            oaTu = mk(st, "oaTu", [128, 2, 512], BF16)
            rr = [mk(st, "rr%d" % i, [128, 2], F32) for i in range(2)]
            nl = [mk(st, "nl%d" % i, [128, 1], F32) for i in range(2)]
            ss2 = mk(st, "ss2", [128, 1], F32)
            t2 = mk(st, "t2", [128, 1], F32)
            rs2 = mk(st, "rs2", [128, 1], F32)
            accUD = mk(st, "accUD", [128, 2, 2, SEQ], F32)
            obTq = mk(st, "obTq", [128, 2, 512], BF16)
            acc = [mkp(st, "acc%d" % i, [128, 512], F32) for i in range(4)]
            pj = [mkp(st, "pj%d" % i, [128, 512], F32) for i in range(3)]
            ptb = [mkp(st, "ptb%d" % i, [128, 1024], BF16) for i in range(1)]

            S.dma("sp", lambda e: e.dma_start(out=gb[:], in_=g_mix_d.partition_broadcast(128)), writes=[gb])
            S.op("dve", lambda e: e.memset(V[:, :, 256:264], 1.0), writes=[V])

            for tt in range(16):
                r0 = s * SEQ + tt * 128
                stg = stage[tt % 2]
                S.dma("sp", lambda e, stg=stg, r0=r0: e.dma_start(out=stg[:], in_=x_d[r0:r0 + 128, :]), writes=[stg])
                S.op("act", lambda e, stg=stg: e.activation(out=junk[:], in_=stg[:], func=AF.Square, accum_out=ss[:]), reads=[stg], writes=[junk, ss])
                rstd_ops(ss, t1, rstd, 1.0 / D, 1e-6)
                S.op("dve", lambda e, stg=stg: e.scalar_tensor_tensor(out=xs[:], in0=stg[:], scalar=rstd[:], in1=gb[:], op0=ALU.mult, op1=ALU.mult),
                     reads=[stg, rstd, gb], writes=[xs])
                for j in range(2):
                    for k in range(8 * j, 8 * j + 8):
                        S.op("pe", lambda e, k=k: e.transpose(ptb[0][:, (k % 8) * 128:(k % 8 + 1) * 128], xs[:, k * 128:(k + 1) * 128], identb[:]),
                             reads=[xs, identb], writes=[ptb[0]], sig=(k % 8 == 7))
                    evac(hT[:, 8 * j:8 * j + 8, tt * 128:(tt + 1) * 128], ptb[0][:].rearrange("p (k t) -> p k t", k=8), [ptb[0]], [hT])

            units = [("A", h) for h in range(8)] + [("B", q * 3 + g) for q in range(2) for g in range(3)]
            if 'units' in cfg:
                units = cfg['units']
            pjn = [0]

            def next_pj():
                pjn[0] = (pjn[0] + 1) % 3
                return pj[pjn[0]]

            for kind, ui in units:
                if kind == "A":
                    h = ui
                    qc, kc, vc = h * 256, 2048 + h * 256, 4096 + h * 256
                    d, nblk = 1, 16
                else:
                    quad, g = divmod(ui, 3)
                    d = DIL[g][1]
                    nblk = (SEQ // d) // 128
                    qc = 6144 + g * 512 + quad * 256
                    kc = 7680 + g * 512 + quad * 256
                    vc = 9216 + g * 512 + quad * 256
                L = SEQ // d
                for Wt, c0 in ((Wq, qc), (Wk, kc), (Wv, vc)):
                    wload(Wt, lambda a, b, Wt=Wt: Wt[:, a:b, :], w_in_v[:, :, c0:c0 + 256], 16, 4)
                if kind == "A":
                    stg = stage[0]
                    S.dma("sp", lambda e, stg=stg, h=h: e.dma_start(out=stg[:], in_=bias_a_d[h]), writes=[stg])
                    S.op("act", lambda e, stg=stg: e.activation(out=EA[:], in_=stg[:], func=AF.Copy, scale=1.0 / SC_A), reads=[stg], writes=[EA])
                elif not cfg.get('Bnobias'):
                    stg = stage[1]
                    S.dma("sp", lambda e, stg=stg, ui=ui: e.dma_start(out=stg[:, 0:1024], in_=bias_b_d[ui]), writes=[stg])
                    if not cfg.get('Bnoexp'):
                        S.op("act", lambda e, stg=stg: e.activation(out=EB[:, 0:1024], in_=stg[:, 0:1024], func=AF.Copy, scale=1.0 / SC_B), reads=[stg], writes=[EB])
                def proj_half(dst, Wt, half, bsplit=False):
                    for tg in range(4):
                        bank = next_pj()
                        for k in range(16):
                            S.op("pe", lambda e, bank=bank, Wt=Wt, k=k, half=half, tg=tg: e.matmul(
                                bank[:, 0:512], lhsT=Wt[:, k, half * 128:(half + 1) * 128], rhs=hT[:, k, tg * 512:(tg + 1) * 512],
                                start=(k == 0), stop=(k == 15)), reads=[Wt, hT], writes=[bank], sig=(k == 15))
                        l0 = tg * 512 // d
                        if not bsplit:
                            if d == 1:
                                evac(dst[:, half, tg * 512:(tg + 1) * 512], bank[:, 0:512], [bank], [dst])
                            else:
                                oap = dst[:, half, :].rearrange("p (c l) -> p c l", c=d)[:, :, l0:l0 + 512 // d]
                                iap = bank[:, 0:512].rearrange("p (l c) -> p c l", c=d)
                                evac(oap, iap, [bank], [dst])
                        else:
                            for hh in range(2):
                                p0 = hh * 64
                                if d == 1:
                                    evac(dst[p0:p0 + 64, hh, tg * 512:(tg + 1) * 512], bank[p0:p0 + 64, 0:512], [bank], [dst])
                                else:
                                    oap = dst[p0:p0 + 64, hh, :].rearrange("p (c l) -> p c l", c=d)[:, :, l0:l0 + 512 // d]
                                    iap = bank[p0:p0 + 64, 0:512].rearrange("p (l c) -> p c l", c=d)
                                    evac(oap, iap, [bank], [dst])

                for half in range(2):
                    proj_half(QT, Wq, half)
                if kind == "A":
                    for half in range(2):
                        proj_half(KT, Wk, half)
                else:
                    S.op("dve", lambda e: e.memset(KT[64:128, 0, :], 0.0), writes=[KT])
                    S.op("dve", lambda e: e.memset(KT[0:64, 1, :], 0.0), writes=[KT])
                for sbk in range(16):
                    c_, n_ = divmod(sbk, nblk)
                    t0 = n_ * 128 * d + c_
                    bank = next_pj()
                    for k in range(16):
                        S.op("pe", lambda e, bank=bank, k=k, t0=t0, d=d: e.matmul(
                            bank[:, 0:256], lhsT=hT[:, k, t0:t0 + 127 * d + 1:d], rhs=Wv[:, k, :],
                            start=(k == 0), stop=(k == 15)), reads=[Wv, hT], writes=[bank], sig=(k == 15))
                    evac(V[:, sbk, 0:256], bank[:, 0:256], [bank], [V])

                if kind == "A":
                    steps = [(c, kb) for c in range(8) for kb in range(2 * c + 2)]

                    def qk_stage(i):
                        c, kb = steps[i]
                        qlo = 2 * c if kb <= 2 * c else 2 * c + 1
                        off = (qlo - 2 * c) * 128
                        bank = pj[i % 3]
                        pt = PT[i % 3]
                        e0 = (qlo - kb) * 128
                        e1 = (2 * c + 2 - kb) * 128
                        for m in range(2):
                            S.op("pe", lambda e, bank=bank, m=m, kb=kb, qlo=qlo, c=c, off=off: e.matmul(
                                bank[:, m * 256 + off:m * 256 + 256], lhsT=KT[:, m, kb * 128:(kb + 1) * 128],
                                rhs=QT[:, m, qlo * 128:(2 * c + 2) * 128], start=True, stop=False),
                                reads=[KT, QT], writes=[bank], sig=False)
                            S.op("pe", lambda e, bank=bank, m=m, off=off, e0=e0, e1=e1: e.matmul(
                                bank[:, m * 256 + off:m * 256 + 256], lhsT=identb[:], rhs=EA[:, e0:e1], start=False, stop=True),
                                reads=[identb, EA], writes=[bank], sig=(m == 1))
                        S.op("act", lambda e, bank=bank, pt=pt, off=off: e.activation(
                            out=pt[:, :, off:256], in_=bank[:, 0:512].rearrange("p (m q) -> p m q", m=2)[:, :, off:256],
                            func=AF.Exp, scale=SC_A), reads=[bank], writes=[pt])

                    def pv_stage(i):
                        c, kb = steps[i]
                        qlo = 2 * c if kb <= 2 * c else 2 * c + 1
                        pt = PT[i % 3]
                        mm = [(m, qb) for m in range(2) for qb in range(qlo, 2 * c + 2)]
                        for ii, (m, qb) in enumerate(mm):
                            j = qb - 2 * c
                            a_ = acc[m * 2 + j]
                            S.op("pe", lambda e, a_=a_, pt=pt, m=m, j=j, kb=kb, qb=qb: e.matmul(
                                a_[:, 0:264], lhsT=pt[:, m, j * 128:(j + 1) * 128], rhs=V[:, kb, 0:264],
                                start=(kb == 0), stop=(kb == qb)), reads=[pt, V], writes=[a_], sig=(ii == len(mm) - 1))

                    def epi1(c):
                        for j in range(2):
                            a0, a1 = acc[j], acc[2 + j]
                            rr_, nl_, o1_, oo_ = rr[j], nl[j], o1[j], oo[j]
                            S.op("dve", lambda e, a0=a0, rr_=rr_: e.reciprocal(out=rr_[:, 0:1], in_=a0[:, 256:257]), reads=[a0], writes=[rr_])
                            S.op("dve", lambda e, a1=a1, rr_=rr_: e.reciprocal(out=rr_[:, 1:2], in_=a1[:, 256:257]), reads=[a1], writes=[rr_])
                            S.op("dve", lambda e, rr_=rr_, nl_=nl_: e.tensor_tensor(out=nl_[:], in0=rr_[:, 1:2], in1=neglam[:], op=ALU.mult), reads=[rr_, neglam], writes=[nl_])
                            S.op("act", lambda e, a0=a0, rr_=rr_, o1_=o1_: e.activation(out=o1_[:], in_=a0[:, 0:256], func=AF.Copy, scale=rr_[:, 0:1]), reads=[a0, rr_], writes=[o1_])
                            S.op("dve", lambda e, a1=a1, nl_=nl_, o1_=o1_, oo_=oo_: e.scalar_tensor_tensor(out=oo_[:], in0=a1[:, 0:256], scalar=nl_[:], in1=o1_[:], op0=ALU.mult, op1=ALU.add),
                                 reads=[a1, nl_, o1_], writes=[oo_])
                        for j in range(2):
                            oo_ = oo[j]
                            ob_ = oab[j]
                            S.op("act", lambda e, oo_=oo_: e.activation(out=junk2[:], in_=oo_[:], func=AF.Square, accum_out=ss2[:]), reads=[oo_], writes=[junk2, ss2])
                            rstd_ops(ss2, t2, rs2, 1.0 / 256, 1e-5)
                            S.op("dve", lambda e, ob_=ob_, oo_=oo_: e.scalar_tensor_tensor(out=ob_[:], in0=oo_[:], scalar=rs2[:], in1=gsub[:], op0=ALU.mult, op1=ALU.mult),
                                 reads=[oo_, rs2, gsub], writes=[ob_])

                    def epi2(c):
                        for j in range(2):
                            qb = 2 * c + j
                            ob_ = oab[j]
                            pb_ = ptb[0]
                            for hh in range(2):
                                S.op("pe", lambda e, pb_=pb_, hh=hh, ob_=ob_: e.transpose(pb_[:, hh * 128:(hh + 1) * 128], ob_[:, hh * 128:(hh + 1) * 128], identb[:]),
                                     reads=[ob_, identb], writes=[pb_], sig=(hh == 1))
                            ql = qb % 4
                            evac(oaTu[:, :, ql * 128:(ql + 1) * 128], pb_[:, 0:256].rearrange("p (k t) -> p k t", k=2), [pb_], [oaTu])
                            if ql == 3:
                                q0 = (qb - 3) * 128
                                S.dma("sp", lambda e, h=h, q0=q0: e.dma_start(out=oaT_d[s, :, 2 * h:2 * h + 2, q0:q0 + 512], in_=oaTu[:]),
                                      reads=[oaTu], writes=[oaT_b])

                    pend_epi2 = None
                    qk_stage(0)
                    qk_stage(1)
                    for i, (c, kb) in enumerate(steps):
                        if i + 2 < len(steps):
                            qk_stage(i + 2)
                        pv_stage(i)
                        if kb == 1 and pend_epi2 is not None:
                            epi2(pend_epi2)
                            pend_epi2 = None
                        if kb == 2 * c + 1:
                            epi1(c)
                            pend_epi2 = c
                    epi2(pend_epi2)
                else:
                    it = 0
                    EBv = EB[:, 0:1024].rearrange("p (h w q) -> p h w q", h=4, w=2)
                    for pair in range(2):
                        proj_half(KT, Wk, pair, bsplit=True)
                        def b_qk(sbk, it_):
                            c_, n_ = divmod(sbk, nblk)
                            hp = n_ > 0
                            bank = pj[it_ % 3]
                            pt = PTB[it_ % 3]
                            nw = 2 if hp else 1
                            i = 0
                            for hh in range(2):
                                for wh in range(nw):
                                    i += 1
                                    S.op("pe", lambda e, bank=bank, hh=hh, wh=wh, pair=pair, sbk=sbk: e.matmul(
                                        bank[:, (hh * 2 + wh) * 128:(hh * 2 + wh + 1) * 128],
                                        lhsT=KT[:, hh, (sbk - wh) * 128:(sbk - wh + 1) * 128],
                                        rhs=QT[:, pair, sbk * 128:(sbk + 1) * 128], start=True, stop=False),
                                        reads=[KT, QT], writes=[bank], sig=False)
                                    S.op("pe", lambda e, bank=bank, hh=hh, wh=wh, pair=pair: e.matmul(
                                        bank[:, (hh * 2 + wh) * 128:(hh * 2 + wh + 1) * 128],
                                        lhsT=identb[:], rhs=EBv[:, pair * 2 + hh, wh, :], start=False, stop=True),
                                        reads=[identb, EB], writes=[bank], sig=(i == 2 * nw))
                            ptv = pt[:].rearrange("p (h w q) -> p h w q", h=2, w=2)
                            bkv = bank[:, 0:512].rearrange("p (h w q) -> p h w q", h=2, w=2)
                            if hp:
                                S.op("act", lambda e, bank=bank, pt=pt: e.activation(out=pt[:], in_=bank[:, 0:512], func=AF.Exp, scale=SC_B),
                                     reads=[bank], writes=[pt])
                            else:
                                S.op("act", lambda e, ptv=ptv, bkv=bkv: e.activation(out=ptv[:, :, 0, :], in_=bkv[:, :, 0, :], func=AF.Exp, scale=SC_B),
                                     reads=[bank], writes=[pt])

                        def b_pv(sbk, it_):
                            c_, n_ = divmod(sbk, nblk)
                            hp = n_ > 0
                            pt = PTB[it_ % 3]
                            ut = acc[it_ % 4]
                            nw = 2 if hp else 1
                            ptv = pt[:].rearrange("p (h w q) -> p h w q", h=2, w=2)
                            for hh in range(2):
                                for wh in range(nw):
                                    kblk = sbk - wh
                                    S.op("pe", lambda e, ut=ut, kblk=kblk, pair=pair, ptv=ptv, hh=hh, wh=wh, nw=nw: e.matmul(
                                        ut[:, hh * 128:(hh + 1) * 128], lhsT=V[:, kblk, pair * 128:(pair + 1) * 128], rhs=ptv[:, hh, wh, :],
                                        start=(wh == 0), stop=(wh == nw - 1)), reads=[V, pt], writes=[ut], sig=False)
                            for hh in range(2):
                                for wh in range(nw):
                                    S.op("pe", lambda e, ut=ut, ptv=ptv, hh=hh, wh=wh, nw=nw: e.matmul(
                                        ut[:, (2 + hh) * 128:(3 + hh) * 128], lhsT=onesb[:], rhs=ptv[:, hh, wh, :],
                                        start=(wh == 0), stop=(wh == nw - 1)), reads=[onesb, pt], writes=[ut], sig=(hh == 1 and wh == nw - 1))
                            t0 = n_ * 128 * d + c_
                            for hh in range(2):
                                p0 = hh * 64
                                oap = accUD[p0:p0 + 64, pair, :, t0:t0 + 127 * d + 1:d]
                                iap = ut[p0:p0 + 64, 0:512].rearrange("p (u h q) -> p u h q", u=2, h=2)[:, :, hh, :]
                                if g == 0:
                                    S.op("dve", lambda e, oap=oap, iap=iap: e.tensor_copy(out=oap, in_=iap), reads=[ut], writes=[accUD])
                                else:
                                    S.op("dve", lambda e, oap=oap, iap=iap: e.tensor_tensor(out=oap, in0=oap, in1=iap, op=ALU.add), reads=[ut], writes=[accUD])

                        b_qk(0, it)
                        b_qk(1, it + 1)
                        for sbk in range(16):
                            if sbk + 2 < 16:
                                b_qk(sbk + 2, it + 2)
                            b_pv(sbk, it)
                            it += 1
                    if g == 2:
                        for pair in range(2):
                            S.op("dve", lambda e, pair=pair: e.reciprocal(out=accUD[:, pair, 1, :], in_=accUD[:, pair, 1, :]), reads=[accUD], writes=[accUD])
                        for tq in range(4):
                            for pair in range(2):
                                S.op("dve", lambda e, pair=pair, tq=tq: e.tensor_tensor(
                                    out=obTq[:, pair, :], in0=accUD[:, pair, 0, tq * 512:(tq + 1) * 512],
                                    in1=accUD[:, pair, 1, tq * 512:(tq + 1) * 512], op=ALU.mult), reads=[accUD], writes=[obTq])
                            S.dma("sp", lambda e, quad=quad, tq=tq: e.dma_start(out=obT_d[s, :, quad * 2:quad * 2 + 2, tq * 512:(tq + 1) * 512], in_=obTq[:]),
                                  reads=[obTq], writes=[obT_b])
            S.barrier()

        with ExitStack() as st:
          if cfg.get('C', True):
              oaTh = mk(st, "oaTh", [128, 16, 1024], BF16)
              obTh = mk(st, "obTh", [128, 4, 1024], BF16)
              mT = mk(st, "mT", [128, 16, 1024], BF16)
              Wg1 = [mk(st, "Wg1_%d" % i, [128, 16, 128], BF16) for i in range(2)]
              Wg2 = [mk(st, "Wg2_%d" % i, [128, 16, 128], BF16) for i in range(2)]
              Wa = [mk(st, "Wa_%d" % i, [128, 16, 128], BF16) for i in range(2)]
              Wb = [mk(st, "Wb_%d" % i, [128, 4, 128], BF16) for i in range(2)]
              Wo = mk(st, "Wo", [128, 16, 512], BF16)
              xt = [mk(st, "xt%d" % i, [128, 512], F32) for i in range(2)]
              x1t = [mk(st, "x1t%d" % i, [128, 512], F32) for i in range(2)]
              s1 = mk(st, "s1", [128, 512], F32)
              s2 = mk(st, "s2", [128, 512], F32)
              pG1 = mkp(st, "pG1", [128, 512], F32)
              pG2 = mkp(st, "pG2", [128, 512], F32)
              pPA = mkp(st, "pPA", [128, 512], F32)
              pPB = mkp(st, "pPB", [128, 512], F32)
              pO = [mkp(st, "pO%d" % i, [128, 512], F32) for i in range(2)]
              wg_v = w_gate_d.rearrange("(k p) c -> p k c", p=128)
              wa_v = w_pa_d.rearrange("(k p) c -> p k c", p=128)
              wb_v = w_pb_d.rearrange("(k p) c -> p k c", p=128)
              wo_v = w_out_d.rearrange("(k p) c -> p k c", p=128)
              for hf in range(2):
                  t0 = hf * 1024
                  for kk in range(4):
                      S.dma("sp", lambda e, kk=kk, t0=t0: e.dma_start(out=oaTh[:, kk * 4:(kk + 1) * 4, :], in_=oaT_d[s, :, kk * 4:(kk + 1) * 4, t0:t0 + 1024]),
                            reads=[oaT_b], writes=[oaTh])
                  S.dma("sp", lambda e, t0=t0: e.dma_start(out=obTh[:], in_=obT_d[s, :, :, t0:t0 + 1024]), reads=[obT_b], writes=[obTh])
                  for c in range(16):
                      w1_, w2_, wa_, wb_ = Wg1[c % 2], Wg2[c % 2], Wa[c % 2], Wb[c % 2]
                      wload(w1_, lambda a, b, w=w1_: w[:, a:b, :], wg_v[:, :, c * 128:(c + 1) * 128], 16, 2)
                      wload(w2_, lambda a, b, w=w2_: w[:, a:b, :], wg_v[:, :, 2048 + c * 128:2048 + (c + 1) * 128], 16, 2)
                      wload(wa_, lambda a, b, w=wa_: w[:, a:b, :], wa_v[:, :, c * 128:(c + 1) * 128], 16, 2)
                      wload(wb_, lambda a, b, w=wb_: w[:, a:b, :], wb_v[:, :, c * 128:(c + 1) * 128], 4, 1)
                      for tg in range(2):
                          ta = t0 + tg * 512
                          for bank, Wt, src, off_, nk in ((pG1, w1_, hT, ta, 16), (pG2, w2_, hT, ta, 16), (pPA, wa_, oaTh, tg * 512, 16), (pPB, wb_, obTh, tg * 512, 4)):
                              for k in range(nk):
                                  S.op("pe", lambda e, bank=bank, Wt=Wt, src=src, off_=off_, k=k, nk=nk: e.matmul(
                                      bank[:, 0:512], lhsT=Wt[:, k, :], rhs=src[:, k, off_:off_ + 512], start=(k == 0), stop=(k == nk - 1)),
                                      reads=[Wt, src], writes=[bank], sig=(k == nk - 1))
                          S.op("act", lambda e: e.activation(out=s1[:], in_=pG1[:, 0:512], func=AF.Sigmoid), reads=[pG1], writes=[s1])
                          S.op("act", lambda e: e.activation(out=s2[:], in_=pG2[:, 0:512], func=AF.Sigmoid), reads=[pG2], writes=[s2])
                          S.op("dve", lambda e: e.tensor_tensor(out=s1[:], in0=s1[:], in1=pPA[:, 0:512], op=ALU.mult), reads=[s1, pPA], writes=[s1])
                          S.op("dve", lambda e: e.tensor_tensor(out=s2[:], in0=s2[:], in1=pPB[:, 0:512], op=ALU.mult), reads=[s2, pPB], writes=[s2])
                          S.op("dve", lambda e, c=c, tg=tg: e.tensor_tensor(out=mT[:, c, tg * 512:(tg + 1) * 512], in0=s1[:], in1=s2[:], op=ALU.add),
                               reads=[s1, s2], writes=[mT])
                  n = 0
                  for cg in range(4):
                      wload(Wo, lambda a, b: Wo[:, a:b, :], wo_v[:, :, cg * 512:(cg + 1) * 512], 16, 4)
                      for tt in range(8):
                          r0 = s * SEQ + t0 + tt * 128
                          bank, xt_, x1_ = pO[n % 2], xt[n % 2], x1t[n % 2]
                          n += 1
                          S.dma("sp", lambda e, xt_=xt_, r0=r0, cg=cg: e.dma_start(out=xt_[:], in_=x_d[r0:r0 + 128, cg * 512:(cg + 1) * 512]), writes=[xt_])
                          for k in range(16):
                              S.op("pe", lambda e, bank=bank, k=k, tt=tt: e.matmul(bank[:, 0:512], lhsT=mT[:, k, tt * 128:(tt + 1) * 128], rhs=Wo[:, k, :],
                                                                                    start=(k == 0), stop=(k == 15)), reads=[mT, Wo], writes=[bank], sig=(k == 15))
                          S.op("dve", lambda e, bank=bank, xt_=xt_, x1_=x1_: e.tensor_tensor(out=x1_[:], in0=bank[:, 0:512], in1=xt_[:], op=ALU.add),
                               reads=[bank, xt_], writes=[x1_])
                          S.dma("sp", lambda e, x1_=x1_, r0=r0, cg=cg: e.dma_start(out=x1_d[r0:r0 + 128, cg * 512:(cg + 1) * 512], in_=x1_[:]),
                                reads=[x1_], writes=[x1_b])
              S.barrier()
        hstack.close()

    with ExitStack() as st:
        stage = [mk(st, "rstage%d" % i, [128, D], F32) for i in range(2)]
        junk = mk(st, "rjunk", [128, D], BF16)
        gb = mk(st, "rgb", [128, D], F32)
        h2 = [mk(st, "h2_%d" % i, [128, D], F32) for i in range(2)]
        h2b = [mk(st, "h2b%d" % i, [128, D], BF16) for i in range(2)]
        h2T = [mk(st, "h2T%d" % i, [128, 16, 128], F32) for i in range(2)]
        Wr = mk(st, "Wr", [128, 16, 36], F32)
        ssr = [mk(st, "rss%d" % i, [128, 1], F32) for i in range(2)]
        t1r = [mk(st, "rt1%d" % i, [128, 1], F32) for i in range(2)]
        rstdr = [mk(st, "rrstd%d" % i, [128, 1], F32) for i in range(2)]
        lgs = [mk(st, "lg%d" % i, [128, 36], F32) for i in range(2)]
        sms = [{n: mk(st, "r%d_%s" % (i, n), [128, w], F32) for n, w in (
            ("cmax", 1), ("ncm", 1), ("ohg", 4), ("ce", 4), ("csum", 1), ("pg", 1), ("fsel", 8), ("v1", 1), ("oh1", 8), ("fm", 8),
            ("v2", 1), ("oh2", 8), ("dl", 1), ("ed", 1), ("den", 1), ("rden", 1), ("A1", 32), ("A2", 32), ("As", 32), ("pos", 32),
            ("ovf", 32), ("sl", 32), ("tmp", 32), ("sf", 2))} for i in range(2)]
        Asbs = [mk(st, "Asb%d" % i, [128, 32], BF16) for i in range(2)]
        ptf = [mkp(st, "ptf%d" % i, [128, 512], F32) for i in range(4)]
        plgs = [mkp(st, "plg%d" % i, [128, 512], F32) for i in range(2)]
        pcns = [mkp(st, "pcn%d" % i, [128, 512], F32) for i in range(2)]
        S.dma("sp", lambda e: e.dma_start(out=gb[:], in_=g_ffn_d.partition_broadcast(128)), writes=[gb])
        S.dma("sp", lambda e: e.dma_start(out=Wr[:], in_=w_rt_d.rearrange("(k p) c -> p k c", p=128)), writes=[Wr])

        def dv(fn, reads, writes):
            S.op("dve", fn, reads=reads, writes=writes)

        def r_front(i):
            stg = stage[i % 2]
            hb = h2b[i % 2]
            h2_ = h2[i % 2]
            h2T_ = h2T[i % 2]
            ss, t1, rstd = ssr[i % 2], t1r[i % 2], rstdr[i % 2]
            plg = plgs[i % 2]
            S.dma("sp", lambda e, stg=stg, i=i: e.dma_start(out=stg[:], in_=x1_d[i * 128:(i + 1) * 128, :]), reads=[x1_b], writes=[stg])
            S.op("act", lambda e, stg=stg: e.activation(out=junk[:], in_=stg[:], func=AF.Square, accum_out=ss[:]), reads=[stg], writes=[junk, ss])
            rstd_ops(ss, t1, rstd, 1.0 / D, 1e-6)
            dv(lambda e, stg=stg: e.scalar_tensor_tensor(out=h2_[:], in0=stg[:], scalar=rstd[:], in1=gb[:], op0=ALU.mult, op1=ALU.mult), [stg, rstd, gb], [h2_])
            S.op("act", lambda e, hb=hb: e.activation(out=hb[:], in_=h2_[:], func=AF.Copy), reads=[h2_], writes=[hb])
            for k in range(16):
                S.op("pe", lambda e, k=k: e.transpose(ptf[k // 4][:, (k % 4) * 128:(k % 4 + 1) * 128], h2_[:, k * 128:(k + 1) * 128], identf[:]),
                     reads=[h2_, identf], writes=[ptf[k // 4]], sig=(k % 4 == 3))
            for j in range(4):
                evac(h2T_[:, 4 * j:4 * j + 4, :], ptf[j][:].rearrange("p (k t) -> p k t", k=4), [ptf[j]], [h2T_])
            for k in range(16):
                S.op("pe", lambda e, k=k: e.matmul(plg[:, 0:36], lhsT=h2T_[:, k, :], rhs=Wr[:, k, :], start=(k == 0), stop=(k == 15)),
                     reads=[h2T_, Wr], writes=[plg], sig=(k == 15))

        def r_back(i):
            hb = h2b[i % 2]
            m = sms[i % 2]
            lg = lgs[i % 2]
            Asb = Asbs[i % 2]
            plg = plgs[i % 2]
            pcn = pcns[i % 2]
            dv(lambda e: e.tensor_copy(out=lg[:], in_=plg[:, 0:36]), [plg], [lg])
            fine = lg[:, 4:36].rearrange("p (g j) -> p g j", g=4)
            dv(lambda e: e.reduce_max(out=m["cmax"][:], in_=lg[:, 0:4], axis=AX.X), [lg], [m["cmax"]])
            dv(lambda e: e.tensor_scalar(out=m["ohg"][:], in0=lg[:, 0:4], scalar1=m["cmax"][:], scalar2=None, op0=ALU.is_equal), [lg, m["cmax"]], [m["ohg"]])
            dv(lambda e: e.tensor_scalar(out=m["ncm"][:], in0=m["cmax"][:], scalar1=-1.0, scalar2=None, op0=ALU.mult), [m["cmax"]], [m["ncm"]])
            S.op("act", lambda e: e.activation(out=m["ce"][:], in_=lg[:, 0:4], func=AF.Exp, bias=m["ncm"][:], accum_out=m["csum"][:]),
                 reads=[lg, m["ncm"]], writes=[m["ce"], m["csum"]])
            dv(lambda e: e.reciprocal(out=m["pg"][:], in_=m["csum"][:]), [m["csum"]], [m["pg"]])
            dv(lambda e: e.tensor_scalar(out=m["fsel"][:], in0=fine[:, 0, :], scalar1=m["ohg"][:, 0:1], scalar2=None, op0=ALU.mult), [lg, m["ohg"]], [m["fsel"]])
            for g in range(1, 4):
                dv(lambda e, g=g: e.scalar_tensor_tensor(out=m["fsel"][:], in0=fine[:, g, :], scalar=m["ohg"][:, g:g + 1], in1=m["fsel"][:], op0=ALU.mult, op1=ALU.add),
                   [lg, m["ohg"], m["fsel"]], [m["fsel"]])
            dv(lambda e: e.reduce_max(out=m["v1"][:], in_=m["fsel"][:], axis=AX.X), [m["fsel"]], [m["v1"]])
            dv(lambda e: e.tensor_scalar(out=m["oh1"][:], in0=m["fsel"][:], scalar1=m["v1"][:], scalar2=None, op0=ALU.is_equal), [m["fsel"], m["v1"]], [m["oh1"]])
            dv(lambda e: e.scalar_tensor_tensor(out=m["fm"][:], in0=m["oh1"][:], scalar=-1e30, in1=m["fsel"][:], op0=ALU.mult, op1=ALU.add),
               [m["oh1"], m["fsel"]], [m["fm"]])
            dv(lambda e: e.reduce_max(out=m["v2"][:], in_=m["fm"][:], axis=AX.X), [m["fm"]], [m["v2"]])
            dv(lambda e: e.tensor_scalar(out=m["oh2"][:], in0=m["fm"][:], scalar1=m["v2"][:], scalar2=None, op0=ALU.is_equal), [m["fm"], m["v2"]], [m["oh2"]])
            dv(lambda e: e.tensor_tensor(out=m["dl"][:], in0=m["v2"][:], in1=m["v1"][:], op=ALU.subtract), [m["v1"], m["v2"]], [m["dl"]])
            S.op("act", lambda e: e.activation(out=m["ed"][:], in_=m["dl"][:], func=AF.Exp), reads=[m["dl"]], writes=[m["ed"]])
            dv(lambda e: e.tensor_scalar(out=m["den"][:], in0=m["ed"][:], scalar1=1.0, scalar2=None, op0=ALU.add), [m["ed"]], [m["den"]])
            dv(lambda e: e.reciprocal(out=m["rden"][:], in_=m["den"][:]), [m["den"]], [m["rden"]])
            dv(lambda e: e.tensor_tensor(out=gates[:, i, 0:1], in0=m["pg"][:], in1=m["rden"][:], op=ALU.mult), [m["pg"], m["rden"]], [gates])
            dv(lambda e: e.tensor_tensor(out=gates[:, i, 1:2], in0=gates[:, i, 0:1], in1=m["ed"][:], op=ALU.mult), [gates, m["ed"]], [gates])
            for g in range(4):
                dv(lambda e, g=g: e.tensor_scalar(out=m["A1"][:, g * 8:(g + 1) * 8], in0=m["oh1"][:], scalar1=m["ohg"][:, g:g + 1], scalar2=None, op0=ALU.mult),
                   [m["oh1"], m["ohg"]], [m["A1"]])
                dv(lambda e, g=g: e.tensor_scalar(out=m["A2"][:, g * 8:(g + 1) * 8], in0=m["oh2"][:], scalar1=m["ohg"][:, g:g + 1], scalar2=None, op0=ALU.mult),
                   [m["oh2"], m["ohg"]], [m["A2"]])
            dv(lambda e: e.tensor_tensor(out=m["As"][:], in0=m["A1"][:], in1=m["A2"][:], op=ALU.add), [m["A1"], m["A2"]], [m["As"]])
            dv(lambda e: e.tensor_copy(out=Asb[:], in_=m["As"][:]), [m["As"]], [Asb])
            S.op("pe", lambda e: e.matmul(pcn[:, 0:32], lhsT=trib[:], rhs=Asb[:], start=True, stop=True), reads=[trib, Asb], writes=[pcn], sig=False)
            S.op("pe", lambda e: e.matmul(pcn[:, 32:64], lhsT=onesb[:], rhs=Asb[:], start=True, stop=True), reads=[onesb, Asb], writes=[pcn])
            dv(lambda e: e.tensor_tensor(out=m["pos"][:], in0=pcn[:, 0:32], in1=cbase[:], op=ALU.add), [pcn, cbase], [m["pos"]])
            dv(lambda e: e.tensor_tensor(out=cbase[:], in0=pcn[:, 32:64], in1=cbase[:], op=ALU.add), [pcn, cbase], [cbase])
            dv(lambda e: e.tensor_scalar(out=m["ovf"][:], in0=m["pos"][:], scalar1=float(CAP), scalar2=None, op0=ALU.is_ge), [m["pos"]], [m["ovf"]])
            dv(lambda e: e.scalar_tensor_tensor(out=m["sl"][:], in0=m["ovf"][:], scalar=1.0e7, in1=m["pos"][:], op0=ALU.mult, op1=ALU.add),
               [m["ovf"], m["pos"]], [m["sl"]])
            dv(lambda e: e.tensor_tensor(out=m["sl"][:], in0=m["sl"][:], in1=ecrow[:], op=ALU.add), [m["sl"], ecrow], [m["sl"]])
            for kx, An in ((0, "A1"), (1, "A2")):
                dv(lambda e, An=An: e.tensor_tensor(out=m["tmp"][:], in0=m[An][:], in1=m["sl"][:], op=ALU.mult), [m[An], m["sl"]], [m["tmp"]])
                dv(lambda e, kx=kx: e.reduce_sum(out=m["sf"][:, kx:kx + 1], in_=m["tmp"][:], axis=AX.X), [m["tmp"]], [m["sf"]])
            dv(lambda e: e.tensor_copy(out=slots[:, 2 * i:2 * i + 2], in_=m["sf"][:]), [m["sf"]], [slots])
            dv(lambda e: e.tensor_scalar(out=m["sf"][:], in0=m["sf"][:], scalar1=float(NSLOT), scalar2=None, op0=ALU.min), [m["sf"]], [m["sf"]])
            dv(lambda e: e.tensor_copy(out=slotg[:, 2 * i:2 * i + 2], in_=m["sf"][:]), [m["sf"]], [slotg])
            for kx in range(2):
                S.dma("pool", lambda e, kx=kx, hb=hb: e.indirect_dma_start(
                    out=xin_d, out_offset=bass.IndirectOffsetOnAxis(ap=slots[:, 2 * i + kx:2 * i + kx + 1], axis=0), in_=hb[:], in_offset=None,
                    bounds_check=bc_reg, oob_is_err=False), reads=[hb, slots], writes=[xin_b])

        ntr = cfg.get('ntR', NT)
        LAG = 3
        for i0 in range(0, ntr, 2):
            tiles = [i0] + ([i0 + 1] if i0 + 1 < ntr else [])
            for i in tiles:
                r_front(i)
            lists = [S.record(lambda i=i: r_back(i)) for i in tiles]
            pos_ = [0] * len(lists)
            step = 0
            while any(pos_[k] < len(lists[k]) for k in range(len(lists))):
                for k in range(len(lists)):
                    if pos_[k] >= len(lists[k]) or (k == 1 and step < LAG and pos_[0] < len(lists[0])):
                        continue
                    S.replay(lists[k][pos_[k]])
                    pos_[k] += 1
                step += 1
        S.barrier()

    with ExitStack() as st:
        xtok = [mk(st, "xtok%d" % i, [128, D], BF16) for i in range(4)]
        XTs = [mk(st, "XT%d" % i, [128, 16, CAP], BF16) for i in range(2)]
        W13 = [mk(st, "W13_%d" % i, [128, 16, 512], BF16) for i in range(4)]
        W2s = [mk(st, "W2s_%d" % i, [128, 8, 512], BF16) for i in range(4)]
        W2f = [mk(st, "W2f_%d" % i, [128, 8, 512], F32) for i in range(2)]
        HT = mk(st, "HT", [128, 8, CAP], BF16)
        sl1 = mk(st, "sl1", [128, CCH], F32)
        yt = [mk(st, "yt%d" % i, [128, 512], F32) for i in range(4)]
        ptb = [mkp(st, "eptb%d" % i, [128, 1024], BF16) for i in range(2)]
        pb1 = [mkp(st, "pb1_%d" % i, [128, 512], F32) for i in range(2)]
        pb3 = [mkp(st, "pb3_%d" % i, [128, 512], F32) for i in range(2)]
        py = [mkp(st, "py%d" % i, [128, 512], F32) for i in range(2)]
        NTI = CAP // 128
        nE = cfg.get('nE', NE)
        S.op("dve", lambda e: e.memset(yt[0][:], 0.0), writes=[yt[0]])
        for cg in range(4):
            S.dma("sp", lambda e, cg=cg: e.dma_start(out=ybuf_d[NSLOT:NSLOT + 128, cg * 512:(cg + 1) * 512], in_=yt[0][:]), reads=[yt[0]], writes=[ybuf_b])
        n13 = 0
        n2 = 0
        ny = 0
        nxc = [0]

        def x_loads(ex):
            for ti in range(NTI):
                xk = xtok[ti % 4]
                r0 = ex * CAP + ti * 128
                S.dma("sp", lambda e, xk=xk, r0=r0: e.dma_start(out=xk[:], in_=xin_d[r0:r0 + 128, :]), reads=[xin_b], writes=[xk])

        def t_chunks(ex):
            XTe = XTs[ex % 2]
            chunks = []
            for ti in range(NTI):
                for j in range(2):
                    def chunk(ti=ti, j=j):
                        xk = xtok[ti % 4]
                        for k in range(8 * j, 8 * j + 8):
                            S.op("pe", lambda e, k=k: e.transpose(ptb[j][:, (k % 8) * 128:(k % 8 + 1) * 128], xk[:, k * 128:(k + 1) * 128], identb[:]),
                                 reads=[xk, identb], writes=[ptb[j]], sig=(k % 8 == 7))
                        evac(XTe[:, 8 * j:8 * j + 8, ti * 128:(ti + 1) * 128], ptb[j][:].rearrange("p (k t) -> p k t", k=8), [ptb[j]], [XTe])
                    chunks.append(chunk)
            return chunks

        if nE:
            x_loads(0)
            for ch in t_chunks(0):
                ch()
        for ex in range(nE):
            XT = XTs[ex % 2]
            w1v = w1_d[ex].rearrange("(k p) c -> p k c", p=128)
            w3v = w3_d[ex].rearrange("(k p) c -> p k c", p=128)
            w2v = w2_d[ex].rearrange("(k p) c -> p k c", p=128)
            w2slot = [W2s[(n2 + cg) % 4] for cg in range(4)]

            def w2_load(cg):
                stg_ = W2f[cg % 2]
                for hh_ in range(2):
                    S.dma("sp", lambda e, hh_=hh_: e.dma_start(out=stg_[:, hh_ * 4:(hh_ + 1) * 4, :], in_=w2v[:, hh_ * 4:(hh_ + 1) * 4, cg * 512:(cg + 1) * 512]),
                          writes=[stg_])

            def w2_cast(cg):
                stg_ = W2f[cg % 2]
                dst_ = w2slot[cg]
                if cg % 2 == 0:
                    S.op("act", lambda e: e.activation(out=dst_[:], in_=stg_[:], func=AF.Copy), reads=[stg_], writes=[dst_])
                else:
                    S.op("dve", lambda e: e.tensor_copy(out=dst_[:], in_=stg_[:]), reads=[stg_], writes=[dst_])

            w2_load(0)
            w2_load(1)
            for hf in range(2):
                if hf == 1 and ex + 1 < nE:
                    x_loads(ex + 1)
                wa_ = W13[n13 % 4]
                wb_ = W13[(n13 + 1) % 4]
                n13 += 2
                wload(wa_, lambda a, b, w=wa_: w[:, a:b, :], w1v[:, :, hf * 512:(hf + 1) * 512], 16, 4)
                wload(wb_, lambda a, b, w=wb_: w[:, a:b, :], w3v[:, :, hf * 512:(hf + 1) * 512], 16, 4)
                for hc in range(4):
                    for ci in range(CAP // CCH):
                        b1, b3 = pb1[ci % 2], pb3[ci % 2]
                        for bank, Wt in ((b1, wa_), (b3, wb_)):
                            for k in range(16):
                                S.op("pe", lambda e, bank=bank, Wt=Wt, k=k, hc=hc, ci=ci: e.matmul(
                                    bank[:, 0:CCH], lhsT=Wt[:, k, hc * 128:(hc + 1) * 128], rhs=XT[:, k, ci * CCH:(ci + 1) * CCH],
                                    start=(k == 0), stop=(k == 15)), reads=[Wt, XT], writes=[bank], sig=(k == 15))
                        S.op("act", lambda e, b1=b1: e.activation(out=sl1[:], in_=b1[:, 0:CCH], func=AF.Silu), reads=[b1], writes=[sl1])
                        S.op("dve", lambda e, hf=hf, hc=hc, ci=ci, b3=b3: e.tensor_tensor(out=HT[:, hf * 4 + hc, ci * CCH:(ci + 1) * CCH], in0=sl1[:], in1=b3[:, 0:CCH], op=ALU.mult),
                             reads=[sl1, b3], writes=[HT])
                    if hc in (1, 3):
                        cgc = hf * 2 + (hc // 2)
                        w2_cast(cgc)
                        if cgc + 2 < 4:
                            w2_load(cgc + 2)
            nxt = t_chunks(ex + 1) if ex + 1 < nE else []
            it_ = 0
            for cg in range(4):
                w2_ = w2slot[cg]
                n2 += 1
                for ti in range(NTI):
                    bank, y_ = py[ny % 2], yt[ny % 4]
                    ny += 1
                    for k in range(8):
                        S.op("pe", lambda e, bank=bank, w2_=w2_, k=k, ti=ti: e.matmul(bank[:, 0:512], lhsT=HT[:, k, ti * 128:(ti + 1) * 128], rhs=w2_[:, k, :],
                                                                                      start=(k == 0), stop=(k == 7)), reads=[HT, w2_], writes=[bank], sig=(k == 7))
                    evac(y_[:], bank[:, 0:512], [bank], [y_])
                    r0 = ex * CAP + ti * 128
                    S.dma("sp", lambda e, y_=y_, r0=r0, cg=cg: e.dma_start(out=ybuf_d[r0:r0 + 128, cg * 512:(cg + 1) * 512], in_=y_[:]),
                          reads=[y_], writes=[ybuf_b])
                    it_ += 1
                    if it_ % 2 == 0 and nxt:
                        nxt.pop(0)()
            while nxt:
                nxt.pop(0)()
        S.barrier()

    with ExitStack() as st:
        Wpg = mk(st, "Wpg", [128, 16, D], BF16)
        Wpp = mk(st, "Wpp", [128, 2, D], BF16)
        gbp = mk(st, "gbp", [128, D], F32)
        gbf = mk(st, "gbf", [128, D], F32)
        junk = mk(st, "fjunk", [128, D], BF16)
        stage = [mk(st, "fstage%d" % i, [128, D], F32) for i in range(2)]
        Y1s = [mk(st, "Y1_%d" % i, [128, D], F32) for i in range(2)]
        Y2s = [mk(st, "Y2_%d" % i, [128, D], F32) for i in range(2)]
        x3s = [mk(st, "x3_%d" % i, [128, D], F32) for i in range(2)]
        h3s = [mk(st, "h3_%d" % i, [128, D], BF16) for i in range(2)]
        h3Ts = [mk(st, "h3T%d" % i, [128, 16, 128], BF16) for i in range(2)]
        ptls = [mk(st, "ptl%d" % i, [128, 256], F32) for i in range(2)]
        pbls = [mk(st, "pbl%d" % i, [128, 256], BF16) for i in range(2)]
        pTls = [mk(st, "pTl%d" % i, [128, 2, 128], BF16) for i in range(2)]
        sgs = [mk(st, "sg%d" % i, [128, 512], F32) for i in range(2)]
        ssa = [mk(st, "fssa%d" % i, [128, 1], F32) for i in range(2)]
        t1a = [mk(st, "ft1a%d" % i, [128, 1], F32) for i in range(2)]
        rsa = [mk(st, "frsa%d" % i, [128, 1], F32) for i in range(2)]
        ssb = [mk(st, "fssb%d" % i, [128, 1], F32) for i in range(2)]
        t1b = [mk(st, "ft1b%d" % i, [128, 1], F32) for i in range(2)]
        rsb = [mk(st, "frsb%d" % i, [128, 1], F32) for i in range(2)]
        ptb = [mkp(st, "fptb%d" % i, [128, 1024], BF16) for i in range(2)]
        ptp = mkp(st, "fptp", [128, 1024], BF16)
        pG = [mkp(st, "fpG%d" % i, [128, 512], F32) for i in range(2)]
        pP = [mkp(st, "fpP%d" % i, [128, 512], F32) for i in range(2)]
        wload(Wpg, lambda a, b: Wpg[:, a:b, :], w_pg_d.rearrange("(k p) c -> p k c", p=128), 16, 16)
        wload(Wpp, lambda a, b: Wpp[:, a:b, :], w_pp_d.rearrange("(k p) c -> p k c", p=128), 2, 2)
        S.dma("sp", lambda e: e.dma_start(out=gbp[:], in_=g_ple_d.partition_broadcast(128)), writes=[gbp])
        S.dma("sp", lambda e: e.dma_start(out=gbf[:], in_=g_fin_d.partition_broadcast(128)), writes=[gbf])
        fn_ = [0]
        ntf = cfg.get('ntF', NT)

        def f_front(i):
            stg, Y1, Y2, h3, ptl, pbl = stage[i % 2], Y1s[i % 2], Y2s[i % 2], h3s[i % 2], ptls[i % 2], pbls[i % 2]
            ss, t1, rstd = ssa[i % 2], t1a[i % 2], rsa[i % 2]
            S.dma("sp", lambda e: e.dma_start(out=stg[:], in_=x1_d[i * 128:(i + 1) * 128, :]), reads=[x1_b], writes=[stg])
            S.dma("sp", lambda e: e.dma_start(out=ptl[:], in_=p_d[i * 128:(i + 1) * 128, :]), writes=[ptl])
            for kx, Y in ((0, Y1), (1, Y2)):
                S.dma("pool", lambda e, kx=kx, Y=Y: e.indirect_dma_start(
                    out=Y[:], out_offset=None, in_=ybuf_d, in_offset=bass.IndirectOffsetOnAxis(ap=slotg[:, 2 * i + kx:2 * i + kx + 1], axis=0),
                    bounds_check=bc_reg2, oob_is_err=False), reads=[ybuf_b, slotg], writes=[Y])

        def f_prep(i):
            stg, Y1, Y2, h3, ptl, pbl = stage[i % 2], Y1s[i % 2], Y2s[i % 2], h3s[i % 2], ptls[i % 2], pbls[i % 2]
            ss, t1, rstd = ssa[i % 2], t1a[i % 2], rsa[i % 2]
            S.op("dve", lambda e: e.scalar_tensor_tensor(out=Y1[:], in0=Y1[:], scalar=gates[:, i, 0:1], in1=stg[:], op0=ALU.mult, op1=ALU.add),
                 reads=[Y1, gates, stg], writes=[Y1])
            S.op("dve", lambda e: e.scalar_tensor_tensor(out=Y1[:], in0=Y2[:], scalar=gates[:, i, 1:2], in1=Y1[:], op0=ALU.mult, op1=ALU.add),
                 reads=[Y2, gates, Y1], writes=[Y1])
            S.op("act", lambda e: e.activation(out=junk[:], in_=Y1[:], func=AF.Square, accum_out=ss[:]), reads=[Y1], writes=[junk, ss])
            rstd_ops(ss, t1, rstd, 1.0 / D, 1e-6)
            S.op("dve", lambda e: e.scalar_tensor_tensor(out=h3[:], in0=Y1[:], scalar=rstd[:], in1=gbp[:], op0=ALU.mult, op1=ALU.mult),
                 reads=[Y1, rstd, gbp], writes=[h3])
            S.op("act", lambda e: e.activation(out=pbl[:], in_=ptl[:], func=AF.Copy), reads=[ptl], writes=[pbl])

        def f_back(i):
            x2, x3, h3, h3T, pbl, pTl, sg = Y1s[i % 2], x3s[i % 2], h3s[i % 2], h3Ts[i % 2], pbls[i % 2], pTls[i % 2], sgs[i % 2]
            ss, t1, rstd = ssb[i % 2], t1b[i % 2], rsb[i % 2]
            for k in range(16):
                S.op("pe", lambda e, k=k: e.transpose(ptb[k // 8][:, (k % 8) * 128:(k % 8 + 1) * 128], h3[:, k * 128:(k + 1) * 128], identb[:]),
                     reads=[h3, identb], writes=[ptb[k // 8]], sig=(k % 8 == 7))
            for j in range(2):
                evac(h3T[:, 8 * j:8 * j + 8, :], ptb[j][:].rearrange("p (k t) -> p k t", k=8), [ptb[j]], [h3T])
            for k in range(2):
                S.op("pe", lambda e, k=k: e.transpose(ptp[:, k * 128:(k + 1) * 128], pbl[:, k * 128:(k + 1) * 128], identb[:]),
                     reads=[pbl, identb], writes=[ptp], sig=(k == 1))
            evac(pTl[:], ptp[:, 0:256].rearrange("p (k t) -> p k t", k=2), [ptp], [pTl])
            for cg in range(4):
                bG, bP = pG[fn_[0] % 2], pP[fn_[0] % 2]
                fn_[0] += 1
                for k in range(16):
                    S.op("pe", lambda e, bG=bG, k=k, cg=cg: e.matmul(bG[:, 0:512], lhsT=h3T[:, k, :], rhs=Wpg[:, k, cg * 512:(cg + 1) * 512],
                                                                     start=(k == 0), stop=(k == 15)), reads=[h3T, Wpg], writes=[bG], sig=(k == 15))
                for k in range(2):
                    S.op("pe", lambda e, bP=bP, k=k, cg=cg: e.matmul(bP[:, 0:512], lhsT=pTl[:, k, :], rhs=Wpp[:, k, cg * 512:(cg + 1) * 512],
                                                                     start=(k == 0), stop=(k == 1)), reads=[pTl, Wpp], writes=[bP], sig=(k == 1))
                S.op("act", lambda e, bG=bG: e.activation(out=sg[:], in_=bG[:, 0:512], func=AF.Sigmoid), reads=[bG], writes=[sg])
                S.op("dve", lambda e, bP=bP: e.tensor_tensor(out=sg[:], in0=sg[:], in1=bP[:, 0:512], op=ALU.mult), reads=[sg, bP], writes=[sg])
                S.op("dve", lambda e, cg=cg: e.tensor_tensor(out=x3[:, cg * 512:(cg + 1) * 512], in0=sg[:], in1=x2[:, cg * 512:(cg + 1) * 512], op=ALU.add),
                     reads=[sg, x2], writes=[x3])
                if cg == 1 and i + 1 < ntf:
                    f_prep(i + 1)
            S.op("act", lambda e: e.activation(out=junk[:], in_=x3[:], func=AF.Square, accum_out=ss[:]), reads=[x3], writes=[junk, ss])
            rstd_ops(ss, t1, rstd, 1.0 / D, 1e-6)
            S.op("dve", lambda e: e.scalar_tensor_tensor(out=x3[:], in0=x3[:], scalar=rstd[:], in1=gbf[:], op0=ALU.mult, op1=ALU.mult),
                 reads=[x3, rstd, gbf], writes=[x3])
            S.dma("sp", lambda e: e.dma_start(out=out_d[i * 128:(i + 1) * 128, :], in_=x3[:]), reads=[x3], writes=[out_b])

        if ntf:
            f_front(0)
            f_prep(0)
        for i in range(ntf):
            if i + 1 < ntf:
                f_front(i + 1)
            f_back(i)
        S.barrier()
    S.finish()
    gstack.close()
    return nc


def _t5_bucket(dist):
    dist = np.maximum(dist, 0)
    d_f = np.maximum(dist, 1).astype(np.float32)
    large = 16 + (np.log(d_f / np.float32(16)) / np.float32(math.log(2048 / 16)) * np.float32(16)).astype(np.int32)
    large = np.minimum(large, 31)
    return np.where(dist < 16, dist, large)


def _bias_layouts(rel_bias):
    rb = np.asarray(rel_bias, np.float32)
    NEG = np.float32(-1e30)
    k = np.arange(128)[:, None]
    col = np.arange(2048)[None, :]
    dist = col - k
    bk = _t5_bucket(dist)
    ba = np.empty((8, 128, 2048), np.float32)
    for h in range(8):
        ba[h] = np.where(dist >= 0, rb[bk, h], NEG)
    bb = np.empty((6, 128, 4, 2, 128), np.float32)
    q = np.arange(128)[None, :]
    for quad in range(2):
        for g in range(3):
            d = DIL[g][1]
            for hl in range(4):
                col_h = 8 + g * 8 + quad * 4 + hl
                rel_c = q - k
                rel_p = q - k + 128
                bb[quad * 3 + g, :, hl, 0, :] = np.where(rel_c >= 0, rb[_t5_bucket(rel_c * d), col_h], NEG)
                bb[quad * 3 + g, :, hl, 1, :] = np.where(rel_p <= 128, rb[_t5_bucket(rel_p * d), col_h], NEG)
    return ba, bb.reshape(6, 128, 1024)


_NC = None


def prep_inputs(x, p, rel_bias, norm_mix_g, w_in, w_gate, lambda_q1, lambda_k1, lambda_q2, lambda_k2, subln_g,
                w_proj_a, w_proj_b, w_out, norm_ffn_g, w_coarse, w_fine, w1, w3, w2, norm_ple_g, w_ple_gate,
                w_ple_proj, final_norm_g):
    f = lambda a: np.ascontiguousarray(np.asarray(a, dtype=np.float32))
    x = f(x).reshape(NCORES, T, D)
    p = f(p)[0].reshape(NCORES, T, 256)
    ba, bb = _bias_layouts(rel_bias)
    w_router = np.ascontiguousarray(np.concatenate(
        [f(w_coarse)[0], np.transpose(f(w_fine)[0], (1, 0, 2)).reshape(D, 32)], axis=1))
    tri = (np.arange(128)[:, None] < np.arange(128)[None, :]).astype(np.float32)
    ecrow = np.ascontiguousarray(np.broadcast_to((np.arange(NE, dtype=np.float32) * CAP)[None, :], (128, NE)))
    lam = np.ascontiguousarray(np.stack([f(lambda_q1)[0], f(lambda_k1)[0], f(lambda_q2)[0], f(lambda_k2)[0]], 0))
    shared = {
        "bias_a": ba, "bias_b": bb,
        "norm_mix_g": f(norm_mix_g)[0], "norm_ffn_g": f(norm_ffn_g)[0], "norm_ple_g": f(norm_ple_g)[0],
        "final_norm_g": f(final_norm_g),
        "w_in": f(w_in)[0], "w_gate": f(w_gate)[0], "lam": lam, "subln_g": f(subln_g)[0],
        "w_proj_a": f(w_proj_a)[0], "w_proj_b": f(w_proj_b)[0], "w_out": f(w_out)[0], "w_router": w_router,
        "w1": f(w1)[0], "w3": f(w3)[0], "w2": f(w2)[0], "w_ple_gate": f(w_ple_gate)[0], "w_ple_proj": f(w_ple_proj)[0],
        "identf": np.eye(128, dtype=np.float32), "tri": tri, "ecrow": ecrow,
    }
    in_maps = []
    for c in range(NCORES):
        m = dict(shared)
        m["x"] = np.ascontiguousarray(x[c])
        m["p"] = np.ascontiguousarray(p[c])
        in_maps.append(m)
    return in_maps


def kernel(**inputs):
    global _NC
    in_maps = prep_inputs(**inputs)
    if _NC is None:
        _NC = build_nc()
    res = run_bass_kernel_spmd(_NC, in_maps, core_ids=list(range(NCORES)))
    out = np.stack([np.asarray(r["out"], dtype=np.float32) for r in res.results], 0)
    return out.reshape(16, SEQ, D)
```

```python
import math
from contextlib import ExitStack

import numpy as np
import concourse.bass as bass
import concourse.mybir as mybir
from concourse.bass_utils import run_bass_kernel_spmd

F32 = mybir.dt.float32
BF16 = mybir.dt.bfloat16
I32 = mybir.dt.int32
AF = mybir.ActivationFunctionType
ALU = mybir.AluOpType
AX = mybir.AxisListType

NCORES = 8
D = 2048
SEQ = 2048
T = 2 * SEQ
NT = T // 128
CAP = 512
CCH = 256
NE = 32
NSLOT = NE * CAP
LAM_INIT = 0.2
SC_A = 128 ** -0.5
SC_B = 64 ** -0.5
DIL = ((128, 1), (512, 4), (2048, 16))


class Buf:
    __slots__ = ("name", "writers", "readers")

    def __init__(self, name=""):
        self.name = name
        self.writers = {}
        self.readers = {}


class Rec:
    __slots__ = ("eng", "cnt", "is_dma", "sem")

    def __init__(self, eng, is_dma, sem):
        self.eng = eng
        self.is_dma = is_dma
        self.sem = sem
        self.cnt = None


class TB:
    def __init__(self, h, name=""):
        self.h = h
        self.b = Buf(name)

    def __getitem__(self, k):
        return self.h[k]


class Sch:
    def __init__(self, nc):
        self.nc = nc
        self.E = {"pe": nc.tensor, "act": nc.scalar, "dve": nc.vector, "pool": nc.gpsimd, "sp": nc.sync}
        self.sem = {e: nc.alloc_semaphore("s_" + e) for e in ("pe", "act", "dve", "pool")}
        self.cnt = {e: 0 for e in self.sem}
        self.waited = {e: {} for e in self.E}
        self.dsem = {}
        self.pending = []
        self.nsem = 4
        self.n_ops = 0
        self.rec = None

    def _deps(self, eng, reads, writes):
        need = {}

        def add(r, raw):
            if not r.is_dma:
                if r.eng == eng and (eng == "pe" or not raw):
                    return
            assert r.cnt is not None, "dependency on unsignaled op (%s)" % r.eng
            k = id(r.sem)
            if need.get(k, (None, 0))[1] < r.cnt:
                need[k] = (r.sem, r.cnt)

        for b in reads:
            for r in b.writers.values():
                add(r, True)
        for b in writes:
            if b.readers:
                for r in b.readers.values():
                    add(r, False)
                for r in b.writers.values():
                    add(r, False)
        return need

    def _emit_waits(self, eng, need):
        E = self.E[eng]
        w = self.waited[eng]
        for k, (sem, val) in need.items():
            if w.get(k, 0) < val:
                E.wait_ge(sem, val)
                w[k] = val

    def _update(self, rec, key, reads, writes):
        for b in writes:
            if b.readers:
                b.writers = {key: rec}
                b.readers = {}
            else:
                b.writers[key] = rec
        for b in reads:
            b.readers[key] = rec

    def record(self, f):
        self.rec = []
        f()
        r, self.rec = self.rec, None
        return r

    def replay(self, item):
        kind, args = item
        (self.op if kind == "op" else self.dma)(*args)

    def op(self, eng, fn, reads=(), writes=(), sig=True):
        if self.rec is not None:
            self.rec.append(("op", (eng, fn, list(reads), list(writes), sig)))
            return None
        reads = [x.b if isinstance(x, TB) else x for x in reads]
        writes = [x.b if isinstance(x, TB) else x for x in writes]
        need = self._deps(eng, reads, writes)
        self._emit_waits(eng, need)
        ins = fn(self.E[eng])
        rec = Rec(eng, False, self.sem[eng])
        if sig:
            self.cnt[eng] += 1
            ins.then_inc(self.sem[eng], 1)
            rec.cnt = self.cnt[eng]
            if eng == "pe" and self.pending:
                for p in self.pending:
                    p.cnt = rec.cnt
                self.pending = []
        else:
            assert eng == "pe"
            self.pending.append(rec)
        self._update(rec, eng, reads, writes)
        self.n_ops += 1
        return rec

    def dma(self, q, fn, reads=(), writes=()):
        if self.rec is not None:
            self.rec.append(("dma", (q, fn, list(reads), list(writes))))
            return None
        reads = [x.b if isinstance(x, TB) else x for x in reads]
        writes = [x.b if isinstance(x, TB) else x for x in writes]
        need = self._deps(q, reads, writes)
        self._emit_waits(q, need)
        b0 = writes[0]
        if id(b0) not in self.dsem:
            self.dsem[id(b0)] = [self.nc.alloc_semaphore("d_%d" % self.nsem), 0]
            self.nsem += 1
            assert self.nsem <= 100, "too many DMA semaphores"
        ds = self.dsem[id(b0)]
        ins = fn(self.E[q])
        ds[1] += 16
        ins.then_inc(ds[0], 16)
        rec = Rec(q, True, ds[0])
        rec.cnt = ds[1]
        self._update(rec, ("dma", id(ds[0])), reads, writes)
        self.n_ops += 1
        return rec

    def _all_need(self, skip=None):
        need = {}
        for e in self.sem:
            if e != skip and self.cnt[e] > 0:
                need[id(self.sem[e])] = (self.sem[e], self.cnt[e])
        for s, c in self.dsem.values():
            if c > 0:
                need[id(s)] = (s, c)
        return need

    def barrier(self):
        assert not self.pending
        for f in self.E:
            self._emit_waits(f, self._all_need(skip=f))

    def finish(self):
        assert not self.pending
        self._emit_waits("sp", self._all_need())


def build_nc(cfg=None):
    cfg = cfg or {}
    dbg = cfg.get('dbg', False)
    nc = bass.Bass("TRN2", target_bir_lowering=False)
    S = Sch(nc)

    def din(name, shape, dt=F32):
        return nc.dram_tensor(name, list(shape), dt, kind="ExternalInput").ap()

    def dscr(name, shape, dt):
        return nc.dram_tensor(name, list(shape), dt, kind=("ExternalOutput" if dbg else "Internal")).ap()

    x_d = din("x", [T, D])
    p_d = din("p", [T, 256])
    bias_a_d = din("bias_a", [8, 128, 2048])
    bias_b_d = din("bias_b", [6, 128, 1024])
    g_mix_d = din("norm_mix_g", [D])
    g_ffn_d = din("norm_ffn_g", [D])
    g_ple_d = din("norm_ple_g", [D])
    g_fin_d = din("final_norm_g", [D])
    w_in_d = din("w_in", [D, 10752])
    w_gate_d = din("w_gate", [D, 4096])
    lam_d = din("lam", [4, 128])
    subln_d = din("subln_g", [256])
    w_pa_d = din("w_proj_a", [D, D])
    w_pb_d = din("w_proj_b", [512, D])
    w_out_d = din("w_out", [D, D])
    w_rt_d = din("w_router", [D, 36])
    NEd = cfg.get('nEdecl', NE)
    w1_d = din("w1", [NEd, D, 1024])
    w3_d = din("w3", [NEd, D, 1024])
    w2_d = din("w2", [NEd, 1024, D])
    w_pg_d = din("w_ple_gate", [D, D])
    w_pp_d = din("w_ple_proj", [256, D])
    identf_d = din("identf", [128, 128])
    tri_d = din("tri", [128, 128])
    ecrow_d = din("ecrow", [128, NE])
    out_d = nc.dram_tensor("out", [T, D], F32, kind="ExternalOutput").ap()
    out_b = Buf("out")

    oaT_d = dscr("oaT", [2, 128, 16, SEQ], BF16)
    obT_d = dscr("obT", [2, 128, 4, SEQ], BF16)
    x1_d = dscr("x1", [T, D], F32)
    xin_d = dscr("xin", [NSLOT, D], BF16)
    ybuf_d = dscr("ybuf", [NSLOT + 128, D], F32)
    oaT_b, obT_b, x1_b, xin_b, ybuf_b = Buf("oaT"), Buf("obT"), Buf("x1"), Buf("xin"), Buf("ybuf")

    gstack = ExitStack()
    bc_reg = nc.gpsimd.to_reg(NSLOT - 1)
    bc_reg2 = nc.gpsimd.to_reg(NSLOT + 127)

    uid = [0]

    def mk(stack, name, shape, dt):
        uid[0] += 1
        return TB(stack.enter_context(nc.sbuf_tensor("sb%d_%s" % (uid[0], name), list(shape), dt)), name)

    def mkp(stack, name, shape, dt):
        uid[0] += 1
        return TB(stack.enter_context(nc.psum_tensor("ps%d_%s" % (uid[0], name), list(shape), dt)), name)

    identf = mk(gstack, "identf", [128, 128], F32)
    identb = mk(gstack, "identb", [128, 128], BF16)
    trib = mk(gstack, "trib", [128, 128], BF16)
    onesb = mk(gstack, "onesb", [128, 128], BF16)
    ecrow = mk(gstack, "ecrow", [128, NE], F32)
    neglam = mk(gstack, "neglam", [128, 1], F32)
    gsub = mk(gstack, "gsub", [128, 256], F32)
    slots = mk(gstack, "slots", [128, NT * 2], I32)
    slotg = mk(gstack, "slotg", [128, NT * 2], I32)
    gates = mk(gstack, "gates", [128, NT, 2], F32)
    cbase = mk(gstack, "cbase", [128, NE], F32)

    S.dma("sp", lambda e: e.dma_start(out=identf[:], in_=identf_d), writes=[identf])
    S.dma("sp", lambda e: e.dma_start(out=ecrow[:], in_=ecrow_d), writes=[ecrow])
    S.op("dve", lambda e: e.tensor_copy(out=identb[:], in_=identf[:]), reads=[identf], writes=[identb])
    S.op("dve", lambda e: e.memset(onesb[:], 1.0), writes=[onesb])
    S.op("dve", lambda e: e.memset(cbase[:], 0.0), writes=[cbase])
    with ExitStack() as st:
        trif = mk(st, "trif", [128, 128], F32)
        lamv = mk(st, "lamv", [128, 4, 128], F32)
        lamp = mk(st, "lamp", [128, 2, 128], F32)
        lams = mk(st, "lams", [128, 2], F32)
        lame = mk(st, "lame", [128, 2], F32)
        S.dma("sp", lambda e: e.dma_start(out=trif[:], in_=tri_d), writes=[trif])
        S.op("dve", lambda e: e.tensor_copy(out=trib[:], in_=trif[:]), reads=[trif], writes=[trib])
        for i in range(4):
            S.dma("sp", lambda e, i=i: e.dma_start(out=lamv[:, i, :], in_=lam_d[i].partition_broadcast(128)), writes=[lamv])
        S.dma("sp", lambda e: e.dma_start(out=gsub[:], in_=subln_d.partition_broadcast(128)), writes=[gsub])
        S.op("dve", lambda e: e.tensor_tensor(out=lamp[:, 0, :], in0=lamv[:, 0, :], in1=lamv[:, 1, :], op=ALU.mult), reads=[lamv], writes=[lamp])
        S.op("dve", lambda e: e.tensor_tensor(out=lamp[:, 1, :], in0=lamv[:, 2, :], in1=lamv[:, 3, :], op=ALU.mult), reads=[lamv], writes=[lamp])
        S.op("dve", lambda e: e.reduce_sum(out=lams[:, 0:1], in_=lamp[:, 0, :], axis=AX.X), reads=[lamp], writes=[lams])
        S.op("dve", lambda e: e.reduce_sum(out=lams[:, 1:2], in_=lamp[:, 1, :], axis=AX.X), reads=[lamp], writes=[lams])
        S.op("act", lambda e: e.activation(out=lame[:], in_=lams[:], func=AF.Exp), reads=[lams], writes=[lame])
        S.op("dve", lambda e: e.tensor_tensor(out=neglam[:], in0=lame[:, 1:2], in1=lame[:, 0:1], op=ALU.subtract), reads=[lame], writes=[neglam])
        S.op("dve", lambda e: e.tensor_scalar(out=neglam[:], in0=neglam[:], scalar1=-LAM_INIT, scalar2=None, op0=ALU.add), reads=[neglam], writes=[neglam])
        S.op("dve", lambda e: e.tensor_scalar(out=gsub[:], in0=gsub[:], scalar1=1.0 - LAM_INIT, scalar2=None, op0=ALU.mult), reads=[gsub], writes=[gsub])
        S.barrier()

    def wload(dst, dst_ap_fn, src_ap, kchunks, nsplit, reads=()):
        step = kchunks // nsplit
        for i in range(nsplit):
            S.dma("pool", lambda e, i=i: e.dma_start(out=dst_ap_fn(i * step, (i + 1) * step), in_=src_ap[:, i * step:(i + 1) * step, :]),
                  reads=list(reads), writes=[dst])

    def rstd_ops(ss, t1, rstd, inv_n, eps):
        S.op("dve", lambda e: e.tensor_scalar(out=t1[:], in0=ss[:], scalar1=inv_n, scalar2=eps, op0=ALU.mult, op1=ALU.add), reads=[ss], writes=[t1])
        S.op("act", lambda e: e.activation(out=t1[:], in_=t1[:], func=AF.Ln), reads=[t1], writes=[t1])
        S.op("act", lambda e: e.activation(out=rstd[:], in_=t1[:], func=AF.Exp, scale=-0.5), reads=[t1], writes=[rstd])

    evac_flip = [0]

    def evac(out_ap, in_ap, reads, writes):
        evac_flip[0] ^= 1
        if evac_flip[0]:
            S.op("act", lambda e: e.activation(out=out_ap, in_=in_ap, func=AF.Copy), reads=reads, writes=writes)
        else:
            S.op("dve", lambda e: e.tensor_copy(out=out_ap, in_=in_ap), reads=reads, writes=writes)

    w_in_v = w_in_d.rearrange("(k p) c -> p k c", p=128)

    for s in range(cfg.get('nseq', 2)):
        hstack = ExitStack()
        hT = mk(hstack, "hT", [128, 16, SEQ], BF16)
        with ExitStack() as st:
            stage = [mk(st, "stage%d" % i, [128, D], F32) for i in range(2)]
            junk = mk(st, "junk", [128, D], BF16)
            xs = mk(st, "xs", [128, D], BF16)
            gb = mk(st, "gb", [128, D], F32)
            ss = mk(st, "ss", [128, 1], F32)
            t1 = mk(st, "t1", [128, 1], F32)
            rstd = mk(st, "rstd", [128, 1], F32)
            Wq = mk(st, "Wq", [128, 16, 256], BF16)
            Wk = mk(st, "Wk", [128, 16, 256], BF16)
            Wv = mk(st, "Wv", [128, 16, 256], BF16)
            QT = mk(st, "QT", [128, 2, SEQ], BF16)
            KT = mk(st, "KT", [128, 2, SEQ], BF16)
            V = mk(st, "V", [128, 16, 264], BF16)
            EA = mk(st, "EA", [128, 2048], BF16)
            EB = EA
            PT = [mk(st, "PT%d" % i, [128, 2, 256], BF16) for i in range(3)]
            PTB = [mk(st, "PTB%d" % i, [128, 512], BF16) for i in range(3)]
            o1 = [mk(st, "o1_%d" % i, [128, 256], F32) for i in range(2)]
            oo = [mk(st, "oo_%d" % i, [128, 256], F32) for i in range(2)]
            junk2 = mk(st, "junk2", [128, 256], BF16)
            oab = [mk(st, "oab%d" % i, [128, 256], BF16) for i in range(2)]
            oaTu = mk(st, "oaTu", [128, 2, 512], BF16)
            rr = [mk(st, "rr%d" % i, [128, 2], F32) for i in range(2)]
            nl = [mk(st, "nl%d" % i, [128, 1], F32) for i in range(2)]
            ss2 = mk(st, "ss2", [128, 1], F32)
            t2 = mk(st, "t2", [128, 1], F32)
            rs2 = mk(st, "rs2", [128, 1], F32)
            accUD = mk(st, "accUD", [128, 2, 2, SEQ], F32)
            obTq = mk(st, "obTq", [128, 2, 512], BF16)
            acc = [mkp(st, "acc%d" % i, [128, 512], F32) for i in range(4)]
            pj = [mkp(st, "pj%d" % i, [128, 512], F32) for i in range(3)]
            ptb = [mkp(st, "ptb%d" % i, [128, 1024], BF16) for i in range(1)]

            S.dma("sp", lambda e: e.dma_start(out=gb[:], in_=g_mix_d.partition_broadcast(128)), writes=[gb])
            S.op("dve", lambda e: e.memset(V[:, :, 256:264], 1.0), writes=[V])

            for tt in range(16):
                r0 = s * SEQ + tt * 128
                stg = stage[tt % 2]
                S.dma("sp", lambda e, stg=stg, r0=r0: e.dma_start(out=stg[:], in_=x_d[r0:r0 + 128, :]), writes=[stg])
                S.op("act", lambda e, stg=stg: e.activation(out=junk[:], in_=stg[:], func=AF.Square, accum_out=ss[:]), reads=[stg], writes=[junk, ss])
                rstd_ops(ss, t1, rstd, 1.0 / D, 1e-6)
                S.op("dve", lambda e, stg=stg: e.scalar_tensor_tensor(out=xs[:], in0=stg[:], scalar=rstd[:], in1=gb[:], op0=ALU.mult, op1=ALU.mult),
                     reads=[stg, rstd, gb], writes=[xs])
                for j in range(2):
                    for k in range(8 * j, 8 * j + 8):
                        S.op("pe", lambda e, k=k: e.transpose(ptb[0][:, (k % 8) * 128:(k % 8 + 1) * 128], xs[:, k * 128:(k + 1) * 128], identb[:]),
                             reads=[xs, identb], writes=[ptb[0]], sig=(k % 8 == 7))
                    evac(hT[:, 8 * j:8 * j + 8, tt * 128:(tt + 1) * 128], ptb[0][:].rearrange("p (k t) -> p k t", k=8), [ptb[0]], [hT])

            units = [("A", h) for h in range(8)] + [("B", q * 3 + g) for q in range(2) for g in range(3)]
            if 'units' in cfg:
                units = cfg['units']
            pjn = [0]

            def next_pj():
                pjn[0] = (pjn[0] + 1) % 3
                return pj[pjn[0]]

            for kind, ui in units:
                if kind == "A":
                    h = ui
                    qc, kc, vc = h * 256, 2048 + h * 256, 4096 + h * 256
                    d, nblk = 1, 16
                else:
                    quad, g = divmod(ui, 3)
                    d = DIL[g][1]
                    nblk = (SEQ // d) // 128
                    qc = 6144 + g * 512 + quad * 256
                    kc = 7680 + g * 512 + quad * 256
                    vc = 9216 + g * 512 + quad * 256
                L = SEQ // d
                for Wt, c0 in ((Wq, qc), (Wk, kc), (Wv, vc)):
                    wload(Wt, lambda a, b, Wt=Wt: Wt[:, a:b, :], w_in_v[:, :, c0:c0 + 256], 16, 4)
                if kind == "A":
                    stg = stage[0]
                    S.dma("sp", lambda e, stg=stg, h=h: e.dma_start(out=stg[:], in_=bias_a_d[h]), writes=[stg])
                    S.op("act", lambda e, stg=stg: e.activation(out=EA[:], in_=stg[:], func=AF.Copy, scale=1.0 / SC_A), reads=[stg], writes=[EA])
                elif not cfg.get('Bnobias'):
                    stg = stage[1]
                    S.dma("sp", lambda e, stg=stg, ui=ui: e.dma_start(out=stg[:, 0:1024], in_=bias_b_d[ui]), writes=[stg])
                    if not cfg.get('Bnoexp'):
                        S.op("act", lambda e, stg=stg: e.activation(out=EB[:, 0:1024], in_=stg[:, 0:1024], func=AF.Copy, scale=1.0 / SC_B), reads=[stg], writes=[EB])
                def proj_half(dst, Wt, half, bsplit=False):
                    for tg in range(4):
                        bank = next_pj()
                        for k in range(16):
                            S.op("pe", lambda e, bank=bank, Wt=Wt, k=k, half=half, tg=tg: e.matmul(
                                bank[:, 0:512], lhsT=Wt[:, k, half * 128:(half + 1) * 128], rhs=hT[:, k, tg * 512:(tg + 1) * 512],
                                start=(k == 0), stop=(k == 15)), reads=[Wt, hT], writes=[bank], sig=(k == 15))
                        l0 = tg * 512 // d
                        if not bsplit:
                            if d == 1:
                                evac(dst[:, half, tg * 512:(tg + 1) * 512], bank[:, 0:512], [bank], [dst])
                            else:
                                oap = dst[:, half, :].rearrange("p (c l) -> p c l", c=d)[:, :, l0:l0 + 512 // d]
                                iap = bank[:, 0:512].rearrange("p (l c) -> p c l", c=d)
                                evac(oap, iap, [bank], [dst])
                        else:
                            for hh in range(2):
                                p0 = hh * 64
                                if d == 1:
                                    evac(dst[p0:p0 + 64, hh, tg * 512:(tg + 1) * 512], bank[p0:p0 + 64, 0:512], [bank], [dst])
                                else:
                                    oap = dst[p0:p0 + 64, hh, :].rearrange("p (c l) -> p c l", c=d)[:, :, l0:l0 + 512 // d]
                                    iap = bank[p0:p0 + 64, 0:512].rearrange("p (l c) -> p c l", c=d)
                                    evac(oap, iap, [bank], [dst])

                for half in range(2):
                    proj_half(QT, Wq, half)
                if kind == "A":
                    for half in range(2):
                        proj_half(KT, Wk, half)
                else:
                    S.op("dve", lambda e: e.memset(KT[64:128, 0, :], 0.0), writes=[KT])
                    S.op("dve", lambda e: e.memset(KT[0:64, 1, :], 0.0), writes=[KT])
                for sbk in range(16):
                    c_, n_ = divmod(sbk, nblk)
                    t0 = n_ * 128 * d + c_
                    bank = next_pj()
                    for k in range(16):
                        S.op("pe", lambda e, bank=bank, k=k, t0=t0, d=d: e.matmul(
                            bank[:, 0:256], lhsT=hT[:, k, t0:t0 + 127 * d + 1:d], rhs=Wv[:, k, :],
                            start=(k == 0), stop=(k == 15)), reads=[Wv, hT], writes=[bank], sig=(k == 15))
                    evac(V[:, sbk, 0:256], bank[:, 0:256], [bank], [V])

                if kind == "A":
                    steps = [(c, kb) for c in range(8) for kb in range(2 * c + 2)]

                    def qk_stage(i):
                        c, kb = steps[i]
                        qlo = 2 * c if kb <= 2 * c else 2 * c + 1
                        off = (qlo - 2 * c) * 128
                        bank = pj[i % 3]
                        pt = PT[i % 3]
                        e0 = (qlo - kb) * 128
                        e1 = (2 * c + 2 - kb) * 128
                        for m in range(2):
                            S.op("pe", lambda e, bank=bank, m=m, kb=kb, qlo=qlo, c=c, off=off: e.matmul(
                                bank[:, m * 256 + off:m * 256 + 256], lhsT=KT[:, m, kb * 128:(kb + 1) * 128],
                                rhs=QT[:, m, qlo * 128:(2 * c + 2) * 128], start=True, stop=False),
                                reads=[KT, QT], writes=[bank], sig=False)
                            S.op("pe", lambda e, bank=bank, m=m, off=off, e0=e0, e1=e1: e.matmul(
                                bank[:, m * 256 + off:m * 256 + 256], lhsT=identb[:], rhs=EA[:, e0:e1], start=False, stop=True),
                                reads=[identb, EA], writes=[bank], sig=(m == 1))
                        S.op("act", lambda e, bank=bank, pt=pt, off=off: e.activation(
                            out=pt[:, :, off:256], in_=bank[:, 0:512].rearrange("p (m q) -> p m q", m=2)[:, :, off:256],
                            func=AF.Exp, scale=SC_A), reads=[bank], writes=[pt])

                    def pv_stage(i):
                        c, kb = steps[i]
                        qlo = 2 * c if kb <= 2 * c else 2 * c + 1
                        pt = PT[i % 3]
                        mm = [(m, qb) for m in range(2) for qb in range(qlo, 2 * c + 2)]
                        for ii, (m, qb) in enumerate(mm):
                            j = qb - 2 * c
                            a_ = acc[m * 2 + j]
                            S.op("pe", lambda e, a_=a_, pt=pt, m=m, j=j, kb=kb, qb=qb: e.matmul(
                                a_[:, 0:264], lhsT=pt[:, m, j * 128:(j + 1) * 128], rhs=V[:, kb, 0:264],
                                start=(kb == 0), stop=(kb == qb)), reads=[pt, V], writes=[a_], sig=(ii == len(mm) - 1))

                    def epi1(c):
                        for j in range(2):
                            a0, a1 = acc[j], acc[2 + j]
                            rr_, nl_, o1_, oo_ = rr[j], nl[j], o1[j], oo[j]
                            S.op("dve", lambda e, a0=a0, rr_=rr_: e.reciprocal(out=rr_[:, 0:1], in_=a0[:, 256:257]), reads=[a0], writes=[rr_])
                            S.op("dve", lambda e, a1=a1, rr_=rr_: e.reciprocal(out=rr_[:, 1:2], in_=a1[:, 256:257]), reads=[a1], writes=[rr_])
                            S.op("dve", lambda e, rr_=rr_, nl_=nl_: e.tensor_tensor(out=nl_[:], in0=rr_[:, 1:2], in1=neglam[:], op=ALU.mult), reads=[rr_, neglam], writes=[nl_])
                            S.op("act", lambda e, a0=a0, rr_=rr_, o1_=o1_: e.activation(out=o1_[:], in_=a0[:, 0:256], func=AF.Copy, scale=rr_[:, 0:1]), reads=[a0, rr_], writes=[o1_])
                            S.op("dve", lambda e, a1=a1, nl_=nl_, o1_=o1_, oo_=oo_: e.scalar_tensor_tensor(out=oo_[:], in0=a1[:, 0:256], scalar=nl_[:], in1=o1_[:], op0=ALU.mult, op1=ALU.add),
                                 reads=[a1, nl_, o1_], writes=[oo_])
                        for j in range(2):
                            oo_ = oo[j]
                            ob_ = oab[j]
                            S.op("act", lambda e, oo_=oo_: e.activation(out=junk2[:], in_=oo_[:], func=AF.Square, accum_out=ss2[:]), reads=[oo_], writes=[junk2, ss2])
                            rstd_ops(ss2, t2, rs2, 1.0 / 256, 1e-5)
                            S.op("dve", lambda e, ob_=ob_, oo_=oo_: e.scalar_tensor_tensor(out=ob_[:], in0=oo_[:], scalar=rs2[:], in1=gsub[:], op0=ALU.mult, op1=ALU.mult),
                                 reads=[oo_, rs2, gsub], writes=[ob_])

                    def epi2(c):
                        for j in range(2):
                            qb = 2 * c + j
                            ob_ = oab[j]
                            pb_ = ptb[0]
                            for hh in range(2):
                                S.op("pe", lambda e, pb_=pb_, hh=hh, ob_=ob_: e.transpose(pb_[:, hh * 128:(hh + 1) * 128], ob_[:, hh * 128:(hh + 1) * 128], identb[:]),
                                     reads=[ob_, identb], writes=[pb_], sig=(hh == 1))
                            ql = qb % 4
                            evac(oaTu[:, :, ql * 128:(ql + 1) * 128], pb_[:, 0:256].rearrange("p (k t) -> p k t", k=2), [pb_], [oaTu])
                            if ql == 3:
                                q0 = (qb - 3) * 128
                                S.dma("sp", lambda e, h=h, q0=q0: e.dma_start(out=oaT_d[s, :, 2 * h:2 * h + 2, q0:q0 + 512], in_=oaTu[:]),
                                      reads=[oaTu], writes=[oaT_b])

                    pend_epi2 = None
                    qk_stage(0)
                    qk_stage(1)
                    for i, (c, kb) in enumerate(steps):
                        if i + 2 < len(steps):
                            qk_stage(i + 2)
                        pv_stage(i)
                        if kb == 1 and pend_epi2 is not None:
                            epi2(pend_epi2)
                            pend_epi2 = None
                        if kb == 2 * c + 1:
                            epi1(c)
                            pend_epi2 = c
                    epi2(pend_epi2)
                else:
                    it = 0
                    EBv = EB[:, 0:1024].rearrange("p (h w q) -> p h w q", h=4, w=2)
                    for pair in range(2):
                        proj_half(KT, Wk, pair, bsplit=True)
                        def b_qk(sbk, it_):
                            c_, n_ = divmod(sbk, nblk)
                            hp = n_ > 0
                            bank = pj[it_ % 3]
                            pt = PTB[it_ % 3]
                            nw = 2 if hp else 1
                            i = 0
                            for hh in range(2):
                                for wh in range(nw):
                                    i += 1
                                    S.op("pe", lambda e, bank=bank, hh=hh, wh=wh, pair=pair, sbk=sbk: e.matmul(
                                        bank[:, (hh * 2 + wh) * 128:(hh * 2 + wh + 1) * 128],
                                        lhsT=KT[:, hh, (sbk - wh) * 128:(sbk - wh + 1) * 128],
                                        rhs=QT[:, pair, sbk * 128:(sbk + 1) * 128], start=True, stop=False),
                                        reads=[KT, QT], writes=[bank], sig=False)
                                    S.op("pe", lambda e, bank=bank, hh=hh, wh=wh, pair=pair: e.matmul(
                                        bank[:, (hh * 2 + wh) * 128:(hh * 2 + wh + 1) * 128],
                                        lhsT=identb[:], rhs=EBv[:, pair * 2 + hh, wh, :], start=False, stop=True),
                                        reads=[identb, EB], writes=[bank], sig=(i == 2 * nw))
                            ptv = pt[:].rearrange("p (h w q) -> p h w q", h=2, w=2)
                            bkv = bank[:, 0:512].rearrange("p (h w q) -> p h w q", h=2, w=2)
                            if hp:
                                S.op("act", lambda e, bank=bank, pt=pt: e.activation(out=pt[:], in_=bank[:, 0:512], func=AF.Exp, scale=SC_B),
                                     reads=[bank], writes=[pt])
                            else:
                                S.op("act", lambda e, ptv=ptv, bkv=bkv: e.activation(out=ptv[:, :, 0, :], in_=bkv[:, :, 0, :], func=AF.Exp, scale=SC_B),
                                     reads=[bank], writes=[pt])

                        def b_pv(sbk, it_):
                            c_, n_ = divmod(sbk, nblk)
                            hp = n_ > 0
                            pt = PTB[it_ % 3]
                            ut = acc[it_ % 4]
                            nw = 2 if hp else 1
                            ptv = pt[:].rearrange("p (h w q) -> p h w q", h=2, w=2)
                            for hh in range(2):
                                for wh in range(nw):
                                    kblk = sbk - wh
                                    S.op("pe", lambda e, ut=ut, kblk=kblk, pair=pair, ptv=ptv, hh=hh, wh=wh, nw=nw: e.matmul(
                                        ut[:, hh * 128:(hh + 1) * 128], lhsT=V[:, kblk, pair * 128:(pair + 1) * 128], rhs=ptv[:, hh, wh, :],
                                        start=(wh == 0), stop=(wh == nw - 1)), reads=[V, pt], writes=[ut], sig=False)
                            for hh in range(2):
                                for wh in range(nw):
                                    S.op("pe", lambda e, ut=ut, ptv=ptv, hh=hh, wh=wh, nw=nw: e.matmul(
                                        ut[:, (2 + hh) * 128:(3 + hh) * 128], lhsT=onesb[:], rhs=ptv[:, hh, wh, :],
                                        start=(wh == 0), stop=(wh == nw - 1)), reads=[onesb, pt], writes=[ut], sig=(hh == 1 and wh == nw - 1))
                            t0 = n_ * 128 * d + c_
                            for hh in range(2):
                                p0 = hh * 64
                                oap = accUD[p0:p0 + 64, pair, :, t0:t0 + 127 * d + 1:d]
                                iap = ut[p0:p0 + 64, 0:512].rearrange("p (u h q) -> p u h q", u=2, h=2)[:, :, hh, :]
                                if g == 0:
                                    S.op("dve", lambda e, oap=oap, iap=iap: e.tensor_copy(out=oap, in_=iap), reads=[ut], writes=[accUD])
                                else:
                                    S.op("dve", lambda e, oap=oap, iap=iap: e.tensor_tensor(out=oap, in0=oap, in1=iap, op=ALU.add), reads=[ut], writes=[accUD])

                        b_qk(0, it)
                        b_qk(1, it + 1)
                        for sbk in range(16):
                            if sbk + 2 < 16:
                                b_qk(sbk + 2, it + 2)
                            b_pv(sbk, it)
                            it += 1
                    if g == 2:
                        for pair in range(2):
                            S.op("dve", lambda e, pair=pair: e.reciprocal(out=accUD[:, pair, 1, :], in_=accUD[:, pair, 1, :]), reads=[accUD], writes=[accUD])
                        for tq in range(4):
                            for pair in range(2):
                                S.op("dve", lambda e, pair=pair, tq=tq: e.tensor_tensor(
                                    out=obTq[:, pair, :], in0=accUD[:, pair, 0, tq * 512:(tq + 1) * 512],
                                    in1=accUD[:, pair, 1, tq * 512:(tq + 1) * 512], op=ALU.mult), reads=[accUD], writes=[obTq])
                            S.dma("sp", lambda e, quad=quad, tq=tq: e.dma_start(out=obT_d[s, :, quad * 2:quad * 2 + 2, tq * 512:(tq + 1) * 512], in_=obTq[:]),
                                  reads=[obTq], writes=[obT_b])
            S.barrier()

        with ExitStack() as st:
          if cfg.get('C', True):
              oaTh = mk(st, "oaTh", [128, 16, 1024], BF16)
              obTh = mk(st, "obTh", [128, 4, 1024], BF16)
              mT = mk(st, "mT", [128, 16, 1024], BF16)
              Wg1 = [mk(st, "Wg1_%d" % i, [128, 16, 128], BF16) for i in range(2)]
              Wg2 = [mk(st, "Wg2_%d" % i, [128, 16, 128], BF16) for i in range(2)]
              Wa = [mk(st, "Wa_%d" % i, [128, 16, 128], BF16) for i in range(2)]
              Wb = [mk(st, "Wb_%d" % i, [128, 4, 128], BF16) for i in range(2)]
              Wo = mk(st, "Wo", [128, 16, 512], BF16)
              xt = [mk(st, "xt%d" % i, [128, 512], F32) for i in range(2)]
              x1t = [mk(st, "x1t%d" % i, [128, 512], F32) for i in range(2)]
              s1 = mk(st, "s1", [128, 512], F32)
              s2 = mk(st, "s2", [128, 512], F32)
              pG1 = mkp(st, "pG1", [128, 512], F32)
              pG2 = mkp(st, "pG2", [128, 512], F32)
              pPA = mkp(st, "pPA", [128, 512], F32)
              pPB = mkp(st, "pPB", [128, 512], F32)
              pO = [mkp(st, "pO%d" % i, [128, 512], F32) for i in range(2)]
              wg_v = w_gate_d.rearrange("(k p) c -> p k c", p=128)
              wa_v = w_pa_d.rearrange("(k p) c -> p k c", p=128)
              wb_v = w_pb_d.rearrange("(k p) c -> p k c", p=128)
              wo_v = w_out_d.rearrange("(k p) c -> p k c", p=128)
              for hf in range(2):
                  t0 = hf * 1024
                  for kk in range(4):
                      S.dma("sp", lambda e, kk=kk, t0=t0: e.dma_start(out=oaTh[:, kk * 4:(kk + 1) * 4, :], in_=oaT_d[s, :, kk * 4:(kk + 1) * 4, t0:t0 + 1024]),
                            reads=[oaT_b], writes=[oaTh])
                  S.dma("sp", lambda e, t0=t0: e.dma_start(out=obTh[:], in_=obT_d[s, :, :, t0:t0 + 1024]), reads=[obT_b], writes=[obTh])
                  for c in range(16):
                      w1_, w2_, wa_, wb_ = Wg1[c % 2], Wg2[c % 2], Wa[c % 2], Wb[c % 2]
                      wload(w1_, lambda a, b, w=w1_: w[:, a:b, :], wg_v[:, :, c * 128:(c + 1) * 128], 16, 2)
                      wload(w2_, lambda a, b, w=w2_: w[:, a:b, :], wg_v[:, :, 2048 + c * 128:2048 + (c + 1) * 128], 16, 2)
                      wload(wa_, lambda a, b, w=wa_: w[:, a:b, :], wa_v[:, :, c * 128:(c + 1) * 128], 16, 2)
                      wload(wb_, lambda a, b, w=wb_: w[:, a:b, :], wb_v[:, :, c * 128:(c + 1) * 128], 4, 1)
                      for tg in range(2):
                          ta = t0 + tg * 512
                          for bank, Wt, src, off_, nk in ((pG1, w1_, hT, ta, 16), (pG2, w2_, hT, ta, 16), (pPA, wa_, oaTh, tg * 512, 16), (pPB, wb_, obTh, tg * 512, 4)):
                              for k in range(nk):
                                  S.op("pe", lambda e, bank=bank, Wt=Wt, src=src, off_=off_, k=k, nk=nk: e.matmul(
                                      bank[:, 0:512], lhsT=Wt[:, k, :], rhs=src[:, k, off_:off_ + 512], start=(k == 0), stop=(k == nk - 1)),
                                      reads=[Wt, src], writes=[bank], sig=(k == nk - 1))
                          S.op("act", lambda e: e.activation(out=s1[:], in_=pG1[:, 0:512], func=AF.Sigmoid), reads=[pG1], writes=[s1])
                          S.op("act", lambda e: e.activation(out=s2[:], in_=pG2[:, 0:512], func=AF.Sigmoid), reads=[pG2], writes=[s2])
                          S.op("dve", lambda e: e.tensor_tensor(out=s1[:], in0=s1[:], in1=pPA[:, 0:512], op=ALU.mult), reads=[s1, pPA], writes=[s1])
                          S.op("dve", lambda e: e.tensor_tensor(out=s2[:], in0=s2[:], in1=pPB[:, 0:512], op=ALU.mult), reads=[s2, pPB], writes=[s2])
                          S.op("dve", lambda e, c=c, tg=tg: e.tensor_tensor(out=mT[:, c, tg * 512:(tg + 1) * 512], in0=s1[:], in1=s2[:], op=ALU.add),
                               reads=[s1, s2], writes=[mT])
                  n = 0
                  for cg in range(4):
                      wload(Wo, lambda a, b: Wo[:, a:b, :], wo_v[:, :, cg * 512:(cg + 1) * 512], 16, 4)
                      for tt in range(8):
                          r0 = s * SEQ + t0 + tt * 128
                          bank, xt_, x1_ = pO[n % 2], xt[n % 2], x1t[n % 2]
                          n += 1
                          S.dma("sp", lambda e, xt_=xt_, r0=r0, cg=cg: e.dma_start(out=xt_[:], in_=x_d[r0:r0 + 128, cg * 512:(cg + 1) * 512]), writes=[xt_])
                          for k in range(16):
                              S.op("pe", lambda e, bank=bank, k=k, tt=tt: e.matmul(bank[:, 0:512], lhsT=mT[:, k, tt * 128:(tt + 1) * 128], rhs=Wo[:, k, :],
                                                                                    start=(k == 0), stop=(k == 15)), reads=[mT, Wo], writes=[bank], sig=(k == 15))
                          S.op("dve", lambda e, bank=bank, xt_=xt_, x1_=x1_: e.tensor_tensor(out=x1_[:], in0=bank[:, 0:512], in1=xt_[:], op=ALU.add),
                               reads=[bank, xt_], writes=[x1_])
                          S.dma("sp", lambda e, x1_=x1_, r0=r0, cg=cg: e.dma_start(out=x1_d[r0:r0 + 128, cg * 512:(cg + 1) * 512], in_=x1_[:]),
                                reads=[x1_], writes=[x1_b])
              S.barrier()
        hstack.close()

    with ExitStack() as st:
        stage = [mk(st, "rstage%d" % i, [128, D], F32) for i in range(2)]
        junk = mk(st, "rjunk", [128, D], BF16)
        gb = mk(st, "rgb", [128, D], F32)
        h2 = [mk(st, "h2_%d" % i, [128, D], F32) for i in range(2)]
        h2b = [mk(st, "h2b%d" % i, [128, D], BF16) for i in range(2)]
        h2T = [mk(st, "h2T%d" % i, [128, 16, 128], F32) for i in range(2)]
        Wr = mk(st, "Wr", [128, 16, 36], F32)
        ssr = [mk(st, "rss%d" % i, [128, 1], F32) for i in range(2)]
        t1r = [mk(st, "rt1%d" % i, [128, 1], F32) for i in range(2)]
        rstdr = [mk(st, "rrstd%d" % i, [128, 1], F32) for i in range(2)]
        lgs = [mk(st, "lg%d" % i, [128, 36], F32) for i in range(2)]
        sms = [{n: mk(st, "r%d_%s" % (i, n), [128, w], F32) for n, w in (
            ("cmax", 1), ("ncm", 1), ("ohg", 4), ("ce", 4), ("csum", 1), ("pg", 1), ("fsel", 8), ("v1", 1), ("oh1", 8), ("fm", 8),
            ("v2", 1), ("oh2", 8), ("dl", 1), ("ed", 1), ("den", 1), ("rden", 1), ("A1", 32), ("A2", 32), ("As", 32), ("pos", 32),
            ("ovf", 32), ("sl", 32), ("tmp", 32), ("sf", 2))} for i in range(2)]
        Asbs = [mk(st, "Asb%d" % i, [128, 32], BF16) for i in range(2)]
        ptf = [mkp(st, "ptf%d" % i, [128, 512], F32) for i in range(4)]
        plgs = [mkp(st, "plg%d" % i, [128, 512], F32) for i in range(2)]
        pcns = [mkp(st, "pcn%d" % i, [128, 512], F32) for i in range(2)]
        S.dma("sp", lambda e: e.dma_start(out=gb[:], in_=g_ffn_d.partition_broadcast(128)), writes=[gb])
        S.dma("sp", lambda e: e.dma_start(out=Wr[:], in_=w_rt_d.rearrange("(k p) c -> p k c", p=128)), writes=[Wr])

        def dv(fn, reads, writes):
            S.op("dve", fn, reads=reads, writes=writes)

        def r_front(i):
            stg = stage[i % 2]
            hb = h2b[i % 2]
            h2_ = h2[i % 2]
            h2T_ = h2T[i % 2]
            ss, t1, rstd = ssr[i % 2], t1r[i % 2], rstdr[i % 2]
            plg = plgs[i % 2]
            S.dma("sp", lambda e, stg=stg, i=i: e.dma_start(out=stg[:], in_=x1_d[i * 128:(i + 1) * 128, :]), reads=[x1_b], writes=[stg])
            S.op("act", lambda e, stg=stg: e.activation(out=junk[:], in_=stg[:], func=AF.Square, accum_out=ss[:]), reads=[stg], writes=[junk, ss])
            rstd_ops(ss, t1, rstd, 1.0 / D, 1e-6)
            dv(lambda e, stg=stg: e.scalar_tensor_tensor(out=h2_[:], in0=stg[:], scalar=rstd[:], in1=gb[:], op0=ALU.mult, op1=ALU.mult), [stg, rstd, gb], [h2_])
            S.op("act", lambda e, hb=hb: e.activation(out=hb[:], in_=h2_[:], func=AF.Copy), reads=[h2_], writes=[hb])
            for k in range(16):
                S.op("pe", lambda e, k=k: e.transpose(ptf[k // 4][:, (k % 4) * 128:(k % 4 + 1) * 128], h2_[:, k * 128:(k + 1) * 128], identf[:]),
                     reads=[h2_, identf], writes=[ptf[k // 4]], sig=(k % 4 == 3))
            for j in range(4):
                evac(h2T_[:, 4 * j:4 * j + 4, :], ptf[j][:].rearrange("p (k t) -> p k t", k=4), [ptf[j]], [h2T_])
            for k in range(16):
                S.op("pe", lambda e, k=k: e.matmul(plg[:, 0:36], lhsT=h2T_[:, k, :], rhs=Wr[:, k, :], start=(k == 0), stop=(k == 15)),
                     reads=[h2T_, Wr], writes=[plg], sig=(k == 15))

        def r_back(i):
            hb = h2b[i % 2]
            m = sms[i % 2]
            lg = lgs[i % 2]
            Asb = Asbs[i % 2]
            plg = plgs[i % 2]
            pcn = pcns[i % 2]
            dv(lambda e: e.tensor_copy(out=lg[:], in_=plg[:, 0:36]), [plg], [lg])
            fine = lg[:, 4:36].rearrange("p (g j) -> p g j", g=4)
            dv(lambda e: e.reduce_max(out=m["cmax"][:], in_=lg[:, 0:4], axis=AX.X), [lg], [m["cmax"]])
            dv(lambda e: e.tensor_scalar(out=m["ohg"][:], in0=lg[:, 0:4], scalar1=m["cmax"][:], scalar2=None, op0=ALU.is_equal), [lg, m["cmax"]], [m["ohg"]])
            dv(lambda e: e.tensor_scalar(out=m["ncm"][:], in0=m["cmax"][:], scalar1=-1.0, scalar2=None, op0=ALU.mult), [m["cmax"]], [m["ncm"]])
            S.op("act", lambda e: e.activation(out=m["ce"][:], in_=lg[:, 0:4], func=AF.Exp, bias=m["ncm"][:], accum_out=m["csum"][:]),
                 reads=[lg, m["ncm"]], writes=[m["ce"], m["csum"]])
            dv(lambda e: e.reciprocal(out=m["pg"][:], in_=m["csum"][:]), [m["csum"]], [m["pg"]])
            dv(lambda e: e.tensor_scalar(out=m["fsel"][:], in0=fine[:, 0, :], scalar1=m["ohg"][:, 0:1], scalar2=None, op0=ALU.mult), [lg, m["ohg"]], [m["fsel"]])
            for g in range(1, 4):
                dv(lambda e, g=g: e.scalar_tensor_tensor(out=m["fsel"][:], in0=fine[:, g, :], scalar=m["ohg"][:, g:g + 1], in1=m["fsel"][:], op0=ALU.mult, op1=ALU.add),
                   [lg, m["ohg"], m["fsel"]], [m["fsel"]])
            dv(lambda e: e.reduce_max(out=m["v1"][:], in_=m["fsel"][:], axis=AX.X), [m["fsel"]], [m["v1"]])
            dv(lambda e: e.tensor_scalar(out=m["oh1"][:], in0=m["fsel"][:], scalar1=m["v1"][:], scalar2=None, op0=ALU.is_equal), [m["fsel"], m["v1"]], [m["oh1"]])
            dv(lambda e: e.scalar_tensor_tensor(out=m["fm"][:], in0=m["oh1"][:], scalar=-1e30, in1=m["fsel"][:], op0=ALU.mult, op1=ALU.add),
               [m["oh1"], m["fsel"]], [m["fm"]])
            dv(lambda e: e.reduce_max(out=m["v2"][:], in_=m["fm"][:], axis=AX.X), [m["fm"]], [m["v2"]])
            dv(lambda e: e.tensor_scalar(out=m["oh2"][:], in0=m["fm"][:], scalar1=m["v2"][:], scalar2=None, op0=ALU.is_equal), [m["fm"], m["v2"]], [m["oh2"]])
            dv(lambda e: e.tensor_tensor(out=m["dl"][:], in0=m["v2"][:], in1=m["v1"][:], op=ALU.subtract), [m["v1"], m["v2"]], [m["dl"]])
            S.op("act", lambda e: e.activation(out=m["ed"][:], in_=m["dl"][:], func=AF.Exp), reads=[m["dl"]], writes=[m["ed"]])
            dv(lambda e: e.tensor_scalar(out=m["den"][:], in0=m["ed"][:], scalar1=1.0, scalar2=None, op0=ALU.add), [m["ed"]], [m["den"]])
            dv(lambda e: e.reciprocal(out=m["rden"][:], in_=m["den"][:]), [m["den"]], [m["rden"]])
            dv(lambda e: e.tensor_tensor(out=gates[:, i, 0:1], in0=m["pg"][:], in1=m["rden"][:], op=ALU.mult), [m["pg"], m["rden"]], [gates])
            dv(lambda e: e.tensor_tensor(out=gates[:, i, 1:2], in0=gates[:, i, 0:1], in1=m["ed"][:], op=ALU.mult), [gates, m["ed"]], [gates])
            for g in range(4):
                dv(lambda e, g=g: e.tensor_scalar(out=m["A1"][:, g * 8:(g + 1) * 8], in0=m["oh1"][:], scalar1=m["ohg"][:, g:g + 1], scalar2=None, op0=ALU.mult),
                   [m["oh1"], m["ohg"]], [m["A1"]])
                dv(lambda e, g=g: e.tensor_scalar(out=m["A2"][:, g * 8:(g + 1) * 8], in0=m["oh2"][:], scalar1=m["ohg"][:, g:g + 1], scalar2=None, op0=ALU.mult),
                   [m["oh2"], m["ohg"]], [m["A2"]])
            dv(lambda e: e.tensor_tensor(out=m["As"][:], in0=m["A1"][:], in1=m["A2"][:], op=ALU.add), [m["A1"], m["A2"]], [m["As"]])
            dv(lambda e: e.tensor_copy(out=Asb[:], in_=m["As"][:]), [m["As"]], [Asb])
            S.op("pe", lambda e: e.matmul(pcn[:, 0:32], lhsT=trib[:], rhs=Asb[:], start=True, stop=True), reads=[trib, Asb], writes=[pcn], sig=False)
            S.op("pe", lambda e: e.matmul(pcn[:, 32:64], lhsT=onesb[:], rhs=Asb[:], start=True, stop=True), reads=[onesb, Asb], writes=[pcn])
            dv(lambda e: e.tensor_tensor(out=m["pos"][:], in0=pcn[:, 0:32], in1=cbase[:], op=ALU.add), [pcn, cbase], [m["pos"]])
            dv(lambda e: e.tensor_tensor(out=cbase[:], in0=pcn[:, 32:64], in1=cbase[:], op=ALU.add), [pcn, cbase], [cbase])
            dv(lambda e: e.tensor_scalar(out=m["ovf"][:], in0=m["pos"][:], scalar1=float(CAP), scalar2=None, op0=ALU.is_ge), [m["pos"]], [m["ovf"]])
            dv(lambda e: e.scalar_tensor_tensor(out=m["sl"][:], in0=m["ovf"][:], scalar=1.0e7, in1=m["pos"][:], op0=ALU.mult, op1=ALU.add),
               [m["ovf"], m["pos"]], [m["sl"]])
            dv(lambda e: e.tensor_tensor(out=m["sl"][:], in0=m["sl"][:], in1=ecrow[:], op=ALU.add), [m["sl"], ecrow], [m["sl"]])
            for kx, An in ((0, "A1"), (1, "A2")):
                dv(lambda e, An=An: e.tensor_tensor(out=m["tmp"][:], in0=m[An][:], in1=m["sl"][:], op=ALU.mult), [m[An], m["sl"]], [m["tmp"]])
                dv(lambda e, kx=kx: e.reduce_sum(out=m["sf"][:, kx:kx + 1], in_=m["tmp"][:], axis=AX.X), [m["tmp"]], [m["sf"]])
            dv(lambda e: e.tensor_copy(out=slots[:, 2 * i:2 * i + 2], in_=m["sf"][:]), [m["sf"]], [slots])
            dv(lambda e: e.tensor_scalar(out=m["sf"][:], in0=m["sf"][:], scalar1=float(NSLOT), scalar2=None, op0=ALU.min), [m["sf"]], [m["sf"]])
            dv(lambda e: e.tensor_copy(out=slotg[:, 2 * i:2 * i + 2], in_=m["sf"][:]), [m["sf"]], [slotg])
            for kx in range(2):
                S.dma("pool", lambda e, kx=kx, hb=hb: e.indirect_dma_start(
                    out=xin_d, out_offset=bass.IndirectOffsetOnAxis(ap=slots[:, 2 * i + kx:2 * i + kx + 1], axis=0), in_=hb[:], in_offset=None,
                    bounds_check=bc_reg, oob_is_err=False), reads=[hb, slots], writes=[xin_b])

        ntr = cfg.get('ntR', NT)
        LAG = 3
        for i0 in range(0, ntr, 2):
            tiles = [i0] + ([i0 + 1] if i0 + 1 < ntr else [])
            for i in tiles:
                r_front(i)
            lists = [S.record(lambda i=i: r_back(i)) for i in tiles]
            pos_ = [0] * len(lists)
            step = 0
            while any(pos_[k] < len(lists[k]) for k in range(len(lists))):
                for k in range(len(lists)):
                    if pos_[k] >= len(lists[k]) or (k == 1 and step < LAG and pos_[0] < len(lists[0])):
                        continue
                    S.replay(lists[k][pos_[k]])
                    pos_[k] += 1
                step += 1
        S.barrier()

    with ExitStack() as st:
        xtok = [mk(st, "xtok%d" % i, [128, D], BF16) for i in range(4)]
        XTs = [mk(st, "XT%d" % i, [128, 16, CAP], BF16) for i in range(2)]
        W13 = [mk(st, "W13_%d" % i, [128, 16, 512], BF16) for i in range(4)]
        W2s = [mk(st, "W2s_%d" % i, [128, 8, 512], BF16) for i in range(4)]
        W2f = [mk(st, "W2f_%d" % i, [128, 8, 512], F32) for i in range(2)]
        HT = mk(st, "HT", [128, 8, CAP], BF16)
        sl1 = mk(st, "sl1", [128, CCH], F32)
        yt = [mk(st, "yt%d" % i, [128, 512], F32) for i in range(4)]
        ptb = [mkp(st, "eptb%d" % i, [128, 1024], BF16) for i in range(2)]
        pb1 = [mkp(st, "pb1_%d" % i, [128, 512], F32) for i in range(2)]
        pb3 = [mkp(st, "pb3_%d" % i, [128, 512], F32) for i in range(2)]
        py = [mkp(st, "py%d" % i, [128, 512], F32) for i in range(2)]
        NTI = CAP // 128
        nE = cfg.get('nE', NE)
        S.op("dve", lambda e: e.memset(yt[0][:], 0.0), writes=[yt[0]])
        for cg in range(4):
            S.dma("sp", lambda e, cg=cg: e.dma_start(out=ybuf_d[NSLOT:NSLOT + 128, cg * 512:(cg + 1) * 512], in_=yt[0][:]), reads=[yt[0]], writes=[ybuf_b])
        n13 = 0
        n2 = 0
        ny = 0
        nxc = [0]

        def x_loads(ex):
            for ti in range(NTI):
                xk = xtok[ti % 4]
                r0 = ex * CAP + ti * 128
                S.dma("sp", lambda e, xk=xk, r0=r0: e.dma_start(out=xk[:], in_=xin_d[r0:r0 + 128, :]), reads=[xin_b], writes=[xk])

        def t_chunks(ex):
            XTe = XTs[ex % 2]
            chunks = []
            for ti in range(NTI):
                for j in range(2):
                    def chunk(ti=ti, j=j):
                        xk = xtok[ti % 4]
                        for k in range(8 * j, 8 * j + 8):
                            S.op("pe", lambda e, k=k: e.transpose(ptb[j][:, (k % 8) * 128:(k % 8 + 1) * 128], xk[:, k * 128:(k + 1) * 128], identb[:]),
                                 reads=[xk, identb], writes=[ptb[j]], sig=(k % 8 == 7))
                        evac(XTe[:, 8 * j:8 * j + 8, ti * 128:(ti + 1) * 128], ptb[j][:].rearrange("p (k t) -> p k t", k=8), [ptb[j]], [XTe])
                    chunks.append(chunk)
            return chunks

        if nE:
            x_loads(0)
            for ch in t_chunks(0):
                ch()
        for ex in range(nE):
            XT = XTs[ex % 2]
            w1v = w1_d[ex].rearrange("(k p) c -> p k c", p=128)
            w3v = w3_d[ex].rearrange("(k p) c -> p k c", p=128)
            w2v = w2_d[ex].rearrange("(k p) c -> p k c", p=128)
            w2slot = [W2s[(n2 + cg) % 4] for cg in range(4)]

            def w2_load(cg):
                stg_ = W2f[cg % 2]
                for hh_ in range(2):
                    S.dma("sp", lambda e, hh_=hh_: e.dma_start(out=stg_[:, hh_ * 4:(hh_ + 1) * 4, :], in_=w2v[:, hh_ * 4:(hh_ + 1) * 4, cg * 512:(cg + 1) * 512]),
                          writes=[stg_])

            def w2_cast(cg):
                stg_ = W2f[cg % 2]
                dst_ = w2slot[cg]
                if cg % 2 == 0:
                    S.op("act", lambda e: e.activation(out=dst_[:], in_=stg_[:], func=AF.Copy), reads=[stg_], writes=[dst_])
                else:
                    S.op("dve", lambda e: e.tensor_copy(out=dst_[:], in_=stg_[:]), reads=[stg_], writes=[dst_])

            w2_load(0)
            w2_load(1)
            for hf in range(2):
                if hf == 1 and ex + 1 < nE:
                    x_loads(ex + 1)
                wa_ = W13[n13 % 4]
                wb_ = W13[(n13 + 1) % 4]
                n13 += 2
                wload(wa_, lambda a, b, w=wa_: w[:, a:b, :], w1v[:, :, hf * 512:(hf + 1) * 512], 16, 4)
                wload(wb_, lambda a, b, w=wb_: w[:, a:b, :], w3v[:, :, hf * 512:(hf + 1) * 512], 16, 4)
                for hc in range(4):
                    for ci in range(CAP // CCH):
                        b1, b3 = pb1[ci % 2], pb3[ci % 2]
                        for bank, Wt in ((b1, wa_), (b3, wb_)):
                            for k in range(16):
                                S.op("pe", lambda e, bank=bank, Wt=Wt, k=k, hc=hc, ci=ci: e.matmul(
                                    bank[:, 0:CCH], lhsT=Wt[:, k, hc * 128:(hc + 1) * 128], rhs=XT[:, k, ci * CCH:(ci + 1) * CCH],
                                    start=(k == 0), stop=(k == 15)), reads=[Wt, XT], writes=[bank], sig=(k == 15))
                        S.op("act", lambda e, b1=b1: e.activation(out=sl1[:], in_=b1[:, 0:CCH], func=AF.Silu), reads=[b1], writes=[sl1])
                        S.op("dve", lambda e, hf=hf, hc=hc, ci=ci, b3=b3: e.tensor_tensor(out=HT[:, hf * 4 + hc, ci * CCH:(ci + 1) * CCH], in0=sl1[:], in1=b3[:, 0:CCH], op=ALU.mult),
                             reads=[sl1, b3], writes=[HT])
                    if hc in (1, 3):
                        cgc = hf * 2 + (hc // 2)
                        w2_cast(cgc)
                        if cgc + 2 < 4:
                            w2_load(cgc + 2)
            nxt = t_chunks(ex + 1) if ex + 1 < nE else []
            it_ = 0
            for cg in range(4):
                w2_ = w2slot[cg]
                n2 += 1
                for ti in range(NTI):
                    bank, y_ = py[ny % 2], yt[ny % 4]
                    ny += 1
                    for k in range(8):
                        S.op("pe", lambda e, bank=bank, w2_=w2_, k=k, ti=ti: e.matmul(bank[:, 0:512], lhsT=HT[:, k, ti * 128:(ti + 1) * 128], rhs=w2_[:, k, :],
                                                                                      start=(k == 0), stop=(k == 7)), reads=[HT, w2_], writes=[bank], sig=(k == 7))
                    evac(y_[:], bank[:, 0:512], [bank], [y_])
                    r0 = ex * CAP + ti * 128
                    S.dma("sp", lambda e, y_=y_, r0=r0, cg=cg: e.dma_start(out=ybuf_d[r0:r0 + 128, cg * 512:(cg + 1) * 512], in_=y_[:]),
                          reads=[y_], writes=[ybuf_b])
                    it_ += 1
                    if it_ % 2 == 0 and nxt:
                        nxt.pop(0)()
            while nxt:
                nxt.pop(0)()
        S.barrier()

    with ExitStack() as st:
        Wpg = mk(st, "Wpg", [128, 16, D], BF16)
        Wpp = mk(st, "Wpp", [128, 2, D], BF16)
        gbp = mk(st, "gbp", [128, D], F32)
        gbf = mk(st, "gbf", [128, D], F32)
        junk = mk(st, "fjunk", [128, D], BF16)
        stage = [mk(st, "fstage%d" % i, [128, D], F32) for i in range(2)]
        Y1s = [mk(st, "Y1_%d" % i, [128, D], F32) for i in range(2)]
        Y2s = [mk(st, "Y2_%d" % i, [128, D], F32) for i in range(2)]
        x3s = [mk(st, "x3_%d" % i, [128, D], F32) for i in range(2)]
        h3s = [mk(st, "h3_%d" % i, [128, D], BF16) for i in range(2)]
        h3Ts = [mk(st, "h3T%d" % i, [128, 16, 128], BF16) for i in range(2)]
        ptls = [mk(st, "ptl%d" % i, [128, 256], F32) for i in range(2)]
        pbls = [mk(st, "pbl%d" % i, [128, 256], BF16) for i in range(2)]
        pTls = [mk(st, "pTl%d" % i, [128, 2, 128], BF16) for i in range(2)]
        sgs = [mk(st, "sg%d" % i, [128, 512], F32) for i in range(2)]
        ssa = [mk(st, "fssa%d" % i, [128, 1], F32) for i in range(2)]
        t1a = [mk(st, "ft1a%d" % i, [128, 1], F32) for i in range(2)]
        rsa = [mk(st, "frsa%d" % i, [128, 1], F32) for i in range(2)]
        ssb = [mk(st, "fssb%d" % i, [128, 1], F32) for i in range(2)]
        t1b = [mk(st, "ft1b%d" % i, [128, 1], F32) for i in range(2)]
        rsb = [mk(st, "frsb%d" % i, [128, 1], F32) for i in range(2)]
        ptb = [mkp(st, "fptb%d" % i, [128, 1024], BF16) for i in range(2)]
        ptp = mkp(st, "fptp", [128, 1024], BF16)
        pG = [mkp(st, "fpG%d" % i, [128, 512], F32) for i in range(2)]
        pP = [mkp(st, "fpP%d" % i, [128, 512], F32) for i in range(2)]
        wload(Wpg, lambda a, b: Wpg[:, a:b, :], w_pg_d.rearrange("(k p) c -> p k c", p=128), 16, 16)
        wload(Wpp, lambda a, b: Wpp[:, a:b, :], w_pp_d.rearrange("(k p) c -> p k c", p=128), 2, 2)
        S.dma("sp", lambda e: e.dma_start(out=gbp[:], in_=g_ple_d.partition_broadcast(128)), writes=[gbp])
        S.dma("sp", lambda e: e.dma_start(out=gbf[:], in_=g_fin_d.partition_broadcast(128)), writes=[gbf])
        fn_ = [0]
        ntf = cfg.get('ntF', NT)

        def f_front(i):
            stg, Y1, Y2, h3, ptl, pbl = stage[i % 2], Y1s[i % 2], Y2s[i % 2], h3s[i % 2], ptls[i % 2], pbls[i % 2]
            ss, t1, rstd = ssa[i % 2], t1a[i % 2], rsa[i % 2]
            S.dma("sp", lambda e: e.dma_start(out=stg[:], in_=x1_d[i * 128:(i + 1) * 128, :]), reads=[x1_b], writes=[stg])
            S.dma("sp", lambda e: e.dma_start(out=ptl[:], in_=p_d[i * 128:(i + 1) * 128, :]), writes=[ptl])
            for kx, Y in ((0, Y1), (1, Y2)):
                S.dma("pool", lambda e, kx=kx, Y=Y: e.indirect_dma_start(
                    out=Y[:], out_offset=None, in_=ybuf_d, in_offset=bass.IndirectOffsetOnAxis(ap=slotg[:, 2 * i + kx:2 * i + kx + 1], axis=0),
                    bounds_check=bc_reg2, oob_is_err=False), reads=[ybuf_b, slotg], writes=[Y])

        def f_prep(i):
            stg, Y1, Y2, h3, ptl, pbl = stage[i % 2], Y1s[i % 2], Y2s[i % 2], h3s[i % 2], ptls[i % 2], pbls[i % 2]
            ss, t1, rstd = ssa[i % 2], t1a[i % 2], rsa[i % 2]
            S.op("dve", lambda e: e.scalar_tensor_tensor(out=Y1[:], in0=Y1[:], scalar=gates[:, i, 0:1], in1=stg[:], op0=ALU.mult, op1=ALU.add),
                 reads=[Y1, gates, stg], writes=[Y1])
            S.op("dve", lambda e: e.scalar_tensor_tensor(out=Y1[:], in0=Y2[:], scalar=gates[:, i, 1:2], in1=Y1[:], op0=ALU.mult, op1=ALU.add),
                 reads=[Y2, gates, Y1], writes=[Y1])
            S.op("act", lambda e: e.activation(out=junk[:], in_=Y1[:], func=AF.Square, accum_out=ss[:]), reads=[Y1], writes=[junk, ss])
            rstd_ops(ss, t1, rstd, 1.0 / D, 1e-6)
            S.op("dve", lambda e: e.scalar_tensor_tensor(out=h3[:], in0=Y1[:], scalar=rstd[:], in1=gbp[:], op0=ALU.mult, op1=ALU.mult),
                 reads=[Y1, rstd, gbp], writes=[h3])
            S.op("act", lambda e: e.activation(out=pbl[:], in_=ptl[:], func=AF.Copy), reads=[ptl], writes=[pbl])

        def f_T(i):
            h3, h3T, pbl, pTl = h3s[i % 2], h3Ts[i % 2], pbls[i % 2], pTls[i % 2]
            for k in range(16):
                S.op("pe", lambda e, k=k: e.transpose(ptb[k // 8][:, (k % 8) * 128:(k % 8 + 1) * 128], h3[:, k * 128:(k + 1) * 128], identb[:]),
                     reads=[h3, identb], writes=[ptb[k // 8]], sig=(k % 8 == 7))
            for j in range(2):
                evac(h3T[:, 8 * j:8 * j + 8, :], ptb[j][:].rearrange("p (k t) -> p k t", k=8), [ptb[j]], [h3T])
            for k in range(2):
                S.op("pe", lambda e, k=k: e.transpose(ptp[:, k * 128:(k + 1) * 128], pbl[:, k * 128:(k + 1) * 128], identb[:]),
                     reads=[pbl, identb], writes=[ptp], sig=(k == 1))
            evac(pTl[:], ptp[:, 0:256].rearrange("p (k t) -> p k t", k=2), [ptp], [pTl])

        def f_back(i):
            x2, x3, h3, h3T, pbl, pTl, sg = Y1s[i % 2], x3s[i % 2], h3s[i % 2], h3Ts[i % 2], pbls[i % 2], pTls[i % 2], sgs[i % 2]
            ss, t1, rstd = ssb[i % 2], t1b[i % 2], rsb[i % 2]
            for cg in range(4):
                bG, bP = pG[fn_[0] % 2], pP[fn_[0] % 2]
                fn_[0] += 1
                for k in range(16):
                    S.op("pe", lambda e, bG=bG, k=k, cg=cg: e.matmul(bG[:, 0:512], lhsT=h3T[:, k, :], rhs=Wpg[:, k, cg * 512:(cg + 1) * 512],
                                                                     start=(k == 0), stop=(k == 15)), reads=[h3T, Wpg], writes=[bG], sig=(k == 15))
                for k in range(2):
                    S.op("pe", lambda e, bP=bP, k=k, cg=cg: e.matmul(bP[:, 0:512], lhsT=pTl[:, k, :], rhs=Wpp[:, k, cg * 512:(cg + 1) * 512],
                                                                     start=(k == 0), stop=(k == 1)), reads=[pTl, Wpp], writes=[bP], sig=(k == 1))
                S.op("act", lambda e, bG=bG: e.activation(out=sg[:], in_=bG[:, 0:512], func=AF.Sigmoid), reads=[bG], writes=[sg])
                S.op("dve", lambda e, bP=bP: e.tensor_tensor(out=sg[:], in0=sg[:], in1=bP[:, 0:512], op=ALU.mult), reads=[sg, bP], writes=[sg])
                S.op("dve", lambda e, cg=cg: e.tensor_tensor(out=x3[:, cg * 512:(cg + 1) * 512], in0=sg[:], in1=x2[:, cg * 512:(cg + 1) * 512], op=ALU.add),
                     reads=[sg, x2], writes=[x3])
                if cg == 0 and i + 1 < ntf:
                    f_prep(i + 1)
            if i + 1 < ntf:
                f_T(i + 1)
            S.op("act", lambda e: e.activation(out=junk[:], in_=x3[:], func=AF.Square, accum_out=ss[:]), reads=[x3], writes=[junk, ss])
            rstd_ops(ss, t1, rstd, 1.0 / D, 1e-6)
            S.op("dve", lambda e: e.scalar_tensor_tensor(out=x3[:], in0=x3[:], scalar=rstd[:], in1=gbf[:], op0=ALU.mult, op1=ALU.mult),
                 reads=[x3, rstd, gbf], writes=[x3])
            S.dma("sp", lambda e: e.dma_start(out=out_d[i * 128:(i + 1) * 128, :], in_=x3[:]), reads=[x3], writes=[out_b])

        if ntf:
            f_front(0)
            f_prep(0)
            f_T(0)
        for i in range(ntf):
            if i + 1 < ntf:
                f_front(i + 1)
            f_back(i)
        S.barrier()
    S.finish()
    gstack.close()
    return nc


def _t5_bucket(dist):
    dist = np.maximum(dist, 0)
    d_f = np.maximum(dist, 1).astype(np.float32)
    large = 16 + (np.log(d_f / np.float32(16)) / np.float32(math.log(2048 / 16)) * np.float32(16)).astype(np.int32)
    large = np.minimum(large, 31)
    return np.where(dist < 16, dist, large)


def _bias_layouts(rel_bias):
    rb = np.asarray(rel_bias, np.float32)
    NEG = np.float32(-1e30)
    k = np.arange(128)[:, None]
    col = np.arange(2048)[None, :]
    dist = col - k
    bk = _t5_bucket(dist)
    ba = np.empty((8, 128, 2048), np.float32)
    for h in range(8):
        ba[h] = np.where(dist >= 0, rb[bk, h], NEG)
    bb = np.empty((6, 128, 4, 2, 128), np.float32)
    q = np.arange(128)[None, :]
    for quad in range(2):
        for g in range(3):
            d = DIL[g][1]
            for hl in range(4):
                col_h = 8 + g * 8 + quad * 4 + hl
                rel_c = q - k
                rel_p = q - k + 128
                bb[quad * 3 + g, :, hl, 0, :] = np.where(rel_c >= 0, rb[_t5_bucket(rel_c * d), col_h], NEG)
                bb[quad * 3 + g, :, hl, 1, :] = np.where(rel_p <= 128, rb[_t5_bucket(rel_p * d), col_h], NEG)
    return ba, bb.reshape(6, 128, 1024)


_NC = None


def prep_inputs(x, p, rel_bias, norm_mix_g, w_in, w_gate, lambda_q1, lambda_k1, lambda_q2, lambda_k2, subln_g,
                w_proj_a, w_proj_b, w_out, norm_ffn_g, w_coarse, w_fine, w1, w3, w2, norm_ple_g, w_ple_gate,
                w_ple_proj, final_norm_g):
    f = lambda a: np.ascontiguousarray(np.asarray(a, dtype=np.float32))
    x = f(x).reshape(NCORES, T, D)
    p = f(p)[0].reshape(NCORES, T, 256)
    ba, bb = _bias_layouts(rel_bias)
    w_router = np.ascontiguousarray(np.concatenate(
        [f(w_coarse)[0], np.transpose(f(w_fine)[0], (1, 0, 2)).reshape(D, 32)], axis=1))
    tri = (np.arange(128)[:, None] < np.arange(128)[None, :]).astype(np.float32)
    ecrow = np.ascontiguousarray(np.broadcast_to((np.arange(NE, dtype=np.float32) * CAP)[None, :], (128, NE)))
    lam = np.ascontiguousarray(np.stack([f(lambda_q1)[0], f(lambda_k1)[0], f(lambda_q2)[0], f(lambda_k2)[0]], 0))
    shared = {
        "bias_a": ba, "bias_b": bb,
        "norm_mix_g": f(norm_mix_g)[0], "norm_ffn_g": f(norm_ffn_g)[0], "norm_ple_g": f(norm_ple_g)[0],
        "final_norm_g": f(final_norm_g),
        "w_in": f(w_in)[0], "w_gate": f(w_gate)[0], "lam": lam, "subln_g": f(subln_g)[0],
        "w_proj_a": f(w_proj_a)[0], "w_proj_b": f(w_proj_b)[0], "w_out": f(w_out)[0], "w_router": w_router,
        "w1": f(w1)[0], "w3": f(w3)[0], "w2": f(w2)[0], "w_ple_gate": f(w_ple_gate)[0], "w_ple_proj": f(w_ple_proj)[0],
        "identf": np.eye(128, dtype=np.float32), "tri": tri, "ecrow": ecrow,
    }
    in_maps = []
    for c in range(NCORES):
        m = dict(shared)
        m["x"] = np.ascontiguousarray(x[c])
        m["p"] = np.ascontiguousarray(p[c])
        in_maps.append(m)
    return in_maps


def kernel(**inputs):
    global _NC
    in_maps = prep_inputs(**inputs)
    if _NC is None:
        _NC = build_nc()
    res = run_bass_kernel_spmd(_NC, in_maps, core_ids=list(range(NCORES)))
    out = np.stack([np.asarray(r["out"], dtype=np.float32) for r in res.results], 0)
    return out.reshape(16, SEQ, D)
```
